# Optimizing a Trainium2 kernel written in Bass

```python
import math
import jax, jax.numpy as jnp
from jax import lax
import numpy as np

D_MODEL = 1024
BATCH = 8
SEQ = 4096
DEPTH = 2

GRID_W = 64
CTX_LEN = 256
EPS = 1e-6

A_HEADS = 4
A_DK = 128
A_DV = 128
A_WIDTH = A_HEADS * A_DK
A_VWIDTH = A_HEADS * A_DV
A_CHUNK = 64

B_GROUPS = 4
B_GDIM = 128
B_WIDTH = B_GROUPS * B_GDIM
B_CHUNK = 128

C_HEADS = 4
C_DH = 64
C_DV = 2 * C_DH
C_QK_WIDTH = C_HEADS * 2 * C_DH
C_WIDTH = C_HEADS * C_DV
Q_BLOCK = 128
ROPE_BASE = 10000.0
ROPE_PAIRS = C_DH // 4

N_BRANCH = 3
BRANCH_WIDTH = 512
IN_SIZES = [A_WIDTH, A_WIDTH, A_WIDTH, A_VWIDTH, A_VWIDTH,
            2 * B_WIDTH,
            C_QK_WIDTH, C_QK_WIDTH, C_WIDTH,
            N_BRANCH * D_MODEL]
IN_COLS = sum(IN_SIZES)

D_FF = 2816
N_EXPERTS = 8
TOP_K = 2
D_FF_E = 3584

kernel_name = "hybrid_dit_hgrn2_gmlp_diffattn_moe"


def rmsnorm(x, g):
    xf = x.astype(jnp.float32)
    y = xf * lax.rsqrt(jnp.mean(xf * xf, axis=-1, keepdims=True) + EPS)
    return (y * g.astype(jnp.float32)).astype(x.dtype)


def adaln(cvec, w, b):
    return jax.nn.silu(cvec) @ w + b


def modulate(hn, shift, scale):
    return hn * (1.0 + scale) + shift


def split_cols(p):
    idx = np.cumsum(IN_SIZES)[:-1].tolist()
    return jnp.split(p, idx, axis=-1)


def to_heads(a, h):
    bsz, n, _ = a.shape
    return a.reshape(bsz, n, h, -1).transpose(0, 2, 1, 3)


def lower_bound(p, layer):
    cs = jnp.cumsum(jax.nn.softmax(p.astype(jnp.float32), axis=0), axis=0)
    return cs[layer] - cs[0]


def forget_gate(z, lb):
    z = z.astype(jnp.float32)
    logf = jnp.logaddexp(jnp.log(lb), jnp.log1p(-lb) + jax.nn.log_sigmoid(z))
    k = (1.0 - lb) * jax.nn.sigmoid(-z)
    return logf, k


def hgrn2_chunk_scan(q, k, v, logf, s0):
    bsz, h, n, dk = q.shape
    dv = v.shape[-1]
    nc = n // A_CHUNK

    def to_chunks(a):
        return jnp.moveaxis(a.astype(jnp.float32).reshape(bsz, h, nc, A_CHUNK, a.shape[-1]), 2, 0)

    mask = jnp.tril(jnp.ones((A_CHUNK, A_CHUNK), dtype=bool))

    def step(S, inp):
        qc, kc, vc, lf = inp
        b = jnp.cumsum(lf, axis=2)
        rel = jnp.where(mask[:, :, None], b[:, :, :, None, :] - b[:, :, None, :, :], -jnp.inf)
        decay = jnp.exp(rel)
        att = jnp.einsum('bhtk,bhsk,bhtsk->bhts', qc, kc, decay)
        o = jnp.einsum('bhts,bhsv->bhtv', att, vc) + jnp.einsum('bhtk,bhkv->bhtv', qc * jnp.exp(b), S)
        b_end = b[:, :, -1:, :]
        S_new = jnp.exp(b_end)[:, :, 0, :, None] * S + jnp.einsum('bhsk,bhsv->bhkv', kc * jnp.exp(b_end - b), vc)
        return S_new, o

    S_fin, o = lax.scan(step, s0, (to_chunks(q), to_chunks(k), to_chunks(v), to_chunks(logf)))
    o = jnp.moveaxis(o, 0, 2).reshape(bsz, h, n, dv)
    return o.astype(v.dtype), S_fin


def hgrn2_direction(q_c, z_c, v_c, q_l, z_l, v_l, lb):
    logf_c, k_c = forget_gate(z_c, lb)
    logf_l, k_l = forget_gate(z_l, lb)
    s0 = jnp.zeros((q_c.shape[0], A_HEADS, A_DK, A_DV), jnp.float32)
    o_c, s_c = hgrn2_chunk_scan(q_c, k_c, v_c, logf_c, s0)
    o_l, _ = hgrn2_chunk_scan(q_l, k_l, v_l, logf_l, s_c)
    return o_c, o_l


def hgrn2_readout(o, og, g):
    bsz, h, n, dv = o.shape
    o = rmsnorm(o.transpose(0, 2, 1, 3), g.reshape(h, dv)).reshape(bsz, n, h * dv)
    return o * jax.nn.silu(og)


def hgrn2_branch(p_l, p_c, lb_f, lb_b, gnorm, with_ctx_out):
    q_l, zf_l, zb_l, v_l, og_l = p_l
    q_c, zf_c, zb_c, v_c, og_c = p_c
    hd = lambda a: to_heads(a, A_HEADS)
    rev = lambda a: jnp.flip(a, axis=2)
    lbf = lb_f.reshape(A_HEADS, 1, A_DK)
    lbb = lb_b.reshape(A_HEADS, 1, A_DK)
    qh_l, vh_l, qh_c, vh_c = hd(q_l), hd(v_l), hd(q_c), hd(v_c)
    of_c, of_l = hgrn2_direction(qh_c, hd(zf_c), vh_c, qh_l, hd(zf_l), vh_l, lbf)
    ob_c, ob_l = hgrn2_direction(rev(qh_c), rev(hd(zb_c)), rev(vh_c), rev(qh_l), rev(hd(zb_l)), rev(vh_l), lbb)
    y_l = hgrn2_readout(of_l + rev(ob_l), og_l, gnorm)
    y_c = hgrn2_readout(of_c + rev(ob_c), og_c, gnorm) if with_ctx_out else None
    return y_l, y_c


def chunk_mlp(uv, vnorm, ws, bs):
    u, v = jnp.split(jax.nn.gelu(uv), 2, axis=-1)
    v = rmsnorm(v, vnorm)
    bsz, n, _ = v.shape
    vc = v.reshape(bsz, n // B_CHUNK, B_CHUNK, B_GROUPS, B_GDIM)
    mixed = jnp.einsum('gts,bcsgd->bctgd', ws, vc) + bs.T[None, None, :, :, None]
    return u * mixed.reshape(bsz, n, B_WIDTH)


def axial_rope_tables(rows):
    t = jnp.arange(rows * GRID_W)
    row = (t // GRID_W).astype(jnp.float32)
    col = (t % GRID_W).astype(jnp.float32)
    freqs = ROPE_BASE ** (-jnp.arange(ROPE_PAIRS, dtype=jnp.float32) / ROPE_PAIRS)
    ang = jnp.stack([row[:, None] * freqs, col[:, None] * freqs], axis=1)
    return jnp.cos(ang), jnp.sin(ang)


def apply_rope(x, cos, sin):
    xs = x.reshape(x.shape[:-1] + (2, 2, ROPE_PAIRS))
    x1, x2 = xs[..., 0, :], xs[..., 1, :]
    c, s = cos.astype(x.dtype), sin.astype(x.dtype)
    out = jnp.stack([x1 * c - x2 * s, x1 * s + x2 * c], axis=-2)
    return out.reshape(x.shape)


def qk_heads(a):
    bsz, n, _ = a.shape
    return a.reshape(bsz, n, C_HEADS, 2, C_DH).transpose(0, 2, 3, 1, 4)


def diff_softmax_attend(q, k, v, lam):
    s = jnp.einsum('bhcqd,bhckd->bhcqk', q.astype(jnp.float32), k.astype(jnp.float32)) * (C_DH ** -0.5)
    p = jax.nn.softmax(s, axis=-1)
    a = p[:, :, 0] - lam * p[:, :, 1]
    return jnp.einsum('bhqk,bhkv->bhqv', a, v.astype(jnp.float32)).astype(v.dtype)


def diff_readout(o, g, lam_init):
    bsz, h, n, dv = o.shape
    o = rmsnorm(o.transpose(0, 2, 1, 3), g.reshape(h, dv)) * (1.0 - lam_init)
    return o.reshape(bsz, n, h * dv)


def diff_attention_branch(p_l, p_c, cos, sin, lam_p, subln, layer, with_ctx_out):
    lam_init = 0.8 - 0.6 * math.exp(-0.3 * layer)
    lam = (jnp.exp(jnp.sum(lam_p[0] * lam_p[1])) - jnp.exp(jnp.sum(lam_p[2] * lam_p[3])) + lam_init).astype(jnp.float32)
    q_l, k_l, v_l = apply_rope(qk_heads(p_l[0]), cos, sin), apply_rope(qk_heads(p_l[1]), cos, sin), to_heads(p_l[2], C_HEADS)
    q_c, k_c, v_c = qk_heads(p_c[0]), qk_heads(p_c[1]), to_heads(p_c[2], C_HEADS)
    k_all = jnp.concatenate([k_l, k_c], axis=3)
    v_all = jnp.concatenate([v_l, v_c], axis=2)
    bsz, h, _, n, dh = q_l.shape
    nb = n // Q_BLOCK
    qb = jnp.moveaxis(q_l.reshape(bsz, h, 2, nb, Q_BLOCK, dh), 3, 0)
    o = lax.map(lambda qblk: diff_softmax_attend(qblk, k_all, v_all, lam), qb)
    o = jnp.moveaxis(o, 0, 2).reshape(bsz, h, n, C_DV)
    y_l = diff_readout(o, subln, lam_init)
    y_c = diff_readout(diff_softmax_attend(q_c, k_c, v_c, lam), subln, lam_init) if with_ctx_out else None
    return y_l, y_c


def merge_branches(ys, gates, w_branch, w_out):
    merged = sum(jax.nn.sigmoid(gates[..., i * D_MODEL:(i + 1) * D_MODEL]) * (ys[i] @ w_branch[i]) for i in range(N_BRANCH))
    return merged @ w_out


def token_mixer(h_l, h_c, layer, cos, sin, w_in_l, hgrn_lb, gnorm_a, vnorm_b, ws_b, bs_b,
                lam_p, subln_c, w_branch_l, w_out_l, with_ctx_out):
    pl = split_cols(h_l @ w_in_l)
    pc = split_cols(h_c @ w_in_l)
    lb_f = lower_bound(hgrn_lb[0], layer)
    lb_b = lower_bound(hgrn_lb[1], layer)
    ya_l, ya_c = hgrn2_branch(pl[0:5], pc[0:5], lb_f, lb_b, gnorm_a, with_ctx_out)
    yb_l = chunk_mlp(pl[5], vnorm_b, ws_b, bs_b)
    yc_l, yc_c = diff_attention_branch(pl[6:9], pc[6:9], cos, sin, lam_p, subln_c, layer, with_ctx_out)
    m_l = merge_branches([ya_l, yb_l, yc_l], pl[9], w_branch_l, w_out_l)
    m_c = None
    if with_ctx_out:
        yb_c = chunk_mlp(pc[5], vnorm_b, ws_b, bs_b)
        m_c = merge_branches([ya_c, yb_c, yc_c], pc[9], w_branch_l, w_out_l)
    return m_l, m_c


def swiglu(x, wg, wu, wd):
    return (jax.nn.silu(x @ wg) * (x @ wu)) @ wd


def moe_ffn(x, w_router, w_eg, w_eu, w_ed):
    logits = (x @ w_router).astype(jnp.float32)
    top_v, top_i = lax.top_k(logits, TOP_K)
    wts = jax.nn.softmax(top_v, axis=-1)
    comb = jnp.sum(jax.nn.one_hot(top_i, N_EXPERTS, dtype=jnp.float32) * wts[..., None], axis=-2)
    out = jnp.zeros_like(x)
    for e in range(N_EXPERTS):
        out = out + comb[..., e:e + 1].astype(x.dtype) * swiglu(x, w_eg[e], w_eu[e], w_ed[e])
    return out


def channel_mixer(h, layer, ffn_wg, ffn_wu, ffn_wd, moe_router, moe_wg, moe_wu, moe_wd):
    j = layer // 2
    if layer % 2 == 0:
        return swiglu(h, ffn_wg[j], ffn_wu[j], ffn_wd[j])
    return moe_ffn(h, moe_router[j], moe_wg[j], moe_wu[j], moe_wd[j])


def setup_inputs(seed: int = 0) -> dict:
    key = jax.random.key(seed)
    ks = jax.random.split(key, 32)
    nrm = lambda k, shape, scale: jax.random.normal(k, shape, jnp.float32) * scale
    n_dense = (DEPTH + 1) // 2
    n_moe = DEPTH // 2
    return {
        "x": nrm(ks[0], (BATCH, SEQ, D_MODEL), 1.0),
        "c": nrm(ks[1], (BATCH, D_MODEL), 1.0),
        "ctx": nrm(ks[2], (BATCH, CTX_LEN, D_MODEL), 1.0),
        "c_ctx": nrm(ks[3], (D_MODEL,), 1.0),
        "w_ada": nrm(ks[4], (DEPTH, D_MODEL, 6 * D_MODEL), 0.5 * D_MODEL ** -0.5),
        "b_ada": nrm(ks[5], (DEPTH, 6 * D_MODEL), 0.02),
        "g_norm1": 1.0 + nrm(ks[6], (DEPTH, D_MODEL), 0.02),
        "g_norm2": 1.0 + nrm(ks[7], (DEPTH, D_MODEL), 0.02),
        "w_in": nrm(ks[8], (DEPTH, D_MODEL, IN_COLS), D_MODEL ** -0.5),
        "hgrn_lb": nrm(ks[9], (2, DEPTH, A_WIDTH), 0.5),
        "hgrn_gnorm": 1.0 + nrm(ks[10], (DEPTH, A_VWIDTH), 0.02),
        "mlp_vnorm": 1.0 + nrm(ks[11], (DEPTH, B_WIDTH), 0.02),
        "mlp_ws": nrm(ks[12], (DEPTH, B_GROUPS, B_CHUNK, B_CHUNK), B_CHUNK ** -0.5),
        "mlp_bs": 1.0 + nrm(ks[13], (DEPTH, B_GROUPS, B_CHUNK), 0.02),
        "diff_lambda": nrm(ks[14], (DEPTH, 4, C_DH), 0.1),
        "diff_subln": 1.0 + nrm(ks[15], (DEPTH, C_WIDTH), 0.02),
        "w_branch": nrm(ks[16], (DEPTH, N_BRANCH, BRANCH_WIDTH, D_MODEL), BRANCH_WIDTH ** -0.5),
        "w_out": nrm(ks[17], (DEPTH, D_MODEL, D_MODEL), D_MODEL ** -0.5),
        "ffn_wg": nrm(ks[18], (n_dense, D_MODEL, D_FF), D_MODEL ** -0.5),
        "ffn_wu": nrm(ks[19], (n_dense, D_MODEL, D_FF), D_MODEL ** -0.5),
        "ffn_wd": nrm(ks[20], (n_dense, D_FF, D_MODEL), D_FF ** -0.5),
        "moe_router": nrm(ks[21], (n_moe, D_MODEL, N_EXPERTS), D_MODEL ** -0.5),
        "moe_wg": nrm(ks[22], (n_moe, N_EXPERTS, D_MODEL, D_FF_E), D_MODEL ** -0.5),
        "moe_wu": nrm(ks[23], (n_moe, N_EXPERTS, D_MODEL, D_FF_E), D_MODEL ** -0.5),
        "moe_wd": nrm(ks[24], (n_moe, N_EXPERTS, D_FF_E, D_MODEL), D_FF_E ** -0.5),
        "g_final": 1.0 + nrm(ks[25], (D_MODEL,), 0.02),
    }


def reference(x, c, ctx, c_ctx, w_ada, b_ada, g_norm1, g_norm2, w_in, hgrn_lb, hgrn_gnorm,
              mlp_vnorm, mlp_ws, mlp_bs, diff_lambda, diff_subln, w_branch, w_out,
              ffn_wg, ffn_wu, ffn_wd, moe_router, moe_wg, moe_wu, moe_wd, g_final):
    n_lat = x.shape[1]
    rows = n_lat // GRID_W
    cos, sin = axial_rope_tables(rows)
    for layer in range(DEPTH):
        last = layer == DEPTH - 1
        mod_l = jnp.split(adaln(c, w_ada[layer], b_ada[layer])[:, None, :], 6, axis=-1)
        mod_c = jnp.split(adaln(c_ctx, w_ada[layer], b_ada[layer]), 6, axis=-1)
        h_l = modulate(rmsnorm(x, g_norm1[layer]), mod_l[0], mod_l[1])
        h_c = modulate(rmsnorm(ctx, g_norm1[layer]), mod_c[0], mod_c[1])
        m_l, m_c = token_mixer(h_l, h_c, layer, cos, sin, w_in[layer], hgrn_lb, hgrn_gnorm[layer],
                               mlp_vnorm[layer], mlp_ws[layer], mlp_bs[layer], diff_lambda[layer],
                               diff_subln[layer], w_branch[layer], w_out[layer], not last)
        x = x + mod_l[2] * m_l
        h_l = modulate(rmsnorm(x, g_norm2[layer]), mod_l[3], mod_l[4])
        x = x + mod_l[5] * channel_mixer(h_l, layer, ffn_wg, ffn_wu, ffn_wd, moe_router, moe_wg, moe_wu, moe_wd)
        if not last:
            ctx = ctx + mod_c[2] * m_c
            h_c = modulate(rmsnorm(ctx, g_norm2[layer]), mod_c[3], mod_c[4])
            ctx = ctx + mod_c[5] * channel_mixer(h_c, layer, ffn_wg, ffn_wu, ffn_wd, moe_router, moe_wg, moe_wu, moe_wd)
    return rmsnorm(x, g_final)
```

```python
import math
import numpy as np
import concourse.bass as bass
import concourse.mybir as mybir
from contextlib import ExitStack
from concourse.bass_utils import run_bass_kernel_spmd

F32 = mybir.dt.float32
BF16 = mybir.dt.bfloat16
AF = mybir.ActivationFunctionType
ALU = mybir.AluOpType
AX = mybir.AxisListType

D = 1024
NLAT = 4096
NCTX = 256
NTOK = NLAT + NCTX
NT = NTOK // 128
EPS = 1e-6
DFF = 2816
DFFE = 3584
NEXP = 8
BLOCKS = [(0, 256)] + [(256 + 512 * i, 512) for i in range(8)]
NCH = NTOK // 64


class Buf:
    __slots__ = ("name", "last_w", "reads")

    def __init__(self, name=""):
        self.name = name
        self.last_w = None
        self.reads = []


class Sched:
    ENG = ("pe", "act", "dve", "pool", "sp")
    NDS = 6

    def __init__(self, nc, stack):
        self.nc = nc
        self.ops = {e: [] for e in self.ENG}
        self.sem = {e: stack.enter_context(nc.semaphore("s_" + e)) for e in self.ENG}
        self.cnt = {e: 0 for e in self.ENG}
        self.seen = {e: {} for e in self.ENG}
        self.dsem = {}
        self.dcnt = {}
        self.drr = {}
        for q in ("sp", "act", "pool"):
            self.dsem[q] = [stack.enter_context(nc.semaphore("d_%s%d" % (q, i))) for i in range(self.NDS)]
            self.dcnt[q] = [0] * self.NDS
            self.drr[q] = 0
        self.out_events = []

    def _deps(self, eng, reads, writes, extra=()):
        need = {}

        def add(ev):
            if ev is None:
                return
            key, sem, val = ev
            if self.seen[eng].get(key, 0) >= val:
                return
            if key not in need or need[key][1] < val:
                need[key] = (sem, val)

        for r in reads:
            add(r.last_w)
        for w in writes:
            add(w.last_w)
            for ev in w.reads:
                add(ev)
        for ev in extra:
            add(ev)
        waits = []
        for key, (sem, val) in need.items():
            self.seen[eng][key] = val
            waits.append((sem, val))
        return waits

    def _commit(self, ev, reads, writes):
        for r in reads:
            r.reads.append(ev)
            if len(r.reads) > 48:
                best = {}
                for e in r.reads:
                    if e[0] not in best or best[e[0]][2] < e[2]:
                        best[e[0]] = e
                r.reads = list(best.values())
        for w in writes:
            w.last_w = ev
            w.reads = []

    def op(self, eng, fns, reads=(), writes=()):
        if callable(fns):
            fns = [fns]
        waits = self._deps(eng, reads, writes)
        self.cnt[eng] += 1
        ev = ("c_" + eng, self.sem[eng], self.cnt[eng])
        self.ops[eng].append((waits, fns, (self.sem[eng], 1)))
        self._commit(ev, reads, writes)
        return ev

    def dma(self, q, out, in_, reads=(), writes=(), is_output=False, **kw):
        i = self.drr[q]
        self.drr[q] = (i + 1) % self.NDS
        sem = self.dsem[q][i]
        key = "d_%s%d" % (q, i)
        prev = self.dcnt[q][i]
        extra = [(key, sem, prev)] if prev > 0 else []
        waits = self._deps(q, reads, writes, extra)
        self.dcnt[q][i] = prev + 16
        ev = (key, sem, prev + 16)
        self.ops[q].append((waits, [lambda e, o=out, a=in_, k=kw: e.dma_start(out=o, in_=a, **k)], (sem, 16)))
        self._commit(ev, reads, writes)
        if is_output:
            self.out_events.append(ev)
        return ev

    def barrier(self):
        allw = {}
        for q in ("sp", "act", "pool"):
            for i in range(self.NDS):
                if self.dcnt[q][i] > 0:
                    allw["d_%s%d" % (q, i)] = (self.dsem[q][i], self.dcnt[q][i])
        for e in self.ENG:
            if self.cnt[e] > 0:
                allw["c_" + e] = (self.sem[e], self.cnt[e])
        for e in self.ENG:
            waits = []
            for key, (sem, val) in allw.items():
                if self.seen[e].get(key, 0) < val:
                    self.seen[e][key] = val
                    waits.append((sem, val))
            self.ops[e].append((waits, [], None))

    def emit(self):
        nc = self.nc
        self.barrier()
        hmap = {"pe": "tensor", "act": "scalar", "dve": "vector", "pool": "gpsimd", "sp": "sync"}
        with nc.Block() as block:
            for e in self.ENG:
                ops = self.ops[e]

                def body(eng, ops=ops):
                    for waits, fns, inc in ops:
                        for sem, val in waits:
                            eng.wait_ge(sem, val)
                        n = len(fns)
                        for j, fn in enumerate(fns):
                            ins = fn(eng)
                            if j == n - 1 and inc is not None:
                                ins.then_inc(inc[0], inc[1])

                getattr(block, hmap[e])(body)
        self.ops = {e: [] for e in self.ENG}


_UID = [0]


def U(name):
    _UID[0] += 1
    return "%s_u%d" % (name, _UID[0])


def AP(t, off, dims):
    return bass.AP(t, off, [list(d) for d in dims])


class Rot:
    def __init__(self, items):
        self.items = items
        self.i = 0

    def next(self):
        it = self.items[self.i]
        self.i = (self.i + 1) % len(self.items)
        return it


class K:
    pass


def build(n_layers=2, debug=None, stop_after=None, small_moe=False):
    nc = bass.Bass("TRN2", target_bir_lowering=False)
    k = K()
    k.nc = nc
    k.debug = debug or []
    dt_in = lambda name, shape: nc.dram_tensor(name, shape, F32, kind="ExternalInput")

    def scratch(name, shape, dt):
        kind = "ExternalOutput" if name in k.debug else "Internal"
        return nc.dram_tensor(name, shape, dt, kind=kind)

    I = {}
    I["x"] = dt_in("x", [NLAT, D])
    I["ctx"] = dt_in("ctx", [NCTX, D])
    I["cT"] = dt_in("cT", [128, 8, 2])
    I["w_ada"] = dt_in("w_ada", [2, D, 6 * D])
    I["b_ada"] = dt_in("b_ada", [2, 6 * D])
    I["g_norm1"] = dt_in("g_norm1", [2, D])
    I["g_norm2"] = dt_in("g_norm2", [2, D])
    I["w_in"] = dt_in("w_in", [2, D, 8192])
    I["w_in_sw"] = dt_in("w_in_sw", [2, D, 1024])
    I["lbT"] = dt_in("lbT", [128, 2, 2, 4])
    I["gnA"] = dt_in("gnA", [128, 2, 4])
    I["vnorm"] = dt_in("vnorm", [2, 512])
    I["wsT"] = dt_in("wsT", [2, 128, 4, 128])
    I["bs"] = dt_in("bs", [2, 512])
    I["dlam"] = dt_in("dlam", [2, 256])
    I["sublnT"] = dt_in("sublnT", [128, 2, 4])
    I["w_branch"] = dt_in("w_branch", [2, 3, 512, D])
    I["w_out"] = dt_in("w_out", [2, D, D])
    I["ffn_wg"] = dt_in("ffn_wg", [1, D, DFF])
    I["ffn_wu"] = dt_in("ffn_wu", [1, D, DFF])
    I["ffn_wd"] = dt_in("ffn_wd", [1, DFF, D])
    I["moe_router"] = dt_in("moe_router", [1, D, NEXP])
    if small_moe:
        I["moe_wg"] = dt_in("moe_wg", [1, 1, 8, 8])
        I["moe_wu"] = dt_in("moe_wu", [1, 1, 8, 8])
        I["moe_wd"] = dt_in("moe_wd", [1, 1, 8, 8])
    else:
        I["moe_wg"] = dt_in("moe_wg", [1, NEXP, D, DFFE])
        I["moe_wu"] = dt_in("moe_wu", [1, NEXP, D, DFFE])
        I["moe_wd"] = dt_in("moe_wd", [1, NEXP, DFFE, D])
    I["g_final"] = dt_in("g_final", [D])
    I["ropeC"] = dt_in("ropeC", [128, NTOK])
    I["ropeS"] = dt_in("ropeS", [128, NTOK])
    k.I = I
    k.out = nc.dram_tensor("out", [NLAT, D], F32, kind="ExternalOutput")
    k.xs = scratch("xs", [NTOK, D], F32)
    k.modrow = scratch("modrow", [2, 6, D], F32)
    k.pAq = scratch("pAq", [512, NTOK], F32)
    k.pAzf = scratch("pAzf", [512, NTOK], F32)
    k.pAzb = scratch("pAzb", [512, NTOK], F32)
    k.pAv = scratch("pAv", [NTOK, 512], BF16)
    k.pAog = scratch("pAog", [512, NTOK], BF16)
    k.pBu = scratch("pBu", [512, NTOK], BF16)
    k.pBv = scratch("pBv", [NTOK, 512], BF16)
    k.pCq = scratch("pCq", [512, NTOK], BF16)
    k.pCk = scratch("pCk", [512, NTOK], BF16)
    k.pCv = scratch("pCv", [NTOK, 512], BF16)
    k.pG = scratch("pG", [3072, NTOK], BF16)
    k.ybr = scratch("ybr", [3, 512, NTOK], BF16)
    k.h2T = scratch("h2T", [D, NTOK], BF16)
    k.B = {n: Buf(n) for n in ["xs", "modrow", "pAq", "pAzf", "pAzb", "pAv", "pAog", "pBu", "pBv", "pCq", "pCk",
                               "pCv", "pG", "ybr0", "ybr1", "ybr2", "h2T", "out"]}

    k.Bxs = [Buf("xs%d" % i) for i in range(NT)]
    with ExitStack() as top:
        S = Sched(nc, top)
        k.S = S
        k.ident = top.enter_context(nc.sbuf_tensor("ident", [128, 128], BF16))
        k.ones = top.enter_context(nc.sbuf_tensor("ones", [128, 128], BF16))
        k.sel0 = top.enter_context(nc.sbuf_tensor("sel0", [128, 128], BF16))
        k.sel1 = top.enter_context(nc.sbuf_tensor("sel1", [128, 128], BF16))
        k.onecol = top.enter_context(nc.sbuf_tensor("onecol", [128, 1], F32))
        k.Bc = Buf("consts")
        phase_consts(k)
        for layer in range(n_layers):
            last = layer == 1
            phases = [("ada", phase_ada), ("n1p", phase_n1p), ("hgrn", phase_hgrn), ("mlp", phase_mlp),
                      ("attn", phase_attn), ("merge", phase_merge), ("ffn", phase_ffn)]
            done = False
            for name, fn in phases:
                fn(k, layer, last)
                if stop_after == (layer, name):
                    done = True
                    break
            if done:
                break
        S.emit()
    return nc


def phase_consts(k):
    nc, S = k.nc, k.S
    with ExitStack() as st:
        tf = st.enter_context(nc.sbuf_tensor(U("c_tf"), [128, 128], F32))
        b = Buf()
        S.op("pool", lambda e: e.memset(tf[:], 0.0), writes=[b])
        S.op("pool", lambda e: e.affine_select(out=tf[:], in_=tf[:], pattern=[[1, 128]], compare_op=ALU.not_equal,
                                               fill=1.0, base=0, channel_multiplier=-1), reads=[b], writes=[b])
        S.op("dve", lambda e: e.tensor_copy(out=k.ident[:], in_=tf[:]), reads=[b], writes=[k.Bc])
        S.op("dve", lambda e: e.memset(k.ones[:], 1.0), writes=[k.Bc])
        S.op("dve", lambda e: e.memset(k.sel0[:], 0.0), writes=[k.Bc])
        S.op("dve", lambda e: e.memset(k.sel0[0:64, :], 1.0), writes=[k.Bc])
        S.op("dve", lambda e: e.memset(k.sel1[:], 0.0), writes=[k.Bc])
        S.op("dve", lambda e: e.memset(k.sel1[64:128, :], 1.0), writes=[k.Bc])
        S.op("dve", lambda e: e.memset(k.onecol[:], 1.0), writes=[k.Bc])
        S.dma("sp", k.xs.ap()[0:NCTX, :], k.I["ctx"].ap(), writes=[k.B["xs"]])
        for i in range(4):
            S.dma("sp", k.xs.ap()[NCTX + i * 1024:NCTX + (i + 1) * 1024, :], k.I["x"].ap()[i * 1024:(i + 1) * 1024, :],
                  writes=[k.B["xs"]])
        S.emit()


def phase_ada(k, layer, last):
    nc, S, I = k.nc, k.S, k.I
    with ExitStack() as st:
        sb = lambda n, s, d: st.enter_context(nc.sbuf_tensor(U("ad_" + n), s, d))
        cT = sb("cT", [128, 8, 2], F32)
        cs = sb("cs", [128, 8, 2], BF16)
        wts = [(sb("w%d" % i, [128, 8, 1024], BF16), Buf()) for i in range(2)]
        brow = sb("brow", [1, 6 * D], F32)
        g1 = sb("g1", [1, D], F32)
        g2 = sb("g2", [1, D], F32)
        res = [(sb("res%d" % i, [1, D], F32), Buf()) for i in range(2)]
        pss = [(st.enter_context(nc.psum_tensor(U("ad_ps%d" % i), [128, 512], F32)), Buf()) for i in range(2)]
        bc, bcs, bb = Buf(), Buf(), Buf()
        S.dma("sp", cT[:], I["cT"].ap(), writes=[bc])
        S.dma("sp", brow[:], I["b_ada"].ap()[layer:layer + 1, :], writes=[bb])
        S.dma("sp", g1[:], I["g_norm1"].ap()[layer:layer + 1, :], writes=[bb])
        S.dma("sp", g2[:], I["g_norm2"].ap()[layer:layer + 1, :], writes=[bb])
        S.op("act", lambda e: e.activation(out=cs[:], in_=cT[:], func=AF.Silu), reads=[bc], writes=[bcs])
        wsrc = I["w_ada"].ap()[layer].rearrange("(k p) n -> p k n", p=128)
        ri = 0
        for j in range(6):
            w, wb = wts[j % 2]
            S.dma("pool", w[:], wsrc[:, :, j * 1024:(j + 1) * 1024], writes=[wb])
            for which in range(2):
                r, rb = res[ri % 2]
                ri += 1
                for half in range(2):
                    ps, pb = pss[half]
                    S.op("pe", [lambda e, kk=kk, ps=ps, w=w, half=half, which=which: e.matmul(
                        ps[0:1, :], lhsT=cs[:, kk, which:which + 1], rhs=w[:, kk, half * 512:(half + 1) * 512],
                        start=(kk == 0), stop=(kk == 7)) for kk in range(8)], reads=[bcs, wb], writes=[pb])
                    S.op("dve", lambda e, ps=ps, r=r, half=half, j=j: e.tensor_tensor(
                        out=r[0:1, half * 512:(half + 1) * 512], in0=ps[0:1, :],
                        in1=brow[0:1, j * 1024 + half * 512:j * 1024 + (half + 1) * 512], op=ALU.add),
                        reads=[pb, bb], writes=[rb])
                if j in (1, 4):
                    g = g1 if j == 1 else g2
                    S.op("dve", lambda e, r=r, g=g: e.scalar_tensor_tensor(out=r[:], in0=r[:], scalar=1.0, in1=g[:],
                                                                            op0=ALU.add, op1=ALU.mult),
                         reads=[rb, bb], writes=[rb])
                S.dma("sp", k.modrow.ap()[which, j:j + 1, :], r[:], reads=[rb], writes=[k.B["modrow"]])
        S.emit()


def load_rep(k, S, tile, buf, which, j):
    S.dma("sp", tile[:], AP(k.modrow, (which * 6 + j) * D, [[0, 128], [1, D]]), reads=[k.B["modrow"]], writes=[buf])


def rms_mod(S, xt, xb, A, Ab, Bt, Bb, hb, hbb, ss, ssb, junk, junkb, D_=D):
    S.op("act", lambda e: e.activation(out=junk[:], in_=xt[:], func=AF.Square, accum_out=ss[:, 0:1]), reads=[xb],
         writes=[junkb, ssb])
    S.op("act", lambda e: e.activation(out=ss[:, 1:2], in_=ss[:, 0:1], func=AF.Ln, scale=1.0 / D_, bias=EPS),
         reads=[ssb], writes=[ssb])
    S.op("act", lambda e: e.activation(out=ss[:, 2:3], in_=ss[:, 1:2], func=AF.Exp, scale=-0.5), reads=[ssb],
         writes=[ssb])
    S.op("dve", lambda e: e.scalar_tensor_tensor(out=junk[:], in0=xt[:], scalar=ss[:, 2:3], in1=A[:], op0=ALU.mult,
                                                 op1=ALU.mult), reads=[xb, ssb, Ab], writes=[junkb])
    if Bt is None:
        return
    S.op("dve", lambda e: e.tensor_tensor(out=hb[:], in0=junk[:], in1=Bt[:], op=ALU.add), reads=[junkb, Bb],
         writes=[hbb])


def phase_n1p(k, layer, last):
    nc, S, I = k.nc, k.S, k.I
    with ExitStack() as st0:
      hT = st0.enter_context(nc.sbuf_tensor(U("np_hT"), [128, 8, NTOK], BF16))
      hTb = Buf()
      with ExitStack() as st:
        sb = lambda n, s, d: st.enter_context(nc.sbuf_tensor(U("np_" + n), s, d))
        A = [sb("A%d" % w, [128, D], F32) for w in range(2)]
        Bm = [sb("B%d" % w, [128, D], F32) for w in range(2)]
        mb = Buf()
        for w in range(2):
            load_rep(k, S, A[w], mb, w, 1)
            load_rep(k, S, Bm[w], mb, w, 0)
        xts = Rot([(sb("xt%d" % i, [128, D], F32), Buf()) for i in range(2)])
        junks = Rot([(sb("junk%d" % i, [128, D], F32), Buf()) for i in range(2)])
        hbs = Rot([(sb("hb%d" % i, [128, D], BF16), Buf()) for i in range(2)])
        sss = Rot([(sb("ss%d" % i, [128, 4], F32), Buf()) for i in range(2)])
        pts = Rot([(st.enter_context(nc.psum_tensor(U("np_pt%d" % i), [128, 512], BF16)), Buf()) for i in range(2)])
        for i in range(NT):
            w = 1 if i < 2 else 0
            xt, xb = xts.next()
            junk, jb = junks.next()
            hb, hbb = hbs.next()
            ss, ssb = sss.next()
            S.dma("sp", xt[:], k.xs.ap()[i * 128:(i + 1) * 128, :], reads=[k.B["xs"]], writes=[xb])
            rms_mod(S, xt, xb, A[w], mb, Bm[w], mb, hb, hbb, ss, ssb, junk, jb)
            for half in range(2):
                pt, ptb = pts.next()
                S.op("pe", [lambda e, pt=pt, hb=hb, j=j, half=half: e.transpose(
                    out=pt[:, j * 128:(j + 1) * 128], in_=hb[:, (half * 4 + j) * 128:(half * 4 + j + 1) * 128],
                    identity=k.ident[:]) for j in range(4)], reads=[hbb, k.Bc], writes=[ptb])
                eng = "act" if half == 0 else "dve"
                outap = hT[:, half * 4:half * 4 + 4, i * 128:(i + 1) * 128]
                inap = AP(pt, 0, [[512, 128], [128, 4], [1, 128]])
                if eng == "act":
                    S.op("act", lambda e, o=outap, a=inap: e.activation(out=o, in_=a, func=AF.Copy), reads=[ptb],
                         writes=[hTb])
                else:
                    S.op("dve", lambda e, o=outap, a=inap: e.tensor_copy(out=o, in_=a), reads=[ptb], writes=[hTb])

        S.emit()
      with ExitStack() as st:
        sb = lambda n, s, d: st.enter_context(nc.sbuf_tensor(U("nq_" + n), s, d))
        junks = Rot([(sb("junk%d" % i, [128, D], F32), Buf()) for i in range(2)])
        sss = Rot([(sb("ss%d" % i, [128, 4], F32), Buf()) for i in range(2)])
        ropeC = sb("ropeC", [128, NTOK], F32)
        ropeS = sb("ropeS", [128, NTOK], F32)
        rb = Buf()
        S.dma("sp", ropeC[:], I["ropeC"].ap(), writes=[rb])
        S.dma("sp", ropeS[:], I["ropeS"].ap(), writes=[rb])
        vn = sb("vn", [128, 512], F32)
        S.dma("sp", vn[:], AP(I["vnorm"], layer * 512, [[0, 128], [1, 512]]), writes=[rb])
        wts = Rot([(sb("w%d" % i, [128, 8, 1024], BF16), Buf()) for i in range(2)])
        wsw = sb("wsw", [128, 8, 512], BF16)
        wswb = Buf()
        stg32 = Rot([(sb("s32_%d" % i, [128, NTOK], F32), Buf()) for i in range(2)])
        pps = Rot([(st.enter_context(nc.psum_tensor(U("nq_pp%d" % i), [128, 512], F32)), Buf()) for i in range(4)])
        wsrc = I["w_in"].ap()[layer].rearrange("(k p) n -> p k n", p=128)
        wswsrc = I["w_in_sw"].ap()[layer].rearrange("(k p) n -> p k n", p=128)

        def fm_mm(ps, pb, w, wb, c0, t0, W):
            S.op("pe", [lambda e, kk=kk: e.matmul(ps[:, 0:W], lhsT=w[:, kk, c0:c0 + 128], rhs=hT[:, kk, t0:t0 + W],
                                                   start=(kk == 0), stop=(kk == 7)) for kk in range(8)],
                 reads=[wb, hTb], writes=[pb])

        def fm_job(c_in, ncols, dst, dstbuf, func, out_dt):
            for g0 in range(0, ncols, 1024):
                gw = min(1024, ncols - g0)
                w, wb = wts.next()
                S.dma("pool", w[:, :, 0:gw], wsrc[:, :, c_in + g0:c_in + g0 + gw], writes=[wb])
                for cc in range(gw // 128):
                    stg, sgb = stg32.next()
                    if out_dt == BF16:
                        stgv = stg[:].bitcast(BF16)[:, 0:NTOK]
                    else:
                        stgv = stg[:]
                    for (t0, W) in BLOCKS:
                        ps, pb = pps.next()
                        fm_mm(ps, pb, w, wb, cc * 128, t0, W)
                        if func is None:
                            S.op("dve", lambda e, ps=ps, o=stgv[:, t0:t0 + W], W=W: e.tensor_copy(out=o, in_=ps[:, 0:W]),
                                 reads=[pb], writes=[sgb])
                        else:
                            S.op("act", lambda e, ps=ps, o=stgv[:, t0:t0 + W], W=W: e.activation(out=o, in_=ps[:, 0:W],
                                                                                             func=func),
                                 reads=[pb], writes=[sgb])
                    r0 = g0 + cc * 128
                    S.dma("sp", dst[r0:r0 + 128, :], stgv, reads=[sgb], writes=[dstbuf])

        def tm_job(c_in, dst, dstbuf, mode):
            w, wb = wts.next()
            S.dma("pool", w[:, :, 0:512], wsrc[:, :, c_in:c_in + 512], writes=[wb])
            stg, sgb = stg32.next()
            stgv = AP(stg, 0, [[NTOK, 128], [1, NTOK]]).bitcast(BF16)
            for i in range(NT):
                ps, pb = pps.next()
                S.op("pe", [lambda e, kk=kk, ps=ps, i=i: e.matmul(ps[:], lhsT=hT[:, kk, i * 128:(i + 1) * 128],
                                                                  rhs=w[:, kk, 0:512], start=(kk == 0), stop=(kk == 7))
                            for kk in range(8)], reads=[wb, hTb], writes=[pb])
                o = stgv[:, (i % 16) * 512:(i % 16 + 1) * 512]
                if mode == "copy":
                    S.op("act", lambda e, ps=ps, o=o: e.activation(out=o, in_=ps[:], func=AF.Copy), reads=[pb],
                         writes=[sgb])
                else:
                    gl, glb = junks.next()
                    ss, ssb = sss.next()
                    S.op("act", lambda e, ps=ps, gl=gl: e.activation(out=gl[:, 0:512], in_=ps[:], func=AF.Gelu_apprx_tanh),
                         reads=[pb], writes=[glb])
                    S.op("act", lambda e, gl=gl, ss=ss: e.activation(out=gl[:, 512:1024], in_=gl[:, 0:512], func=AF.Square,
                                                                     accum_out=ss[:, 0:1]), reads=[glb], writes=[glb, ssb])
                    S.op("act", lambda e, ss=ss: e.activation(out=ss[:, 1:2], in_=ss[:, 0:1], func=AF.Ln, scale=1.0 / 512,
                                                              bias=EPS), reads=[ssb], writes=[ssb])
                    S.op("act", lambda e, ss=ss: e.activation(out=ss[:, 2:3], in_=ss[:, 1:2], func=AF.Exp, scale=-0.5),
                         reads=[ssb], writes=[ssb])
                    S.op("dve", lambda e, gl=gl, ss=ss, o=o: e.scalar_tensor_tensor(
                        out=o, in0=gl[:, 0:512], scalar=ss[:, 2:3], in1=vn[:], op0=ALU.mult, op1=ALU.mult),
                        reads=[glb, ssb, rb], writes=[sgb])
                if i % 16 == 15 or i == NT - 1:
                    i0 = (i // 16) * 16
                    n = i - i0 + 1
                    S.dma("sp", dst.ap()[i0 * 128:(i + 1) * 128, :].rearrange("(n p) c -> p n c", p=128),
                          AP(stg, 0, [[NTOK, 128], [1, NTOK]]).bitcast(BF16)[:, 0:n * 512].rearrange("p (n c) -> p n c", c=512),
                          reads=[sgb], writes=[dstbuf])
                    if i != NT - 1:
                        stg, sgb = stg32.next()
                        stgv = AP(stg, 0, [[NTOK, 128], [1, NTOK]]).bitcast(BF16)

        def rope_job(c_in, sw0, dst, dstbuf):
            w, wb = wts.next()
            S.dma("pool", w[:, :, 0:512], wsrc[:, :, c_in:c_in + 512], writes=[wb])
            S.dma("pool", wsw[:], wswsrc[:, :, sw0:sw0 + 512], writes=[wswb])
            t1s = Rot([(junks.items[0][0], junks.items[0][1]), (junks.items[1][0], junks.items[1][1])])
            for cc in range(4):
                stg, sgb = stg32.next()
                stgv = stg[:].bitcast(BF16)[:, 0:NTOK]
                for (t0, W) in BLOCKS:
                    ps, pb = pps.next()
                    ps2, pb2 = pps.next()
                    fm_mm(ps, pb, w, wb, cc * 128, t0, W)
                    fm_mm(ps2, pb2, wsw, wswb, cc * 128, t0, W)
                    t1, t1b = t1s.next()
                    S.op("dve", lambda e, ps=ps, t1=t1, t0=t0, W=W: e.tensor_tensor(
                        out=t1[:, 0:W], in0=ps[:, 0:W], in1=ropeC[:, t0:t0 + W], op=ALU.mult), reads=[pb, rb], writes=[t1b])
                    S.op("dve", lambda e, ps2=ps2, t1=t1, t0=t0, W=W: e.tensor_tensor(
                        out=t1[:, 512:512 + W], in0=ps2[:, 0:W], in1=ropeS[:, t0:t0 + W], op=ALU.mult), reads=[pb2, rb],
                        writes=[t1b])
                    S.op("dve", lambda e, t1=t1, o=stgv[:, t0:t0 + W], W=W: e.tensor_tensor(
                        out=o, in0=t1[:, 0:W], in1=t1[:, 512:512 + W], op=ALU.add), reads=[t1b], writes=[sgb])
                S.dma("sp", dst[cc * 128:(cc + 1) * 128, :], stgv, reads=[sgb], writes=[dstbuf])

        B = k.B
        fm_job(0, 512, k.pAq.ap(), B["pAq"], None, F32)
        fm_job(512, 512, k.pAzf.ap(), B["pAzf"], None, F32)
        fm_job(1024, 512, k.pAzb.ap(), B["pAzb"], None, F32)
        tm_job(1536, k.pAv, B["pAv"], "copy")
        fm_job(2048, 512, k.pAog.ap(), B["pAog"], AF.Silu, BF16)
        fm_job(2560, 512, k.pBu.ap(), B["pBu"], AF.Gelu_apprx_tanh, BF16)
        tm_job(3072, k.pBv, B["pBv"], "gelu_rms")
        rope_job(3584, 0, k.pCq.ap(), B["pCq"])
        rope_job(4096, 512, k.pCk.ap(), B["pCk"])
        tm_job(4608, k.pCv, B["pCv"], "copy")
        fm_job(5120, 3072, k.pG.ap(), B["pG"], AF.Sigmoid, BF16)
        S.emit()


def head_readout(k, S, st, prefix, src, srcb, t0, W, scale_col, scb, mul_tile, mulb, dst, dstb, pss, sq_rot, tmp_rot):
    sq, sqb = sq_rot.next()
    tmp, tb = tmp_rot.next()
    ps, pb = pss.next()
    S.op("act", lambda e: e.activation(out=sq[:, 0:W], in_=src, func=AF.Square), reads=[srcb], writes=[sqb])
    S.op("pe", lambda e: e.matmul(ps[:, 0:W], lhsT=k.ones[:], rhs=sq[:, 0:W], start=True, stop=True), reads=[sqb, k.Bc],
         writes=[pb])
    S.op("act", lambda e: e.activation(out=tmp[:, 0:W], in_=ps[:, 0:W], func=AF.Ln, scale=1.0 / 128, bias=EPS),
         reads=[pb], writes=[tb])
    S.op("act", lambda e: e.activation(out=tmp[:, 0:W], in_=tmp[:, 0:W], func=AF.Exp, scale=-0.5), reads=[tb],
         writes=[tb])
    if mul_tile is None:
        S.op("dve", lambda e: e.scalar_tensor_tensor(out=dst, in0=src, scalar=scale_col, in1=tmp[:, 0:W],
                                                     op0=ALU.mult, op1=ALU.mult), reads=[srcb, scb, tb], writes=[dstb])
    else:
        S.op("dve", lambda e: e.scalar_tensor_tensor(out=tmp[:, 0:W], in0=src, scalar=scale_col, in1=tmp[:, 0:W],
                                                     op0=ALU.mult, op1=ALU.mult), reads=[srcb, scb, tb], writes=[tb])
        S.op("dve", lambda e: e.tensor_tensor(out=dst, in0=tmp[:, 0:W], in1=mul_tile, op=ALU.mult), reads=[tb, mulb],
             writes=[dstb])


def phase_hgrn(k, layer, last):
    nc, S, I = k.nc, k.S, k.I
    with ExitStack() as st:
        sb = lambda n, s, d: st.enter_context(nc.sbuf_tensor(U("hg_" + n), s, d))
        T1 = sb("T1", [128, NTOK], F32)
        T2 = sb("T2", [128, NTOK], F32)
        Gp = sb("Gp", [128, NTOK + 1], F32)
        T4 = sb("T4", [128, NTOK], F32)
        q1 = sb("q1", [128, NTOK], BF16)
        k1 = sb("k1", [128, NTOK], BF16)
        k1z = sb("k1z", [128, NTOK], BF16)
        q2 = sb("q2", [128, NTOK], BF16)
        k2f = sb("k2f", [128, NTOK], BF16)
        k2T = sb("k2T", [64, NCH, 128], BF16)
        vS = sb("vS", [64, NCH, 128], BF16)
        oacc = sb("oacc", [128, NTOK], F32)
        dec = sb("dec", [128, NCH], F32)
        cqk = sb("cqk", [128, 2, NCH], F32)
        S32p = [(sb("S32_%d" % i, [128, 128], F32), Buf()) for i in range(2)]
        SbA = sb("SbA", [128, NCH * 128], BF16)
        slotb = [Buf() for _ in range(NCH + 1)]
        lbt = sb("lbt", [128, 16], F32)
        lbe = sb("lbe", [128, 16], F32)
        lbv = sb("lbv", [128, 8, 2], F32)
        gn = sb("gn", [128, 8], F32)
        maskF = sb("maskF", [64, 64], F32)
        maskB = sb("maskB", [64, 64], F32)
        attTs = Rot([(sb("attT%d" % i, [64, 512], BF16), Buf()) for i in range(3)])
        sqs = Rot([(sb("sq%d" % i, [128, 512], BF16), Buf()) for i in range(2)])
        tmps = Rot([(sb("tmp%d" % i, [128, 512], F32), Buf()) for i in range(2)])
        b = {n: Buf(n) for n in "T1 T2 Gp T4 q1 k1 k1z q2 k2f k2T vS oacc dec S32 Sb lb gn mask cqk".split()}
        pa = Rot([(st.enter_context(nc.psum_tensor(U("hg_pa%d" % i), [128, 512], F32)), Buf()) for i in range(2)])
        po = Rot([(st.enter_context(nc.psum_tensor(U("hg_po%d" % i), [128, 512], F32)), Buf()) for i in range(2)])
        psS = Rot([(st.enter_context(nc.psum_tensor(U("hg_ps%d" % i), [128, 512], F32)), Buf()) for i in range(2)])
        ptr = Rot([(st.enter_context(nc.psum_tensor(U("hg_pt%d" % i), [128, 512], BF16)), Buf()) for i in range(2)])

        S.dma("sp", lbt[:], I["lbT"].ap().rearrange("p a b c -> p (a b c)"), writes=[b["lb"]])
        S.dma("sp", gn[:], I["gnA"].ap().rearrange("p a b -> p (a b)"), writes=[b["gn"]])
        S.op("act", lambda e: e.activation(out=lbe[:], in_=lbt[:], func=AF.Exp), reads=[b["lb"]], writes=[b["lb"]])
        for d_ in range(2):
            e0 = lbe[:, d_ * 8:d_ * 8 + 4]
            e1 = lbe[:, d_ * 8 + 4:d_ * 8 + 8]
            tot = lbt[:, d_ * 8:d_ * 8 + 4]
            S.op("dve", lambda e, e0=e0, e1=e1, tot=tot: e.tensor_tensor(out=tot, in0=e0, in1=e1, op=ALU.add),
                 reads=[b["lb"]], writes=[b["lb"]])
            S.op("dve", lambda e, tot=tot: e.reciprocal(out=tot, in_=tot), reads=[b["lb"]], writes=[b["lb"]])
            num = lbt[:, d_ * 8 + 4:d_ * 8 + 8]
            if layer == 0:
                S.op("dve", lambda e, num=num, e0=e0: e.tensor_tensor(out=num, in0=e0, in1=e0, op=ALU.subtract),
                     reads=[b["lb"]], writes=[b["lb"]])
            else:
                S.op("dve", lambda e, num=num, e1=e1: e.tensor_copy(out=num, in_=e1), reads=[b["lb"]], writes=[b["lb"]])
            lbcol = AP(lbv, d_ * 8, [[16, 128], [2, 4]])
            omcol = AP(lbv, d_ * 8 + 1, [[16, 128], [2, 4]])
            S.op("dve", lambda e, lbcol=lbcol, num=num, tot=tot: e.tensor_tensor(out=lbcol, in0=num, in1=tot, op=ALU.mult),
                 reads=[b["lb"]], writes=[b["lb"]])
            S.op("dve", lambda e, lbcol=lbcol, omcol=omcol: e.tensor_scalar(out=omcol, in0=lbcol, scalar1=-1.0, scalar2=1.0,
                                                                            op0=ALU.mult, op1=ALU.add),
                 reads=[b["lb"]], writes=[b["lb"]])
        S.op("pool", lambda e: e.memset(maskF[:], 1.0), writes=[b["mask"]])
        S.op("pool", lambda e: e.affine_select(out=maskF[:], in_=maskF[:], pattern=[[1, 64]], compare_op=ALU.is_ge,
                                               fill=0.0, base=0, channel_multiplier=-1), reads=[b["mask"]],
             writes=[b["mask"]])
        S.op("pool", lambda e: e.memset(maskB[:], 1.0), writes=[b["mask"]])
        S.op("pool", lambda e: e.affine_select(out=maskB[:], in_=maskB[:], pattern=[[-1, 64]], compare_op=ALU.is_ge,
                                               fill=0.0, base=0, channel_multiplier=1), reads=[b["mask"]],
             writes=[b["mask"]])
        S.op("dve", lambda e: e.memset(Gp[:, 0:1], 0.0), writes=[b["Gp"]])

        def view(t, off, W=NTOK + 1):
            return AP(t, off, [[W, 128], [64, NCH], [1, 64]])

        def anchor(off):
            return AP(Gp, off, [[NTOK + 1, 128], [64, NCH], [0, 64]])

        def v3(t):
            return AP(t, 0, [[NTOK, 128], [64, NCH], [1, 64]])

        for h in range(4):
            S.dma("sp", T4[:], k.pAq.ap()[h * 128:(h + 1) * 128, :], reads=[k.B["pAq"]], writes=[b["T4"]])
            S.dma("sp", vS[:], k.pAv.ap().rearrange("(c s) d -> s c d", s=64)[:, :, h * 128:(h + 1) * 128],
                  reads=[k.B["pAv"]], writes=[b["vS"]])
            for d_ in range(2):
                sg = 1.0 if d_ == 0 else -1.0
                zsrc = k.pAzf if d_ == 0 else k.pAzb
                zb_ = k.B["pAzf"] if d_ == 0 else k.B["pAzb"]
                lbc = lbv[:, d_ * 4 + h, 0:1]
                omc = lbv[:, d_ * 4 + h, 1:2]
                S.dma("sp", T1[:], zsrc.ap()[h * 128:(h + 1) * 128, :], reads=[zb_], writes=[b["T1"]])
                S.op("act", lambda e: e.activation(out=T1[:], in_=T1[:], func=AF.Sigmoid), reads=[b["T1"]], writes=[b["T1"]])
                S.op("dve", lambda e, lbc=lbc, omc=omc: e.tensor_scalar(out=T1[:], in0=T1[:], scalar1=omc, scalar2=lbc,
                                                                        op0=ALU.mult, op1=ALU.add),
                     reads=[b["T1"], b["lb"]], writes=[b["T1"]])
                S.op("dve", lambda e: e.tensor_scalar(out=T2[:], in0=T1[:], scalar1=-1.0, scalar2=1.0, op0=ALU.mult,
                                                      op1=ALU.add), reads=[b["T1"]], writes=[b["T2"]])
                S.op("act", lambda e: e.activation(out=T1[:], in_=T1[:], func=AF.Ln), reads=[b["T1"]], writes=[b["T1"]])
                S.op("dve", lambda e: e.tensor_tensor_scan(out=Gp[:, 1:NTOK + 1],
                                                           data0=AP(k.onecol, 0, [[1, 128], [0, NTOK]]), data1=T1[:],
                                                           initial=0.0, op0=ALU.mult, op1=ALU.add),
                     reads=[b["T1"], k.Bc], writes=[b["Gp"]])
                eoff = 1 if d_ == 0 else 0
                E = view(Gp, eoff)
                a_mid = anchor(32)
                a_q2 = anchor(0) if d_ == 0 else anchor(64)
                a_k2 = anchor(64) if d_ == 0 else anchor(0)

                def prep(anch, scale, src, srcb, dst, dstb, E=E):
                    S.op("dve", lambda e: e.tensor_tensor(out=v3(T1), in0=E, in1=anch, op=ALU.subtract), reads=[b["Gp"]],
                         writes=[b["T1"]])
                    S.op("act", lambda e: e.activation(out=T1[:], in_=T1[:], func=AF.Exp, scale=scale), reads=[b["T1"]],
                         writes=[b["T1"]])
                    S.op("dve", lambda e: e.tensor_tensor(out=dst[:], in0=src[:], in1=T1[:], op=ALU.mult),
                         reads=[b["T1"], srcb], writes=[dstb])

                prep(a_mid, sg, T4, b["T4"], q1, b["q1"])
                prep(a_mid, -sg, T2, b["T2"], k1, b["k1"])
                oq = 0 if d_ == 0 else 64
                ok_ = 64 if d_ == 0 else 0
                gmid = AP(Gp, 32, [[NTOK + 1, 128], [64, NCH]])
                S.op("dve", lambda e, oq=oq: e.tensor_tensor(out=cqk[:, 0, :], in0=gmid, in1=AP(Gp, oq, [[NTOK + 1, 128], [64, NCH]]),
                                                             op=ALU.subtract), reads=[b["Gp"]], writes=[b["cqk"]])
                S.op("dve", lambda e, ok_=ok_: e.tensor_tensor(out=cqk[:, 1, :], in0=gmid, in1=AP(Gp, ok_, [[NTOK + 1, 128], [64, NCH]]),
                                                               op=ALU.subtract), reads=[b["Gp"]], writes=[b["cqk"]])
                S.op("act", lambda e, sg=sg: e.activation(out=cqk[:, 0, :], in_=cqk[:, 0, :], func=AF.Exp, scale=sg),
                     reads=[b["cqk"]], writes=[b["cqk"]])
                S.op("act", lambda e, sg=sg: e.activation(out=cqk[:, 1, :], in_=cqk[:, 1, :], func=AF.Exp, scale=-sg),
                     reads=[b["cqk"]], writes=[b["cqk"]])

                def v3b(t):
                    return AP(t, 0, [[NTOK, 128], [64, NCH], [1, 64]])

                S.op("dve", lambda e: e.tensor_tensor(out=v3b(q2), in0=v3b(q1), in1=AP(cqk, 0, [[2 * NCH, 128], [1, NCH], [0, 64]]),
                                                      op=ALU.mult), reads=[b["q1"], b["cqk"]], writes=[b["q2"]])
                S.op("dve", lambda e: e.tensor_tensor(out=v3b(k2f), in0=v3b(k1), in1=AP(cqk, NCH, [[2 * NCH, 128], [1, NCH], [0, 64]]),
                                                      op=ALU.mult), reads=[b["k1"], b["cqk"]], writes=[b["k2f"]])
                zoff, koff = (32, 0) if d_ == 0 else (0, 32)
                zv = AP(k1z, zoff, [[NTOK, 128], [64, NCH], [1, 32]])
                kv_o = AP(k1z, koff, [[NTOK, 128], [64, NCH], [1, 32]])
                kv_i = AP(k1, koff, [[NTOK, 128], [64, NCH], [1, 32]])
                S.op("pool", lambda e, zv=zv: e.memset(zv, 0.0), writes=[b["k1z"]])
                S.op("pool", lambda e, kv_o=kv_o, kv_i=kv_i: e.tensor_copy(out=kv_o, in_=kv_i), reads=[b["k1"]],
                     writes=[b["k1z"]])
                S.op("dve", lambda e: e.tensor_tensor(out=dec[:], in0=AP(Gp, 64, [[NTOK + 1, 128], [64, NCH]]),
                                                      in1=AP(Gp, 0, [[NTOK + 1, 128], [64, NCH]]), op=ALU.subtract),
                     reads=[b["Gp"]], writes=[b["dec"]])
                S.op("act", lambda e: e.activation(out=dec[:], in_=dec[:], func=AF.Exp), reads=[b["dec"]],
                     writes=[b["dec"]])
                for c4 in range(NCH // 4):
                    pt, ptb = ptr.next()
                    S.op("pe", [lambda e, pt=pt, j=j, c4=c4: e.transpose(
                        out=pt[0:64, j * 128:(j + 1) * 128], in_=k2f[:, (c4 * 4 + j) * 64:(c4 * 4 + j + 1) * 64],
                        identity=k.ident[:]) for j in range(4)], reads=[b["k2f"], k.Bc], writes=[ptb])
                    o = k2T[:, c4 * 4:c4 * 4 + 4, :]
                    a = AP(pt, 0, [[512, 64], [128, 4], [1, 128]])
                    if c4 % 2 == 0:
                        S.op("act", lambda e, o=o, a=a: e.activation(out=o, in_=a, func=AF.Copy), reads=[ptb],
                             writes=[b["k2T"]])
                    else:
                        S.op("dve", lambda e, o=o, a=a: e.tensor_copy(out=o, in_=a), reads=[ptb], writes=[b["k2T"]])
                order = list(range(NCH)) if d_ == 0 else [3, 2, 1, 0] + list(range(NCH - 1, 3, -1))
                mask = maskF if d_ == 0 else maskB
                S.op("dve", lambda e: e.memset(S32p[0][0][:], 0.0), writes=[S32p[0][1]])
                S.op("dve", lambda e: e.memset(SbA[:, 0:128], 0.0), writes=[slotb[0]])

                granges = [(0, 4)] + [(4 + 8 * g, 8) for g in range(8)]
                if d_ == 0:
                    gorder = granges
                else:
                    gorder = [granges[0]] + granges[:0:-1]
                pos_of = {c: i for i, c in enumerate(order)}

                def att_group(g0, n, mask=mask, d_=d_):
                    pA, pAb = pa.next()
                    safe, unsafe = (32, 0) if d_ == 0 else (0, 32)
                    fns = []
                    for s_ in range(n):
                        cs_ = (g0 + s_) * 64
                        fns.append(lambda e, cs_=cs_, s_=s_: e.matmul(pA[0:64, s_ * 64 + safe:s_ * 64 + safe + 32],
                                                                      lhsT=k1[:, cs_:cs_ + 64],
                                                                      rhs=q1[:, cs_ + safe:cs_ + safe + 32], start=True, stop=True))
                        fns.append(lambda e, cs_=cs_, s_=s_: e.matmul(pA[0:64, s_ * 64 + unsafe:s_ * 64 + unsafe + 32],
                                                                      lhsT=k1z[:, cs_:cs_ + 64],
                                                                      rhs=q1[:, cs_ + unsafe:cs_ + unsafe + 32], start=True,
                                                                      stop=True))
                    S.op("pe", fns, reads=[b["k1"], b["k1z"], b["q1"]], writes=[pAb])
                    aT, aTb = attTs.next()
                    S.op("dve", lambda e: e.tensor_tensor(out=AP(aT, 0, [[512, 64], [64, n], [1, 64]]),
                                                          in0=AP(pA, 0, [[512, 64], [64, n], [1, 64]]),
                                                          in1=AP(mask, 0, [[64, 64], [0, n], [1, 64]]), op=ALU.mult),
                         reads=[pAb, b["mask"]], writes=[aTb])
                    return aT, aTb

                def state_step(i, c):
                    pS, pSb = psS.next()
                    src, srcb = S32p[i % 2]
                    dst, dstb = S32p[(i + 1) % 2]
                    S.op("pe", lambda e: e.matmul(pS[:, 0:128], lhsT=k2T[:, c, :], rhs=vS[:, c, :], start=True, stop=True),
                         reads=[b["k2T"], b["vS"]], writes=[pSb])
                    S.op("dve", lambda e: e.scalar_tensor_tensor(out=dst[:], in0=src[:], scalar=dec[:, c:c + 1],
                                                                 in1=pS[:, 0:128], op0=ALU.mult, op1=ALU.add),
                         reads=[pSb, srcb, b["dec"]], writes=[dstb])
                    S.op("act", lambda e: e.activation(out=SbA[:, (i + 1) * 128:(i + 2) * 128], in_=dst[:], func=AF.Copy),
                         reads=[dstb], writes=[slotb[i + 1]])

                def out_group(g0, n, aT, aTb, d_=d_):
                    pO, pOb = po.next()
                    fns = []
                    rd = [b["vS"], aTb, b["q2"]]
                    for s_ in range(n):
                        c = g0 + s_
                        i = pos_of[c]
                        cs_ = c * 64
                        rd.append(slotb[i])
                        fns.append(lambda e, c=c, s_=s_: e.matmul(pO[:, s_ * 64:(s_ + 1) * 64], lhsT=vS[:, c, :],
                                                                  rhs=aT[:, s_ * 64:(s_ + 1) * 64], start=True, stop=False))
                        fns.append(lambda e, i=i, s_=s_, cs_=cs_: e.matmul(pO[:, s_ * 64:(s_ + 1) * 64],
                                                                           lhsT=SbA[:, i * 128:(i + 1) * 128],
                                                                           rhs=q2[:, cs_:cs_ + 64], start=False, stop=True))
                    S.op("pe", fns, reads=rd, writes=[pOb])
                    c0_, c1_ = g0 * 64, (g0 + n) * 64
                    if d_ == 0:
                        S.op("act", lambda e: e.activation(out=oacc[:, c0_:c1_], in_=pO[:, 0:n * 64], func=AF.Copy),
                             reads=[pOb], writes=[b["oacc"]])
                    else:
                        S.op("dve", lambda e: e.tensor_tensor(out=oacc[:, c0_:c1_], in0=oacc[:, c0_:c1_], in1=pO[:, 0:n * 64],
                                                              op=ALU.add), reads=[pOb, b["oacc"]], writes=[b["oacc"]])

                n_ = len(order)
                pend = None
                for (g0, n) in gorder:
                    aT_, aTb_ = att_group(g0, n)
                    cl = list(range(g0, g0 + n)) if d_ == 0 else list(range(g0 + n - 1, g0 - 1, -1))
                    for c in cl:
                        i = pos_of[c]
                        if i + 1 < n_:
                            state_step(i, c)
                    if pend is not None:
                        out_group(*pend)
                    pend = (g0, n, aT_, aTb_)
                out_group(*pend)
            if True:
                og = k1
                S.dma("sp", og[:], k.pAog.ap()[h * 128:(h + 1) * 128, :], reads=[k.B["pAog"]], writes=[b["k1"]])
                for (t0, W) in BLOCKS:
                    if last and t0 == 0:
                        continue
                    head_readout(k, S, st, "hg", oacc[:, t0:t0 + W], b["oacc"], t0, W, gn[:, layer * 4 + h:layer * 4 + h + 1],
                                 b["gn"], og[:, t0:t0 + W], b["k1"], q1[:, t0:t0 + W], b["q1"], pa, sqs, tmps)
                S.dma("pool", k.ybr.ap()[0, h * 128:(h + 1) * 128, :], q1[:], reads=[b["q1"]], writes=[k.B["ybr0"]])
        S.emit()


def phase_mlp(k, layer, last):
    nc, S, I = k.nc, k.S, k.I
    with ExitStack() as st:
        sb = lambda n, s, d: st.enter_context(nc.sbuf_tensor(U("ml_" + n), s, d))
        uT = sb("uT", [128, 4, NTOK], BF16)
        vB = sb("vB", [128, NT, 512], BF16)
        yb = sb("yb", [128, 4, NTOK], BF16)
        wsT32 = sb("wsT32", [128, 4, 128], F32)
        wsT = sb("wsT", [128, 4, 128], BF16)
        bsr = sb("bsr", [128, 512], F32)
        tmps = Rot([(sb("tmp%d" % i, [128, 512], F32), Buf()) for i in range(2)])
        pms = Rot([(st.enter_context(nc.psum_tensor(U("ml_pm%d" % i), [128, 512], F32)), Buf()) for i in range(2)])
        bu, bv, by, bw, bb = Buf(), Buf(), Buf(), Buf(), Buf()
        S.dma("sp", uT[:], k.pBu.ap().rearrange("(g d) t -> d g t", d=128), reads=[k.B["pBu"]], writes=[bu])
        S.dma("sp", vB[:], k.pBv.ap().rearrange("(n p) c -> p n c", p=128), reads=[k.B["pBv"]], writes=[bv])
        S.dma("sp", wsT32[:], I["wsT"].ap()[layer], writes=[bw])
        S.dma("sp", bsr[:], AP(I["bs"], layer * 512, [[0, 128], [1, 512]]), writes=[bb])
        S.op("dve", lambda e: e.tensor_copy(out=wsT[:], in_=wsT32[:]), reads=[bw], writes=[bw])
        for i in range(NT):
            if last and i < 2:
                continue
            pm, pmb = pms.next()
            for g in range(4):
                S.op("pe", lambda e, g=g, pm=pm, i=i: e.matmul(pm[:, g * 128:(g + 1) * 128], lhsT=vB[:, i, g * 128:(g + 1) * 128],
                                                               rhs=wsT[:, g, :], start=True, stop=True), reads=[bv, bw],
                     writes=[pmb])
            tmp, tb = tmps.next()
            S.op("dve", lambda e, pm=pm, tmp=tmp: e.tensor_tensor(out=tmp[:], in0=pm[:], in1=bsr[:], op=ALU.add),
                 reads=[pmb, bb], writes=[tb])
            S.op("dve", lambda e, tmp=tmp, i=i: e.tensor_tensor(
                out=yb[:, :, i * 128:(i + 1) * 128], in0=AP(tmp, 0, [[512, 128], [128, 4], [1, 128]]),
                in1=uT[:, :, i * 128:(i + 1) * 128], op=ALU.mult), reads=[tb, bu], writes=[by])
        if last:
            S.op("dve", lambda e: e.memset(yb[:, :, 0:256], 0.0), writes=[by])
        S.dma("sp", k.ybr.ap()[1].rearrange("(g d) t -> d g t", d=128), yb[:], reads=[by], writes=[k.B["ybr1"]])
        S.emit()


def phase_attn(k, layer, last):
    nc, S, I = k.nc, k.S, k.I
    lam_init = 0.8 - 0.6 * math.exp(-0.3 * layer)
    with ExitStack() as st:
        sb = lambda n, s, d: st.enter_context(nc.sbuf_tensor(U("at_" + n), s, d))
        qT = sb("qT", [128, NTOK], BF16)
        kT = sb("kT", [128, NTOK], BF16)
        vC = sb("vC", [128, NT, 128], BF16)
        yc = sb("yc", [128, NTOK], BF16)
        dl = sb("dl", [128, 256], F32)
        dl2 = sb("dl2", [128, 128], F32)
        lam = sb("lam", [128, 4], F32)
        sl = sb("sl", [128, 8], F32)
        slc = sb("slc", [128, 8], F32)
        mx = sb("mx", [128, 2, 2, 16], F32)
        nb = sb("nb", [128, 8], F32)
        pTs = Rot([(sb("pT%d" % i, [128, 1024], BF16), Buf()) for i in range(4)])
        t2s = Rot([(sb("t2_%d" % i, [128, 512], BF16), Buf()) for i in range(6)])
        accPs = [(sb("accP%d" % i, [128, 512], F32), Buf()) for i in range(2)]
        ones32 = sb("ones32", [128, 128], F32)
        sqs = Rot([(sb("sq%d" % i, [128, 512], BF16), Buf()) for i in range(2)])
        tmps = Rot([(sb("tmp%d" % i, [128, 512], F32), Buf()) for i in range(2)])
        o0 = sb("o0", [128, 512], F32)
        o1 = sb("o1", [128, 512], F32)
        rl = sb("rl", [128, 512], F32)
        b = {n: Buf(n) for n in "qT kT vC yc dl lam sl mx nb o0 o1 rl ones32".split()}
        pss = Rot([(st.enter_context(nc.psum_tensor(U("at_ps%d" % i), [128, 1024], F32)), Buf()) for i in range(2)])
        pos = [(st.enter_context(nc.psum_tensor(U("at_po%d" % i), [128, 512], F32)), Buf()) for i in range(2)]
        prs = Rot([(st.enter_context(nc.psum_tensor(U("at_pr%d" % i), [128, 512], F32)), Buf()) for i in range(2)])

        S.dma("sp", dl[:], AP(I["dlam"], layer * 256, [[0, 128], [1, 256]]), writes=[b["dl"]])
        S.dma("sp", sl[:], I["sublnT"].ap().rearrange("p a b -> p (a b)"), writes=[b["sl"]])
        S.op("dve", lambda e: e.tensor_tensor(out=AP(dl2, 0, [[128, 128], [64, 2], [1, 64]]),
                                              in0=AP(dl, 0, [[256, 128], [128, 2], [1, 64]]),
                                              in1=AP(dl, 64, [[256, 128], [128, 2], [1, 64]]), op=ALU.mult),
             reads=[b["dl"]], writes=[b["dl"]])
        S.op("dve", lambda e: e.tensor_reduce(out=lam[:, 0:2], in_=AP(dl2, 0, [[128, 128], [64, 2], [1, 64]]), axis=AX.X,
                                              op=ALU.add), reads=[b["dl"]], writes=[b["lam"]])
        S.op("act", lambda e: e.activation(out=lam[:, 0:2], in_=lam[:, 0:2], func=AF.Exp), reads=[b["lam"]],
             writes=[b["lam"]])
        S.op("dve", lambda e: e.tensor_tensor(out=lam[:, 2:3], in0=lam[:, 1:2], in1=lam[:, 0:1], op=ALU.subtract),
             reads=[b["lam"]], writes=[b["lam"]])
        S.op("dve", lambda e: e.tensor_scalar(out=lam[:, 3:4], in0=lam[:, 2:3], scalar1=-lam_init, scalar2=None,
                                              op0=ALU.add), reads=[b["lam"]], writes=[b["lam"]])
        S.op("dve", lambda e: e.tensor_scalar(out=slc[:], in0=sl[:], scalar1=(1.0 - lam_init), scalar2=None, op0=ALU.mult),
             reads=[b["sl"]], writes=[b["sl"]])

        for h in range(4):
            S.dma("sp", qT[:], k.pCq.ap()[h * 128:(h + 1) * 128, :], reads=[k.B["pCq"]], writes=[b["qT"]])
            S.dma("sp", kT[:], k.pCk.ap()[h * 128:(h + 1) * 128, :], reads=[k.B["pCk"]], writes=[b["kT"]])
            S.dma("sp", vC[:], k.pCv.ap().rearrange("(n p) d -> p n d", p=128)[:, :, h * 128:(h + 1) * 128],
                  reads=[k.B["pCv"]], writes=[b["vC"]])
            S.op("dve", lambda e: e.memset(mx[:], 0.0), writes=[b["mx"]])
            for qi, (src, srcb) in enumerate([(qT, b["qT"]), (kT, b["kT"])]):
                for bi, (t0, W) in enumerate(BLOCKS):
                    sq, sqb = sqs.next()
                    S.op("act", lambda e, sq=sq, src=src, t0=t0, W=W: e.activation(out=sq[:, 0:W], in_=src[:, t0:t0 + W],
                                                                                   func=AF.Square), reads=[srcb],
                         writes=[sqb])
                    for c in range(2):
                        pr, prb = prs.next()
                        sel = k.sel0 if c == 0 else k.sel1
                        S.op("pe", lambda e, pr=pr, sel=sel, sq=sq, W=W: e.matmul(pr[:, 0:W], lhsT=sel[:], rhs=sq[:, 0:W],
                                                                                  start=True, stop=True),
                             reads=[sqb, k.Bc], writes=[prb])
                        S.op("dve", lambda e, pr=pr, qi=qi, c=c, bi=bi, W=W: e.tensor_reduce(
                            out=mx[:, qi, c, bi:bi + 1], in_=pr[:, 0:W], axis=AX.X, op=ALU.max), reads=[prb],
                            writes=[b["mx"]])
            S.op("dve", lambda e: e.tensor_reduce(out=nb[:, 0:4], in_=AP(mx, 0, [[64, 128], [16, 4], [1, 16]]), axis=AX.X,
                                                  op=ALU.max), reads=[b["mx"]], writes=[b["nb"]])
            S.op("dve", lambda e: e.tensor_tensor(out=nb[:, 4:6], in0=nb[:, 0:2], in1=nb[:, 2:4], op=ALU.mult),
                 reads=[b["nb"]], writes=[b["nb"]])
            S.op("act", lambda e: e.activation(out=nb[:, 4:6], in_=nb[:, 4:6], func=AF.Ln), reads=[b["nb"]], writes=[b["nb"]])
            S.op("act", lambda e: e.activation(out=nb[:, 4:6], in_=nb[:, 4:6], func=AF.Exp, scale=0.5), reads=[b["nb"]],
                 writes=[b["nb"]])
            S.op("dve", lambda e: e.tensor_scalar(out=nb[:, 6:8], in0=nb[:, 4:6], scalar1=-0.125, scalar2=None,
                                                  op0=ALU.mult), reads=[b["nb"]], writes=[b["nb"]])
            S.op("dve", lambda e: e.tensor_tensor(out=nb[:, 5:6], in0=nb[:, 6:7], in1=nb[:, 7:8], op=ALU.min),
                 reads=[b["nb"]], writes=[b["nb"]])
            seq = []
            for (t0, W) in BLOCKS:
                if t0 == 0:
                    if last:
                        continue
                    keys = list(range(0, 2))
                else:
                    keys = list(range(0, NT))
                for ji, j in enumerate(keys):
                    seq.append((t0, W, ji, j, len(keys)))

            def pair(t, W):
                return AP(t, 0, [[1024, 128], [512, 2], [1, W]])

            def qk_exp(t0, W, ji, j, nk):
                ps, psb = pss.next()
                S.op("pe", [lambda e, c=c: e.matmul(ps[:, c * 512:c * 512 + W],
                                                    lhsT=kT[c * 64:(c + 1) * 64, j * 128:(j + 1) * 128],
                                                    rhs=qT[c * 64:(c + 1) * 64, t0:t0 + W], start=True, stop=True)
                            for c in range(2)], reads=[b["kT"], b["qT"]], writes=[psb])
                pT, pTb = pTs.next()
                S.op("act", lambda e: e.activation(out=pair(pT, W), in_=pair(ps, W), func=AF.Exp, scale=0.125,
                                                   bias=nb[:, 5:6]), reads=[psb, b["nb"]], writes=[pTb])
                return pT, pTb

            prev = {}

            def av(t0, W, ji, j, nk, pT, pTb, h=h):
                S.op("pe", [lambda e, c=c: e.matmul(pos[c][0][:, 0:W], lhsT=vC[:, j, :], rhs=pT[:, c * 512:c * 512 + W],
                                                    start=(ji == 0), stop=(ji == nk - 1)) for c in range(2)],
                     reads=[b["vC"], pTb], writes=[pos[0][1], pos[1][1]])
                if ji % 2 == 0:
                    prev["p"] = (pT, pTb)
                    return
                pP, pPb = prev["p"]
                for c in range(2):
                    aP, aPb = accPs[c]
                    if ji == 1:
                        S.op("dve", lambda e, c=c, aP=aP: e.tensor_tensor(out=aP[:, 0:W], in0=pP[:, c * 512:c * 512 + W],
                                                                          in1=pT[:, c * 512:c * 512 + W], op=ALU.add),
                             reads=[pTb, pPb], writes=[aPb])
                    else:
                        t2, t2b = t2s.next()
                        S.op("dve", lambda e, c=c, t2=t2: e.tensor_tensor(out=t2[:, 0:W], in0=pP[:, c * 512:c * 512 + W],
                                                                          in1=pT[:, c * 512:c * 512 + W], op=ALU.add),
                             reads=[pTb, pPb], writes=[t2b])
                        S.op("dve", lambda e, aP=aP, t2=t2: e.tensor_tensor(out=aP[:, 0:W], in0=aP[:, 0:W], in1=t2[:, 0:W],
                                                                            op=ALU.add), reads=[t2b, aPb], writes=[aPb])
                if ji != nk - 1:
                    return
                for c in range(2):
                    aP, aPb = accPs[c]
                    po, pob = pos[c]
                    pl, plb = prs.next()
                    hi, hib = t2s.next()
                    lo, lob = t2s.next()
                    S.op("dve", lambda e, hi=hi, aP=aP: e.tensor_copy(out=hi[:, 0:W], in_=aP[:, 0:W]), reads=[aPb], writes=[hib])
                    S.op("dve", lambda e, hi=hi, lo=lo, aP=aP: e.tensor_tensor(out=lo[:, 0:W], in0=aP[:, 0:W], in1=hi[:, 0:W],
                                                                               op=ALU.subtract), reads=[aPb, hib], writes=[lob])
                    S.op("pe", [lambda e, pl=pl, hi=hi: e.matmul(pl[:, 0:W], lhsT=k.ones[:], rhs=hi[:, 0:W], start=True, stop=False),
                                lambda e, pl=pl, lo=lo: e.matmul(pl[:, 0:W], lhsT=k.ones[:], rhs=lo[:, 0:W], start=False, stop=True)],
                         reads=[hib, lob, k.Bc], writes=[plb])
                    oc, ocb = (o0, b["o0"]) if c == 0 else (o1, b["o1"])
                    S.op("act", lambda e, pl=pl: e.activation(out=rl[:, 0:W], in_=pl[:, 0:W], func=AF.Ln), reads=[plb],
                         writes=[b["rl"]])
                    S.op("act", lambda e: e.activation(out=rl[:, 0:W], in_=rl[:, 0:W], func=AF.Exp, scale=-1.0), reads=[b["rl"]],
                         writes=[b["rl"]])
                    S.op("dve", lambda e, po=po, oc=oc: e.tensor_tensor(out=oc[:, 0:W], in0=po[:, 0:W], in1=rl[:, 0:W],
                                                                        op=ALU.mult), reads=[pob, b["rl"]], writes=[ocb])
                S.op("dve", lambda e: e.scalar_tensor_tensor(out=o0[:, 0:W], in0=o1[:, 0:W], scalar=lam[:, 3:4], in1=o0[:, 0:W],
                                                             op0=ALU.mult, op1=ALU.add), reads=[b["o0"], b["o1"], b["lam"]],
                     writes=[b["o0"]])
                head_readout(k, S, st, "at", o0[:, 0:W], b["o0"], t0, W, slc[:, layer * 4 + h:layer * 4 + h + 1], b["sl"], None,
                             None, yc[:, t0:t0 + W], b["yc"], prs, sqs, tmps)

            LOOK = 1
            pend = []
            for idx in range(len(seq) + LOOK):
                if idx < len(seq):
                    pend.append(qk_exp(*seq[idx]))
                if idx >= LOOK:
                    pT_, pTb_ = pend.pop(0)
                    av(*seq[idx - LOOK], pT_, pTb_)
            if last:
                S.op("dve", lambda e: e.memset(yc[:, 0:256], 0.0), writes=[b["yc"]])
            S.dma("pool", k.ybr.ap()[2, h * 128:(h + 1) * 128, :], yc[:], reads=[b["yc"]], writes=[k.B["ybr2"]])
        S.emit()


def phase_merge(k, layer, last):
    nc, S, I = k.nc, k.S, k.I
    with ExitStack() as st:
        sb = lambda n, s, d: st.enter_context(nc.sbuf_tensor(U("mg_" + n), s, d))
        wb = sb("wb", [128, 3, 4, D], BF16)
        wo = sb("wo", [128, 8, D], BF16)
        bw = Buf()
        for i in range(3):
            S.dma("pool", wb[:, i, :, :], I["w_branch"].ap()[layer, i].rearrange("(k p) n -> p k n", p=128), writes=[bw])
        S.dma("pool", wo[:], I["w_out"].ap()[layer].rearrange("(k p) n -> p k n", p=128), writes=[bw])
        G1 = [sb("G1_%d" % w, [128, D], F32) for w in range(2)]
        A2 = [sb("A2_%d" % w, [128, D], F32) for w in range(2)]
        B2 = [sb("B2_%d" % w, [128, D], F32) for w in range(2)]
        mb = Buf()
        for w in range(2):
            if last and w == 1:
                continue
            load_rep(k, S, G1[w], mb, w, 2)
            load_rep(k, S, A2[w], mb, w, 4)
            load_rep(k, S, B2[w], mb, w, 3)
        ys = Rot([(sb("y%d" % i, [128, 3, 4, 512], BF16), Buf()) for i in range(2)])
        sgs = Rot([(sb("sg%d" % i, [128, 24, 512], BF16), Buf()) for i in range(2)])
        mTs = Rot([(sb("mT%d" % i, [128, 8, 512], BF16), Buf()) for i in range(2)])
        macc = sb("macc", [128, 512], F32)
        maccb = Buf()
        tmps = Rot([(sb("tmp%d" % i, [128, 512], F32), Buf()) for i in range(2)])
        xts = Rot([(sb("xt%d" % i, [128, D], F32), Buf()) for i in range(2)])
        junks = Rot([(sb("junk%d" % i, [128, D], F32), Buf()) for i in range(2)])
        hbs = Rot([(sb("hb%d" % i, [128, D], BF16), Buf()) for i in range(2)])
        sss = Rot([(sb("ss%d" % i, [128, 4], F32), Buf()) for i in range(2)])
        h2s = Rot([(sb("h2s%d" % i, [128, 8, 512], BF16), Buf()) for i in range(2)])
        pbs = Rot([(st.enter_context(nc.psum_tensor(U("mg_pb%d" % i), [128, 512], F32)), Buf()) for i in range(3)])
        pms = Rot([(st.enter_context(nc.psum_tensor(U("mg_pm%d" % i), [128, 512], F32)), Buf()) for i in range(2)])
        pts = Rot([(st.enter_context(nc.psum_tensor(U("mg_pt%d" % i), [128, 512], BF16)), Buf()) for i in range(2)])
        ybufs = [k.B["ybr0"], k.B["ybr1"], k.B["ybr2"]]
        def part1(t0, W):
            mT, mTb = mTs.next()
            y, yb_ = ys.next()
            sg, sgb = sgs.next()
            for i in range(3):
                S.dma("sp", y[:, i, :, 0:W], k.ybr.ap()[i].rearrange("(k p) t -> p k t", p=128)[:, :, t0:t0 + W],
                      reads=[ybufs[i]], writes=[yb_])
            S.dma("sp", sg[:, :, 0:W], k.pG.ap().rearrange("(k p) t -> p k t", p=128)[:, :, t0:t0 + W], reads=[k.B["pG"]],
                  writes=[sgb])
            for oc in range(8):
                for i in range(3):
                    pb, pbb = pbs.next()
                    S.op("pe", [lambda e, pb=pb, i=i, kk=kk, oc=oc, y=y: e.matmul(
                        pb[:, 0:W], lhsT=wb[:, i, kk, oc * 128:(oc + 1) * 128], rhs=y[:, i, kk, 0:W], start=(kk == 0),
                        stop=(kk == 3)) for kk in range(4)], reads=[bw, yb_], writes=[pbb])
                    if i == 0:
                        S.op("dve", lambda e, pb=pb, sg=sg, oc=oc: e.tensor_tensor(out=macc[:, 0:W], in0=pb[:, 0:W],
                                                                                   in1=sg[:, oc, 0:W], op=ALU.mult),
                             reads=[pbb, sgb], writes=[maccb])
                    else:
                        tmp, tb = tmps.next()
                        S.op("dve", lambda e, pb=pb, sg=sg, oc=oc, i=i, tmp=tmp: e.tensor_tensor(
                            out=tmp[:, 0:W], in0=pb[:, 0:W], in1=sg[:, i * 8 + oc, 0:W], op=ALU.mult), reads=[pbb, sgb],
                            writes=[tb])
                        if i == 1:
                            S.op("dve", lambda e, tmp=tmp: e.tensor_tensor(out=macc[:, 0:W], in0=macc[:, 0:W], in1=tmp[:, 0:W],
                                                                           op=ALU.add), reads=[tb, maccb], writes=[maccb])
                        else:
                            S.op("dve", lambda e, tmp=tmp, oc=oc: e.tensor_tensor(out=mT[:, oc, 0:W], in0=macc[:, 0:W],
                                                                                  in1=tmp[:, 0:W], op=ALU.add),
                                 reads=[tb, maccb], writes=[mTb])
            return mT, mTb

        def part2(t0, W, mT, mTb):
            w_ = 1 if t0 == 0 else 0
            h2, h2b = h2s.next()
            for ts in range(W // 128):
                row0 = t0 + ts * 128
                xt, xb = xts.next()
                S.dma("sp", xt[:], k.xs.ap()[row0:row0 + 128, :], reads=[k.Bxs[row0 // 128]], writes=[xb])
                for half in range(2):
                    pm, pmb = pms.next()
                    S.op("pe", [lambda e, pm=pm, kk=kk, ts=ts, half=half: e.matmul(
                        pm[:], lhsT=mT[:, kk, ts * 128:(ts + 1) * 128], rhs=wo[:, kk, half * 512:(half + 1) * 512],
                        start=(kk == 0), stop=(kk == 7)) for kk in range(8)], reads=[mTb, bw], writes=[pmb])
                    tmp, tb = tmps.next()
                    S.op("dve", lambda e, pm=pm, tmp=tmp, half=half: e.tensor_tensor(
                        out=tmp[:], in0=pm[:], in1=G1[w_][:, half * 512:(half + 1) * 512], op=ALU.mult), reads=[pmb, mb],
                        writes=[tb])
                    S.op("dve", lambda e, tmp=tmp, xt=xt, half=half: e.tensor_tensor(
                        out=xt[:, half * 512:(half + 1) * 512], in0=xt[:, half * 512:(half + 1) * 512], in1=tmp[:],
                        op=ALU.add), reads=[tb, xb], writes=[xb])
                S.dma("pool", k.xs.ap()[row0:row0 + 128, :], xt[:], reads=[xb], writes=[k.Bxs[row0 // 128]])
                junk, jb = junks.next()
                hb, hbb = hbs.next()
                ss, ssb = sss.next()
                rms_mod(S, xt, xb, A2[w_], mb, B2[w_], mb, hb, hbb, ss, ssb, junk, jb)
                for half in range(2):
                    pt, ptb = pts.next()
                    S.op("pe", [lambda e, pt=pt, hb=hb, j=j, half=half: e.transpose(
                        out=pt[:, j * 128:(j + 1) * 128], in_=hb[:, (half * 4 + j) * 128:(half * 4 + j + 1) * 128],
                        identity=k.ident[:]) for j in range(4)], reads=[hbb, k.Bc], writes=[ptb])
                    outap = h2[:, half * 4:half * 4 + 4, ts * 128:(ts + 1) * 128]
                    inap = AP(pt, 0, [[512, 128], [128, 4], [1, 128]])
                    S.op("act", lambda e, o=outap, a=inap: e.activation(out=o, in_=a, func=AF.Copy), reads=[ptb],
                         writes=[h2b])
            S.dma("pool", k.h2T.ap().rearrange("(k p) t -> p k t", p=128)[:, :, t0:t0 + W], h2[:, :, 0:W], reads=[h2b],
                  writes=[k.B["h2T"]])

        pend = None
        for (t0, W) in BLOCKS:
            if last and t0 == 0:
                continue
            mT_, mTb_ = part1(t0, W)
            if pend is not None:
                part2(*pend)
            pend = (t0, W, mT_, mTb_)
        if pend is not None:
            part2(*pend)
        S.emit()


def phase_ffn(k, layer, last):
    nc, S, I = k.nc, k.S, k.I
    moe = (layer % 2 == 1)
    if moe:
        FU = 4
        experts = [(I["moe_wg"].ap()[0, e], I["moe_wu"].ap()[0, e], I["moe_wd"].ap()[0, e], DFFE) for e in range(NEXP)]
        groups = [list(range(2, 18)), list(range(18, 34))]
    else:
        FU = 2
        experts = [(I["ffn_wg"].ap()[0], I["ffn_wu"].ap()[0], I["ffn_wd"].ap()[0], DFF)]
        groups = [list(range(0, 17)), list(range(17, 34))]
    with ExitStack() as st:
        sb = lambda n, s, d: st.enter_context(nc.sbuf_tensor(U("ff_" + n), s, d))
        NTG = 17
        acc = sb("acc", [128, NTG, D], F32)
        accb = [Buf() for _ in range(NTG)]
        h2 = sb("h2", [128, 8, NTG * 128], BF16)
        h2b = Buf()
        wgu = Rot([(sb("wgu%d" % i, [128, 8, 2, FU * 128], BF16), Buf()) for i in range(2)])
        wds = Rot([(sb("wd%d" % i, [128, FU, D], BF16), Buf()) for i in range(2)])
        acts = Rot([(sb("act%d" % i, [128, FU, 512], BF16), Buf()) for i in range(2)])
        sgs = Rot([(sb("sg%d" % i, [128, 512], F32), Buf()) for i in range(2)])
        G2 = sb("G2", [128, D], F32)
        gf = sb("gf", [128, D], F32)
        mb = Buf()
        xts = Rot([(sb("xt%d" % i, [128, D], F32), Buf()) for i in range(2)])
        junks = Rot([(sb("junk%d" % i, [128, D], F32), Buf()) for i in range(2)])
        sss = Rot([(sb("ss%d" % i, [128, 4], F32), Buf()) for i in range(2)])
        comb = sb("comb", [128, NTG, 8], F32)
        combb = Buf()
        wr32 = sb("wr32", [128, 8, 8], F32)
        wr = sb("wr", [128, 8, 8], BF16)
        wrb = Buf()
        rt = sb("rt", [128, 8, 8], F32)
        rtb = Buf()
        pgs = Rot([(st.enter_context(nc.psum_tensor(U("ff_pg%d" % i), [128, 512], F32)), Buf()) for i in range(2)])
        pus = Rot([(st.enter_context(nc.psum_tensor(U("ff_pu%d" % i), [128, 512], F32)), Buf()) for i in range(2)])
        pds = Rot([(st.enter_context(nc.psum_tensor(U("ff_pd%d" % i), [128, 512], F32)), Buf()) for i in range(3)])
        prt = st.enter_context(nc.psum_tensor(U("ff_pr"), [128, 512], F32))
        prtb = Buf()
        if moe:
            S.dma("sp", wr32[:], I["moe_router"].ap()[0].rearrange("(k p) e -> p k e", p=128), writes=[wrb])
            S.op("dve", lambda e: e.tensor_copy(out=wr[:], in_=wr32[:]), reads=[wrb], writes=[wrb])
        if last:
            S.dma("sp", gf[:], AP(I["g_final"], 0, [[0, 128], [1, D]]), writes=[mb])
        for gi, tiles in enumerate(groups):
            ntg = len(tiles)
            tok0 = tiles[0] * 128
            ntok = ntg * 128
            blocks = [(o, min(512, ntok - o)) for o in range(0, ntok, 512)]
            S.dma("sp", h2[:, :, 0:ntok], k.h2T.ap().rearrange("(k p) t -> p k t", p=128)[:, :, tok0:tok0 + ntok],
                  reads=[k.B["h2T"]], writes=[h2b])
            if moe:
                for ti in range(ntg):
                    S.op("pe", [lambda e, kk=kk, ti=ti: e.matmul(prt[:, 0:8], lhsT=h2[:, kk, ti * 128:(ti + 1) * 128],
                                                                 rhs=wr[:, kk, :], start=(kk == 0), stop=(kk == 7))
                                for kk in range(8)], reads=[h2b, wrb], writes=[prtb])
                    S.op("dve", lambda e: e.tensor_copy(out=rt[:, 0, :], in_=prt[:, 0:8]), reads=[prtb], writes=[rtb])
                    S.op("dve", lambda e: e.max(out=rt[:, 1, :], in_=rt[:, 0, :]), reads=[rtb], writes=[rtb])
                    S.op("dve", lambda e: e.tensor_scalar(out=rt[:, 2, :], in0=rt[:, 0, :], scalar1=rt[:, 1, 1:2], scalar2=None,
                                                          op0=ALU.is_ge), reads=[rtb], writes=[rtb])
                    S.op("dve", lambda e: e.tensor_scalar(out=rt[:, 5, 0:1], in0=rt[:, 1, 0:1], scalar1=-1.0, scalar2=None,
                                                          op0=ALU.mult), reads=[rtb], writes=[rtb])
                    S.op("act", lambda e: e.activation(out=rt[:, 3, :], in_=rt[:, 0, :], func=AF.Exp, bias=rt[:, 5, 0:1]),
                         reads=[rtb], writes=[rtb])
                    S.op("dve", lambda e: e.tensor_tensor(out=rt[:, 4, :], in0=rt[:, 3, :], in1=rt[:, 2, :], op=ALU.mult),
                         reads=[rtb], writes=[rtb])
                    S.op("dve", lambda e: e.tensor_reduce(out=rt[:, 5, 1:2], in_=rt[:, 4, :], axis=AX.X, op=ALU.add),
                         reads=[rtb], writes=[rtb])
                    S.op("dve", lambda e: e.reciprocal(out=rt[:, 5, 2:3], in_=rt[:, 5, 1:2]), reads=[rtb], writes=[rtb])
                    S.op("dve", lambda e, ti=ti: e.tensor_scalar(out=comb[:, ti, :], in0=rt[:, 4, :], scalar1=rt[:, 5, 2:3],
                                                                 scalar2=None, op0=ALU.mult), reads=[rtb], writes=[combb])
            def up(o, W, w, wbuf):
                at, atb = acts.next()
                for fc in range(FU):
                    pg, pgb = pgs.next()
                    pu, pub = pus.next()
                    S.op("pe", [lambda e, pg=pg, kk=kk, fc=fc: e.matmul(
                        pg[:, 0:W], lhsT=w[:, kk, 0, fc * 128:(fc + 1) * 128], rhs=h2[:, kk, o:o + W], start=(kk == 0),
                        stop=(kk == 7)) for kk in range(8)], reads=[wbuf, h2b], writes=[pgb])
                    S.op("pe", [lambda e, pu=pu, kk=kk, fc=fc: e.matmul(
                        pu[:, 0:W], lhsT=w[:, kk, 1, fc * 128:(fc + 1) * 128], rhs=h2[:, kk, o:o + W], start=(kk == 0),
                        stop=(kk == 7)) for kk in range(8)], reads=[wbuf, h2b], writes=[pub])
                    sg, sgb = sgs.next()
                    S.op("act", lambda e, pg=pg, sg=sg: e.activation(out=sg[:, 0:W], in_=pg[:, 0:W], func=AF.Silu),
                         reads=[pgb], writes=[sgb])
                    S.op("dve", lambda e, pu=pu, sg=sg, fc=fc: e.tensor_tensor(
                        out=at[:, fc, 0:W], in0=sg[:, 0:W], in1=pu[:, 0:W], op=ALU.mult), reads=[sgb, pub],
                        writes=[atb])
                return at, atb

            def down(o, W, at, atb, wd, wdb, ei, first):
                for ts in range(W // 128):
                    ti = o // 128 + ts
                    for half in range(2):
                        pd, pdb = pds.next()
                        S.op("pe", [lambda e, pd=pd, fc=fc, ts=ts, half=half: e.matmul(
                            pd[:], lhsT=at[:, fc, ts * 128:(ts + 1) * 128], rhs=wd[:, fc, half * 512:(half + 1) * 512],
                            start=(fc == 0), stop=(fc == FU - 1)) for fc in range(FU)], reads=[atb, wdb],
                            writes=[pdb])
                        av = acc[:, ti, half * 512:(half + 1) * 512]
                        if moe:
                            cs_ = comb[:, ti, ei:ei + 1]
                            if first:
                                S.op("dve", lambda e, pd=pd, av=av, cs_=cs_: e.tensor_scalar(
                                    out=av, in0=pd[:], scalar1=cs_, scalar2=None, op0=ALU.mult),
                                    reads=[pdb, combb], writes=[accb[ti]])
                            else:
                                S.op("dve", lambda e, pd=pd, av=av, cs_=cs_: e.scalar_tensor_tensor(
                                    out=av, in0=pd[:], scalar=cs_, in1=av, op0=ALU.mult, op1=ALU.add),
                                    reads=[pdb, combb, accb[ti]], writes=[accb[ti]])
                        else:
                            if first:
                                S.op("act", lambda e, pd=pd, av=av: e.activation(out=av, in_=pd[:], func=AF.Copy),
                                     reads=[pdb], writes=[accb[ti]])
                            else:
                                S.op("dve", lambda e, pd=pd, av=av: e.tensor_tensor(out=av, in0=av, in1=pd[:], op=ALU.add),
                                     reads=[pdb, accb[ti]], writes=[accb[ti]])

            first = True
            pending = None
            for ei, (wg_ap, wu_ap, wd_ap, dff) in enumerate(experts):
                wg_v = wg_ap.rearrange("(k p) n -> p k n", p=128)
                wu_v = wu_ap.rearrange("(k p) n -> p k n", p=128)
                wd_v = wd_ap.rearrange("(f p) n -> p f n", p=128)
                nfc = dff // 128
                for u0 in range(0, nfc, FU):
                    w, wbuf = wgu.next()
                    wd, wdb = wds.next()
                    S.dma("pool", w[:, :, 0, :], wg_v[:, :, u0 * 128:(u0 + FU) * 128], writes=[wbuf])
                    S.dma("pool", w[:, :, 1, :], wu_v[:, :, u0 * 128:(u0 + FU) * 128], writes=[wbuf])
                    S.dma("pool", wd[:], wd_v[:, u0:u0 + FU, :], writes=[wdb])
                    for (o, W) in blocks:
                        at, atb = up(o, W, w, wbuf)
                        if pending is not None:
                            down(*pending)
                        pending = (o, W, at, atb, wd, wdb, ei, first)
                    first = False
            if pending is not None:
                down(*pending)
            for ti, tile in enumerate(tiles):
                w_ = 1 if tile < 2 else 0
                if ti == 0 or (tile == 2 and not moe):
                    load_rep(k, S, G2, mb, w_, 5)
                xt, xb = xts.next()
                row0 = tile * 128
                S.dma("sp", xt[:], k.xs.ap()[row0:row0 + 128, :], reads=[k.Bxs[row0 // 128]], writes=[xb])
                S.op("dve", lambda e, ti=ti: e.tensor_tensor(out=acc[:, ti, :], in0=acc[:, ti, :], in1=G2[:], op=ALU.mult),
                     reads=[accb[ti], mb], writes=[accb[ti]])
                S.op("dve", lambda e, ti=ti, xt=xt: e.tensor_tensor(out=xt[:], in0=xt[:], in1=acc[:, ti, :], op=ALU.add),
                     reads=[accb[ti], xb], writes=[xb])
                if not last:
                    S.dma("pool", k.xs.ap()[row0:row0 + 128, :], xt[:], reads=[xb], writes=[k.Bxs[row0 // 128]])
                else:
                    junk, jb = junks.next()
                    ss, ssb = sss.next()
                    rms_mod(S, xt, xb, gf, mb, None, None, None, None, ss, ssb, junk, jb)
                    S.dma("pool", k.out.ap()[row0 - NCTX:row0 - NCTX + 128, :], junk[:], reads=[jb], writes=[k.B["out"]],
                          is_output=True)
        S.emit()


def _rope_tables():
    t = np.arange(NLAT)
    row = (t // 64).astype(np.float32)
    col = (t % 64).astype(np.float32)
    freqs = (10000.0 ** (-np.arange(16, dtype=np.float32) / 16)).astype(np.float32)
    C = np.ones((128, NTOK), np.float32)
    Sn = np.zeros((128, NTOK), np.float32)
    for p in range(128):
        d = p % 64
        axis, half, i = d // 32, (d % 32) // 16, d % 16
        ang = (row if axis == 0 else col) * freqs[i]
        C[p, NCTX:] = np.cos(ang)
        Sn[p, NCTX:] = np.sin(ang) * (-1.0 if half == 0 else 1.0)
    return C, Sn


def _swap_perm():
    p = np.arange(512)
    d = p % 64
    half = (d % 32) // 16
    return p + np.where(half == 0, 16, -16)


_NC_CACHE = {}


def prepare_inputs(inputs):
    f = lambda a: np.ascontiguousarray(np.asarray(a, dtype=np.float32))
    x, c, ctx, c_ctx = f(inputs["x"]), f(inputs["c"]), f(inputs["ctx"]), f(inputs["c_ctx"])
    w_in = f(inputs["w_in"])
    perm = _swap_perm()
    w_in_sw = np.ascontiguousarray(np.concatenate([w_in[:, :, 3584 + perm], w_in[:, :, 4096 + perm]], axis=2))
    ropeC, ropeS = _rope_tables()
    hl = f(inputs["hgrn_lb"])
    lbT = np.ascontiguousarray(hl.reshape(2, 2, 4, 128).transpose(3, 0, 1, 2))
    gnA = np.ascontiguousarray(f(inputs["hgrn_gnorm"]).reshape(2, 4, 128).transpose(2, 0, 1))
    sublnT = np.ascontiguousarray(f(inputs["diff_subln"]).reshape(2, 4, 128).transpose(2, 0, 1))
    wsT = np.ascontiguousarray(f(inputs["mlp_ws"]).transpose(0, 3, 1, 2))
    shared = {
        "w_ada": f(inputs["w_ada"]), "b_ada": f(inputs["b_ada"]), "g_norm1": f(inputs["g_norm1"]),
        "g_norm2": f(inputs["g_norm2"]), "w_in": w_in, "w_in_sw": w_in_sw, "lbT": lbT, "gnA": gnA,
        "vnorm": f(inputs["mlp_vnorm"]), "wsT": wsT, "bs": f(inputs["mlp_bs"]).reshape(2, 512),
        "dlam": f(inputs["diff_lambda"]).reshape(2, 256), "sublnT": sublnT, "w_branch": f(inputs["w_branch"]),
        "w_out": f(inputs["w_out"]), "ffn_wg": f(inputs["ffn_wg"]), "ffn_wu": f(inputs["ffn_wu"]),
        "ffn_wd": f(inputs["ffn_wd"]), "moe_router": f(inputs["moe_router"]), "moe_wg": f(inputs["moe_wg"]),
        "moe_wu": f(inputs["moe_wu"]), "moe_wd": f(inputs["moe_wd"]), "g_final": f(inputs["g_final"]),
        "ropeC": ropeC, "ropeS": ropeS,
    }
    in_maps = []
    for b in range(8):
        cT = np.stack([c[b].reshape(8, 128).T, c_ctx.reshape(8, 128).T], axis=2)
        m = dict(shared)
        m["x"] = x[b]
        m["ctx"] = ctx[b]
        m["cT"] = np.ascontiguousarray(cT)
        in_maps.append(m)
    return in_maps


def kernel(**inputs):
    if "nc" not in _NC_CACHE:
        _NC_CACHE["nc"] = build()
    nc = _NC_CACHE["nc"]
    in_maps = prepare_inputs(inputs)
    res = run_bass_kernel_spmd(nc, in_maps, core_ids=list(range(8)))
    return np.stack([np.asarray(r["out"], dtype=np.float32) for r in res.results], axis=0)
```

```python
import math
import numpy as np
import concourse.bass as bass
import concourse.mybir as mybir
from contextlib import ExitStack
from concourse.bass_utils import run_bass_kernel_spmd

F32 = mybir.dt.float32
BF16 = mybir.dt.bfloat16
AF = mybir.ActivationFunctionType
ALU = mybir.AluOpType
AX = mybir.AxisListType

D = 1024
NLAT = 4096
NCTX = 256
NTOK = NLAT + NCTX
NT = NTOK // 128
EPS = 1e-6
DFF = 2816
DFFE = 3584
NEXP = 8
BLOCKS = [(0, 256)] + [(256 + 512 * i, 512) for i in range(8)]
NCH = NTOK // 64


class Buf:
    __slots__ = ("name", "last_w", "reads")

    def __init__(self, name=""):
        self.name = name
        self.last_w = None
        self.reads = []


class Sched:
    ENG = ("pe", "act", "dve", "pool", "sp")
    NDS = 6

    def __init__(self, nc, stack):
        self.nc = nc
        self.ops = {e: [] for e in self.ENG}
        self.sem = {e: stack.enter_context(nc.semaphore("s_" + e)) for e in self.ENG}
        self.cnt = {e: 0 for e in self.ENG}
        self.seen = {e: {} for e in self.ENG}
        self.dsem = {}
        self.dcnt = {}
        self.drr = {}
        for q in ("sp", "act", "pool"):
            self.dsem[q] = [stack.enter_context(nc.semaphore("d_%s%d" % (q, i))) for i in range(self.NDS)]
            self.dcnt[q] = [0] * self.NDS
            self.drr[q] = 0
        self.out_events = []

    def _deps(self, eng, reads, writes, extra=()):
        need = {}

        def add(ev):
            if ev is None:
                return
            key, sem, val = ev
            if self.seen[eng].get(key, 0) >= val:
                return
            if key not in need or need[key][1] < val:
                need[key] = (sem, val)

        for r in reads:
            add(r.last_w)
        for w in writes:
            add(w.last_w)
            for ev in w.reads:
                add(ev)
        for ev in extra:
            add(ev)
        waits = []
        for key, (sem, val) in need.items():
            self.seen[eng][key] = val
            waits.append((sem, val))
        return waits

    def _commit(self, ev, reads, writes):
        for r in reads:
            r.reads.append(ev)
            if len(r.reads) > 48:
                best = {}
                for e in r.reads:
                    if e[0] not in best or best[e[0]][2] < e[2]:
                        best[e[0]] = e
                r.reads = list(best.values())
        for w in writes:
            w.last_w = ev
            w.reads = []

    def op(self, eng, fns, reads=(), writes=()):
        if callable(fns):
            fns = [fns]
        waits = self._deps(eng, reads, writes)
        self.cnt[eng] += 1
        ev = ("c_" + eng, self.sem[eng], self.cnt[eng])
        self.ops[eng].append((waits, fns, (self.sem[eng], 1)))
        self._commit(ev, reads, writes)
        return ev

    def dma(self, q, out, in_, reads=(), writes=(), is_output=False, **kw):
        i = self.drr[q]
        self.drr[q] = (i + 1) % self.NDS
        sem = self.dsem[q][i]
        key = "d_%s%d" % (q, i)
        prev = self.dcnt[q][i]
        extra = [(key, sem, prev)] if prev > 0 else []
        waits = self._deps(q, reads, writes, extra)
        self.dcnt[q][i] = prev + 16
        ev = (key, sem, prev + 16)
        self.ops[q].append((waits, [lambda e, o=out, a=in_, k=kw: e.dma_start(out=o, in_=a, **k)], (sem, 16)))
        self._commit(ev, reads, writes)
        if is_output:
            self.out_events.append(ev)
        return ev

    def barrier(self):
        allw = {}
        for q in ("sp", "act", "pool"):
            for i in range(self.NDS):
                if self.dcnt[q][i] > 0:
                    allw["d_%s%d" % (q, i)] = (self.dsem[q][i], self.dcnt[q][i])
        for e in self.ENG:
            if self.cnt[e] > 0:
                allw["c_" + e] = (self.sem[e], self.cnt[e])
        for e in self.ENG:
            waits = []
            for key, (sem, val) in allw.items():
                if self.seen[e].get(key, 0) < val:
                    self.seen[e][key] = val
                    waits.append((sem, val))
            self.ops[e].append((waits, [], None))

    def emit(self):
        nc = self.nc
        self.barrier()
        hmap = {"pe": "tensor", "act": "scalar", "dve": "vector", "pool": "gpsimd", "sp": "sync"}
        with nc.Block() as block:
            for e in self.ENG:
                ops = self.ops[e]

                def body(eng, ops=ops):
                    for waits, fns, inc in ops:
                        for sem, val in waits:
                            eng.wait_ge(sem, val)
                        n = len(fns)
                        for j, fn in enumerate(fns):
                            ins = fn(eng)
                            if j == n - 1 and inc is not None:
                                ins.then_inc(inc[0], inc[1])

                getattr(block, hmap[e])(body)
        self.ops = {e: [] for e in self.ENG}


_UID = [0]


def U(name):
    _UID[0] += 1
    return "%s_u%d" % (name, _UID[0])


def AP(t, off, dims):
    return bass.AP(t, off, [list(d) for d in dims])


class Rot:
    def __init__(self, items):
        self.items = items
        self.i = 0

    def next(self):
        it = self.items[self.i]
        self.i = (self.i + 1) % len(self.items)
        return it


class K:
    pass


def build(n_layers=2, debug=None, stop_after=None, small_moe=False):
    nc = bass.Bass("TRN2", target_bir_lowering=False)
    k = K()
    k.nc = nc
    k.debug = debug or []
    dt_in = lambda name, shape: nc.dram_tensor(name, shape, F32, kind="ExternalInput")

    def scratch(name, shape, dt):
        kind = "ExternalOutput" if name in k.debug else "Internal"
        return nc.dram_tensor(name, shape, dt, kind=kind)

    I = {}
    I["x"] = dt_in("x", [NLAT, D])
    I["ctx"] = dt_in("ctx", [NCTX, D])
    I["cT"] = dt_in("cT", [128, 8, 2])
    I["w_ada"] = dt_in("w_ada", [2, D, 6 * D])
    I["b_ada"] = dt_in("b_ada", [2, 6 * D])
    I["g_norm1"] = dt_in("g_norm1", [2, D])
    I["g_norm2"] = dt_in("g_norm2", [2, D])
    I["w_in"] = dt_in("w_in", [2, D, 8192])
    I["w_in_sw"] = dt_in("w_in_sw", [2, D, 1024])
    I["lbT"] = dt_in("lbT", [128, 2, 2, 4])
    I["gnA"] = dt_in("gnA", [128, 2, 4])
    I["vnorm"] = dt_in("vnorm", [2, 512])
    I["wsT"] = dt_in("wsT", [2, 128, 4, 128])
    I["bs"] = dt_in("bs", [2, 512])
    I["dlam"] = dt_in("dlam", [2, 256])
    I["sublnT"] = dt_in("sublnT", [128, 2, 4])
    I["w_branch"] = dt_in("w_branch", [2, 3, 512, D])
    I["w_out"] = dt_in("w_out", [2, D, D])
    I["ffn_wg"] = dt_in("ffn_wg", [1, D, DFF])
    I["ffn_wu"] = dt_in("ffn_wu", [1, D, DFF])
    I["ffn_wd"] = dt_in("ffn_wd", [1, DFF, D])
    I["moe_router"] = dt_in("moe_router", [1, D, NEXP])
    if small_moe:
        I["moe_wg"] = dt_in("moe_wg", [1, 1, 8, 8])
        I["moe_wu"] = dt_in("moe_wu", [1, 1, 8, 8])
        I["moe_wd"] = dt_in("moe_wd", [1, 1, 8, 8])
    else:
        I["moe_wg"] = dt_in("moe_wg", [1, NEXP, D, DFFE])
        I["moe_wu"] = dt_in("moe_wu", [1, NEXP, D, DFFE])
        I["moe_wd"] = dt_in("moe_wd", [1, NEXP, DFFE, D])
    I["g_final"] = dt_in("g_final", [D])
    I["ropeC"] = dt_in("ropeC", [128, NTOK])
    I["ropeS"] = dt_in("ropeS", [128, NTOK])
    k.I = I
    k.out = nc.dram_tensor("out", [NLAT, D], F32, kind="ExternalOutput")
    k.xs = scratch("xs", [NTOK, D], F32)
    k.modrow = scratch("modrow", [2, 6, D], F32)
    k.pAq = scratch("pAq", [512, NTOK], F32)
    k.pAzf = scratch("pAzf", [512, NTOK], F32)
    k.pAzb = scratch("pAzb", [512, NTOK], F32)
    k.pAv = scratch("pAv", [NTOK, 512], BF16)
    k.pAog = scratch("pAog", [512, NTOK], BF16)
    k.pBu = scratch("pBu", [512, NTOK], BF16)
    k.pBv = scratch("pBv", [NTOK, 512], BF16)
    k.pCq = scratch("pCq", [512, NTOK], BF16)
    k.pCk = scratch("pCk", [512, NTOK], BF16)
    k.pCv = scratch("pCv", [NTOK, 512], BF16)
    k.pG = scratch("pG", [3072, NTOK], BF16)
    k.ybr = scratch("ybr", [3, 512, NTOK], BF16)
    k.h2T = scratch("h2T", [D, NTOK], BF16)
    k.B = {n: Buf(n) for n in ["xs", "modrow", "pAq", "pAzf", "pAzb", "pAv", "pAog", "pBu", "pBv", "pCq", "pCk",
                               "pCv", "pG", "ybr0", "ybr1", "ybr2", "h2T", "out"]}

    k.Bxs = [Buf("xs%d" % i) for i in range(NT)]
    with ExitStack() as top:
        S = Sched(nc, top)
        k.S = S
        k.ident = top.enter_context(nc.sbuf_tensor("ident", [128, 128], BF16))
        k.ones = top.enter_context(nc.sbuf_tensor("ones", [128, 128], BF16))
        k.sel0 = top.enter_context(nc.sbuf_tensor("sel0", [128, 128], BF16))
        k.sel1 = top.enter_context(nc.sbuf_tensor("sel1", [128, 128], BF16))
        k.onecol = top.enter_context(nc.sbuf_tensor("onecol", [128, 1], F32))
        k.Bc = Buf("consts")
        phase_consts(k)
        for layer in range(n_layers):
            last = layer == 1
            phases = [("ada", phase_ada), ("n1p", phase_n1p), ("hgrn", phase_hgrn), ("mlp", phase_mlp),
                      ("attn", phase_attn), ("merge", phase_merge), ("ffn", phase_ffn)]
            done = False
            for name, fn in phases:
                fn(k, layer, last)
                if stop_after == (layer, name):
                    done = True
                    break
            if done:
                break
        S.emit()
    return nc


def phase_consts(k):
    nc, S = k.nc, k.S
    with ExitStack() as st:
        tf = st.enter_context(nc.sbuf_tensor(U("c_tf"), [128, 128], F32))
        b = Buf()
        S.op("pool", lambda e: e.memset(tf[:], 0.0), writes=[b])
        S.op("pool", lambda e: e.affine_select(out=tf[:], in_=tf[:], pattern=[[1, 128]], compare_op=ALU.not_equal,
                                               fill=1.0, base=0, channel_multiplier=-1), reads=[b], writes=[b])
        S.op("dve", lambda e: e.tensor_copy(out=k.ident[:], in_=tf[:]), reads=[b], writes=[k.Bc])
        S.op("dve", lambda e: e.memset(k.ones[:], 1.0), writes=[k.Bc])
        S.op("dve", lambda e: e.memset(k.sel0[:], 0.0), writes=[k.Bc])
        S.op("dve", lambda e: e.memset(k.sel0[0:64, :], 1.0), writes=[k.Bc])
        S.op("dve", lambda e: e.memset(k.sel1[:], 0.0), writes=[k.Bc])
        S.op("dve", lambda e: e.memset(k.sel1[64:128, :], 1.0), writes=[k.Bc])
        S.op("dve", lambda e: e.memset(k.onecol[:], 1.0), writes=[k.Bc])
        S.dma("sp", k.xs.ap()[0:NCTX, :], k.I["ctx"].ap(), writes=[k.B["xs"]])
        for i in range(4):
            S.dma("sp", k.xs.ap()[NCTX + i * 1024:NCTX + (i + 1) * 1024, :], k.I["x"].ap()[i * 1024:(i + 1) * 1024, :],
                  writes=[k.B["xs"]])
        S.emit()


def phase_ada(k, layer, last):
    nc, S, I = k.nc, k.S, k.I
    with ExitStack() as st:
        sb = lambda n, s, d: st.enter_context(nc.sbuf_tensor(U("ad_" + n), s, d))
        cT = sb("cT", [128, 8, 2], F32)
        cs = sb("cs", [128, 8, 2], BF16)
        wts = [(sb("w%d" % i, [128, 8, 1024], BF16), Buf()) for i in range(2)]
        brow = sb("brow", [1, 6 * D], F32)
        g1 = sb("g1", [1, D], F32)
        g2 = sb("g2", [1, D], F32)
        res = [(sb("res%d" % i, [1, D], F32), Buf()) for i in range(2)]
        pss = [(st.enter_context(nc.psum_tensor(U("ad_ps%d" % i), [128, 512], F32)), Buf()) for i in range(2)]
        bc, bcs, bb = Buf(), Buf(), Buf()
        S.dma("sp", cT[:], I["cT"].ap(), writes=[bc])
        S.dma("sp", brow[:], I["b_ada"].ap()[layer:layer + 1, :], writes=[bb])
        S.dma("sp", g1[:], I["g_norm1"].ap()[layer:layer + 1, :], writes=[bb])
        S.dma("sp", g2[:], I["g_norm2"].ap()[layer:layer + 1, :], writes=[bb])
        S.op("act", lambda e: e.activation(out=cs[:], in_=cT[:], func=AF.Silu), reads=[bc], writes=[bcs])
        wsrc = I["w_ada"].ap()[layer].rearrange("(k p) n -> p k n", p=128)
        ri = 0
        for j in range(6):
            w, wb = wts[j % 2]
            S.dma("pool", w[:], wsrc[:, :, j * 1024:(j + 1) * 1024], writes=[wb])
            for which in range(2):
                r, rb = res[ri % 2]
                ri += 1
                for half in range(2):
                    ps, pb = pss[half]
                    S.op("pe", [lambda e, kk=kk, ps=ps, w=w, half=half, which=which: e.matmul(
                        ps[0:1, :], lhsT=cs[:, kk, which:which + 1], rhs=w[:, kk, half * 512:(half + 1) * 512],
                        start=(kk == 0), stop=(kk == 7)) for kk in range(8)], reads=[bcs, wb], writes=[pb])
                    S.op("dve", lambda e, ps=ps, r=r, half=half, j=j: e.tensor_tensor(
                        out=r[0:1, half * 512:(half + 1) * 512], in0=ps[0:1, :],
                        in1=brow[0:1, j * 1024 + half * 512:j * 1024 + (half + 1) * 512], op=ALU.add),
                        reads=[pb, bb], writes=[rb])
                if j in (1, 4):
                    g = g1 if j == 1 else g2
                    S.op("dve", lambda e, r=r, g=g: e.scalar_tensor_tensor(out=r[:], in0=r[:], scalar=1.0, in1=g[:],
                                                                            op0=ALU.add, op1=ALU.mult),
                         reads=[rb, bb], writes=[rb])
                S.dma("sp", k.modrow.ap()[which, j:j + 1, :], r[:], reads=[rb], writes=[k.B["modrow"]])
        S.emit()


def load_rep(k, S, tile, buf, which, j):
    S.dma("sp", tile[:], AP(k.modrow, (which * 6 + j) * D, [[0, 128], [1, D]]), reads=[k.B["modrow"]], writes=[buf])


def rms_mod(S, xt, xb, A, Ab, Bt, Bb, hb, hbb, ss, ssb, junk, junkb, D_=D):
    S.op("act", lambda e: e.activation(out=junk[:], in_=xt[:], func=AF.Square, accum_out=ss[:, 0:1]), reads=[xb],
         writes=[junkb, ssb])
    S.op("act", lambda e: e.activation(out=ss[:, 1:2], in_=ss[:, 0:1], func=AF.Ln, scale=1.0 / D_, bias=EPS),
         reads=[ssb], writes=[ssb])
    S.op("act", lambda e: e.activation(out=ss[:, 2:3], in_=ss[:, 1:2], func=AF.Exp, scale=-0.5), reads=[ssb],
         writes=[ssb])
    S.op("dve", lambda e: e.scalar_tensor_tensor(out=junk[:], in0=xt[:], scalar=ss[:, 2:3], in1=A[:], op0=ALU.mult,
                                                 op1=ALU.mult), reads=[xb, ssb, Ab], writes=[junkb])
    if Bt is None:
        return
    S.op("dve", lambda e: e.tensor_tensor(out=hb[:], in0=junk[:], in1=Bt[:], op=ALU.add), reads=[junkb, Bb],
         writes=[hbb])


def phase_n1p(k, layer, last):
    nc, S, I = k.nc, k.S, k.I
    with ExitStack() as st0:
      hT = st0.enter_context(nc.sbuf_tensor(U("np_hT"), [128, 8, NTOK], BF16))
      hTb = Buf()
      with ExitStack() as st:
        sb = lambda n, s, d: st.enter_context(nc.sbuf_tensor(U("np_" + n), s, d))
        A = [sb("A%d" % w, [128, D], F32) for w in range(2)]
        Bm = [sb("B%d" % w, [128, D], F32) for w in range(2)]
        mb = Buf()
        for w in range(2):
            load_rep(k, S, A[w], mb, w, 1)
            load_rep(k, S, Bm[w], mb, w, 0)
        xts = Rot([(sb("xt%d" % i, [128, D], F32), Buf()) for i in range(4)])
        junks = Rot([(sb("junk%d" % i, [128, D], F32), Buf()) for i in range(4)])
        hbs = Rot([(sb("hb%d" % i, [128, D], BF16), Buf()) for i in range(4)])
        sss = Rot([(sb("ss%d" % i, [128, 4], F32), Buf()) for i in range(4)])
        pts = Rot([(st.enter_context(nc.psum_tensor(U("np_pt%d" % i), [128, 512], BF16)), Buf()) for i in range(4)])
        for i in range(NT):
            w = 1 if i < 2 else 0
            xt, xb = xts.next()
            junk, jb = junks.next()
            hb, hbb = hbs.next()
            ss, ssb = sss.next()
            S.dma("sp", xt[:], k.xs.ap()[i * 128:(i + 1) * 128, :], reads=[k.B["xs"]], writes=[xb])
            rms_mod(S, xt, xb, A[w], mb, Bm[w], mb, hb, hbb, ss, ssb, junk, jb)
            for half in range(2):
                pt, ptb = pts.next()
                S.op("pe", [lambda e, pt=pt, hb=hb, j=j, half=half: e.transpose(
                    out=pt[:, j * 128:(j + 1) * 128], in_=hb[:, (half * 4 + j) * 128:(half * 4 + j + 1) * 128],
                    identity=k.ident[:]) for j in range(4)], reads=[hbb, k.Bc], writes=[ptb])
                eng = "act" if half == 0 else "dve"
                outap = hT[:, half * 4:half * 4 + 4, i * 128:(i + 1) * 128]
                inap = AP(pt, 0, [[512, 128], [128, 4], [1, 128]])
                if eng == "act":
                    S.op("act", lambda e, o=outap, a=inap: e.activation(out=o, in_=a, func=AF.Copy), reads=[ptb],
                         writes=[hTb])
                else:
                    S.op("dve", lambda e, o=outap, a=inap: e.tensor_copy(out=o, in_=a), reads=[ptb], writes=[hTb])

        S.emit()
      with ExitStack() as st:
        sb = lambda n, s, d: st.enter_context(nc.sbuf_tensor(U("nq_" + n), s, d))
        junks = Rot([(sb("junk%d" % i, [128, D], F32), Buf()) for i in range(2)])
        sss = Rot([(sb("ss%d" % i, [128, 4], F32), Buf()) for i in range(2)])
        ropeC = sb("ropeC", [128, NTOK], F32)
        ropeS = sb("ropeS", [128, NTOK], F32)
        rb = Buf()
        S.dma("sp", ropeC[:], I["ropeC"].ap(), writes=[rb])
        S.dma("sp", ropeS[:], I["ropeS"].ap(), writes=[rb])
        vn = sb("vn", [128, 512], F32)
        S.dma("sp", vn[:], AP(I["vnorm"], layer * 512, [[0, 128], [1, 512]]), writes=[rb])
        wts = Rot([(sb("w%d" % i, [128, 8, 1024], BF16), Buf()) for i in range(2)])
        wsw = sb("wsw", [128, 8, 512], BF16)
        wswb = Buf()
        stg32 = Rot([(sb("s32_%d" % i, [128, NTOK], F32), Buf()) for i in range(2)])
        pps = Rot([(st.enter_context(nc.psum_tensor(U("nq_pp%d" % i), [128, 512], F32)), Buf()) for i in range(4)])
        wsrc = I["w_in"].ap()[layer].rearrange("(k p) n -> p k n", p=128)
        wswsrc = I["w_in_sw"].ap()[layer].rearrange("(k p) n -> p k n", p=128)

        def fm_mm(ps, pb, w, wb, c0, t0, W):
            S.op("pe", [lambda e, kk=kk: e.matmul(ps[:, 0:W], lhsT=w[:, kk, c0:c0 + 128], rhs=hT[:, kk, t0:t0 + W],
                                                   start=(kk == 0), stop=(kk == 7)) for kk in range(8)],
                 reads=[wb, hTb], writes=[pb])

        def fm_job(c_in, ncols, dst, dstbuf, func, out_dt):
            for g0 in range(0, ncols, 1024):
                gw = min(1024, ncols - g0)
                w, wb = wts.next()
                S.dma("pool", w[:, :, 0:gw], wsrc[:, :, c_in + g0:c_in + g0 + gw], writes=[wb])
                for cc in range(gw // 128):
                    stg, sgb = stg32.next()
                    if out_dt == BF16:
                        stgv = stg[:].bitcast(BF16)[:, 0:NTOK]
                    else:
                        stgv = stg[:]
                    for (t0, W) in BLOCKS:
                        ps, pb = pps.next()
                        fm_mm(ps, pb, w, wb, cc * 128, t0, W)
                        if func is None:
                            S.op("dve", lambda e, ps=ps, o=stgv[:, t0:t0 + W], W=W: e.tensor_copy(out=o, in_=ps[:, 0:W]),
                                 reads=[pb], writes=[sgb])
                        else:
                            S.op("act", lambda e, ps=ps, o=stgv[:, t0:t0 + W], W=W: e.activation(out=o, in_=ps[:, 0:W],
                                                                                             func=func),
                                 reads=[pb], writes=[sgb])
                    r0 = g0 + cc * 128
                    S.dma("sp", dst[r0:r0 + 128, :], stgv, reads=[sgb], writes=[dstbuf])

        def tm_job(c_in, dst, dstbuf, mode):
            w, wb = wts.next()
            S.dma("pool", w[:, :, 0:512], wsrc[:, :, c_in:c_in + 512], writes=[wb])
            stg, sgb = stg32.next()
            stgv = AP(stg, 0, [[NTOK, 128], [1, NTOK]]).bitcast(BF16)
            for i in range(NT):
                ps, pb = pps.next()
                S.op("pe", [lambda e, kk=kk, ps=ps, i=i: e.matmul(ps[:], lhsT=hT[:, kk, i * 128:(i + 1) * 128],
                                                                  rhs=w[:, kk, 0:512], start=(kk == 0), stop=(kk == 7))
                            for kk in range(8)], reads=[wb, hTb], writes=[pb])
                o = stgv[:, (i % 16) * 512:(i % 16 + 1) * 512]
                if mode == "copy":
                    S.op("act", lambda e, ps=ps, o=o: e.activation(out=o, in_=ps[:], func=AF.Copy), reads=[pb],
                         writes=[sgb])
                else:
                    gl, glb = junks.next()
                    ss, ssb = sss.next()
                    S.op("act", lambda e, ps=ps, gl=gl: e.activation(out=gl[:, 0:512], in_=ps[:], func=AF.Gelu_apprx_tanh),
                         reads=[pb], writes=[glb])
                    S.op("act", lambda e, gl=gl, ss=ss: e.activation(out=gl[:, 512:1024], in_=gl[:, 0:512], func=AF.Square,
                                                                     accum_out=ss[:, 0:1]), reads=[glb], writes=[glb, ssb])
                    S.op("act", lambda e, ss=ss: e.activation(out=ss[:, 1:2], in_=ss[:, 0:1], func=AF.Ln, scale=1.0 / 512,
                                                              bias=EPS), reads=[ssb], writes=[ssb])
                    S.op("act", lambda e, ss=ss: e.activation(out=ss[:, 2:3], in_=ss[:, 1:2], func=AF.Exp, scale=-0.5),
                         reads=[ssb], writes=[ssb])
                    S.op("dve", lambda e, gl=gl, ss=ss, o=o: e.scalar_tensor_tensor(
                        out=o, in0=gl[:, 0:512], scalar=ss[:, 2:3], in1=vn[:], op0=ALU.mult, op1=ALU.mult),
                        reads=[glb, ssb, rb], writes=[sgb])
                if i % 16 == 15 or i == NT - 1:
                    i0 = (i // 16) * 16
                    n = i - i0 + 1
                    S.dma("sp", dst.ap()[i0 * 128:(i + 1) * 128, :].rearrange("(n p) c -> p n c", p=128),
                          AP(stg, 0, [[NTOK, 128], [1, NTOK]]).bitcast(BF16)[:, 0:n * 512].rearrange("p (n c) -> p n c", c=512),
                          reads=[sgb], writes=[dstbuf])
                    if i != NT - 1:
                        stg, sgb = stg32.next()
                        stgv = AP(stg, 0, [[NTOK, 128], [1, NTOK]]).bitcast(BF16)

        def rope_job(c_in, sw0, dst, dstbuf):
            w, wb = wts.next()
            S.dma("pool", w[:, :, 0:512], wsrc[:, :, c_in:c_in + 512], writes=[wb])
            S.dma("pool", wsw[:], wswsrc[:, :, sw0:sw0 + 512], writes=[wswb])
            t1s = Rot([(junks.items[0][0], junks.items[0][1]), (junks.items[1][0], junks.items[1][1])])
            for cc in range(4):
                stg, sgb = stg32.next()
                stgv = stg[:].bitcast(BF16)[:, 0:NTOK]
                for (t0, W) in BLOCKS:
                    ps, pb = pps.next()
                    ps2, pb2 = pps.next()
                    fm_mm(ps, pb, w, wb, cc * 128, t0, W)
                    fm_mm(ps2, pb2, wsw, wswb, cc * 128, t0, W)
                    t1, t1b = t1s.next()
                    S.op("dve", lambda e, ps=ps, t1=t1, t0=t0, W=W: e.tensor_tensor(
                        out=t1[:, 0:W], in0=ps[:, 0:W], in1=ropeC[:, t0:t0 + W], op=ALU.mult), reads=[pb, rb], writes=[t1b])
                    S.op("dve", lambda e, ps2=ps2, t1=t1, t0=t0, W=W: e.tensor_tensor(
                        out=t1[:, 512:512 + W], in0=ps2[:, 0:W], in1=ropeS[:, t0:t0 + W], op=ALU.mult), reads=[pb2, rb],
                        writes=[t1b])
                    S.op("dve", lambda e, t1=t1, o=stgv[:, t0:t0 + W], W=W: e.tensor_tensor(
                        out=o, in0=t1[:, 0:W], in1=t1[:, 512:512 + W], op=ALU.add), reads=[t1b], writes=[sgb])
                S.dma("sp", dst[cc * 128:(cc + 1) * 128, :], stgv, reads=[sgb], writes=[dstbuf])

        B = k.B
        fm_job(0, 512, k.pAq.ap(), B["pAq"], None, F32)
        fm_job(512, 512, k.pAzf.ap(), B["pAzf"], None, F32)
        fm_job(1024, 512, k.pAzb.ap(), B["pAzb"], None, F32)
        tm_job(1536, k.pAv, B["pAv"], "copy")
        fm_job(2048, 512, k.pAog.ap(), B["pAog"], AF.Silu, BF16)
        fm_job(2560, 512, k.pBu.ap(), B["pBu"], AF.Gelu_apprx_tanh, BF16)
        tm_job(3072, k.pBv, B["pBv"], "gelu_rms")
        rope_job(3584, 0, k.pCq.ap(), B["pCq"])
        rope_job(4096, 512, k.pCk.ap(), B["pCk"])
        tm_job(4608, k.pCv, B["pCv"], "copy")
        fm_job(5120, 3072, k.pG.ap(), B["pG"], AF.Sigmoid, BF16)
        S.emit()


def head_readout(k, S, st, prefix, src, srcb, t0, W, scale_col, scb, mul_tile, mulb, dst, dstb, pss, sq_rot, tmp_rot):
    sq, sqb = sq_rot.next()
    tmp, tb = tmp_rot.next()
    ps, pb = pss.next()
    S.op("act", lambda e: e.activation(out=sq[:, 0:W], in_=src, func=AF.Square), reads=[srcb], writes=[sqb])
    S.op("pe", lambda e: e.matmul(ps[:, 0:W], lhsT=k.ones[:], rhs=sq[:, 0:W], start=True, stop=True), reads=[sqb, k.Bc],
         writes=[pb])
    S.op("act", lambda e: e.activation(out=tmp[:, 0:W], in_=ps[:, 0:W], func=AF.Ln, scale=1.0 / 128, bias=EPS),
         reads=[pb], writes=[tb])
    S.op("act", lambda e: e.activation(out=tmp[:, 0:W], in_=tmp[:, 0:W], func=AF.Exp, scale=-0.5), reads=[tb],
         writes=[tb])
    if mul_tile is None:
        S.op("dve", lambda e: e.scalar_tensor_tensor(out=dst, in0=src, scalar=scale_col, in1=tmp[:, 0:W],
                                                     op0=ALU.mult, op1=ALU.mult), reads=[srcb, scb, tb], writes=[dstb])
    else:
        S.op("dve", lambda e: e.scalar_tensor_tensor(out=tmp[:, 0:W], in0=src, scalar=scale_col, in1=tmp[:, 0:W],
                                                     op0=ALU.mult, op1=ALU.mult), reads=[srcb, scb, tb], writes=[tb])
        S.op("dve", lambda e: e.tensor_tensor(out=dst, in0=tmp[:, 0:W], in1=mul_tile, op=ALU.mult), reads=[tb, mulb],
             writes=[dstb])


def phase_hgrn(k, layer, last):
    nc, S, I = k.nc, k.S, k.I
    with ExitStack() as st:
        sb = lambda n, s, d: st.enter_context(nc.sbuf_tensor(U("hg_" + n), s, d))
        T1 = sb("T1", [128, NTOK], F32)
        T2 = sb("T2", [128, NTOK], F32)
        Gp = sb("Gp", [128, NTOK + 1], F32)
        T4 = sb("T4", [128, NTOK], F32)
        q1 = sb("q1", [128, NTOK], BF16)
        k1 = sb("k1", [128, NTOK], BF16)
        k1z = sb("k1z", [128, NTOK], BF16)
        q2 = sb("q2", [128, NTOK], BF16)
        k2f = sb("k2f", [128, NTOK], BF16)
        k2T = sb("k2T", [64, NCH, 128], BF16)
        vS = sb("vS", [64, NCH, 128], BF16)
        oacc = sb("oacc", [128, NTOK], F32)
        dec = sb("dec", [128, NCH], F32)
        cqk = sb("cqk", [128, 2, NCH], F32)
        S32p = [(sb("S32_%d" % i, [128, 128], F32), Buf()) for i in range(2)]
        SbA = sb("SbA", [128, NCH * 128], BF16)
        slotb = [Buf() for _ in range(NCH + 1)]
        lbt = sb("lbt", [128, 16], F32)
        lbe = sb("lbe", [128, 16], F32)
        lbv = sb("lbv", [128, 8, 2], F32)
        gn = sb("gn", [128, 8], F32)
        maskF = sb("maskF", [64, 64], F32)
        maskB = sb("maskB", [64, 64], F32)
        attTs = Rot([(sb("attT%d" % i, [64, 512], BF16), Buf()) for i in range(3)])
        sqs = Rot([(sb("sq%d" % i, [128, 512], BF16), Buf()) for i in range(2)])
        tmps = Rot([(sb("tmp%d" % i, [128, 512], F32), Buf()) for i in range(2)])
        b = {n: Buf(n) for n in "T1 T2 Gp T4 q1 k1 k1z q2 k2f k2T vS oacc dec S32 Sb lb gn mask cqk".split()}
        pa = Rot([(st.enter_context(nc.psum_tensor(U("hg_pa%d" % i), [128, 512], F32)), Buf()) for i in range(2)])
        po = Rot([(st.enter_context(nc.psum_tensor(U("hg_po%d" % i), [128, 512], F32)), Buf()) for i in range(2)])
        psS = Rot([(st.enter_context(nc.psum_tensor(U("hg_ps%d" % i), [128, 512], F32)), Buf()) for i in range(2)])
        ptr = Rot([(st.enter_context(nc.psum_tensor(U("hg_pt%d" % i), [128, 512], BF16)), Buf()) for i in range(2)])

        S.dma("sp", lbt[:], I["lbT"].ap().rearrange("p a b c -> p (a b c)"), writes=[b["lb"]])
        S.dma("sp", gn[:], I["gnA"].ap().rearrange("p a b -> p (a b)"), writes=[b["gn"]])
        S.op("act", lambda e: e.activation(out=lbe[:], in_=lbt[:], func=AF.Exp), reads=[b["lb"]], writes=[b["lb"]])
        for d_ in range(2):
            e0 = lbe[:, d_ * 8:d_ * 8 + 4]
            e1 = lbe[:, d_ * 8 + 4:d_ * 8 + 8]
            tot = lbt[:, d_ * 8:d_ * 8 + 4]
            S.op("dve", lambda e, e0=e0, e1=e1, tot=tot: e.tensor_tensor(out=tot, in0=e0, in1=e1, op=ALU.add),
                 reads=[b["lb"]], writes=[b["lb"]])
            S.op("dve", lambda e, tot=tot: e.reciprocal(out=tot, in_=tot), reads=[b["lb"]], writes=[b["lb"]])
            num = lbt[:, d_ * 8 + 4:d_ * 8 + 8]
            if layer == 0:
                S.op("dve", lambda e, num=num, e0=e0: e.tensor_tensor(out=num, in0=e0, in1=e0, op=ALU.subtract),
                     reads=[b["lb"]], writes=[b["lb"]])
            else:
                S.op("dve", lambda e, num=num, e1=e1: e.tensor_copy(out=num, in_=e1), reads=[b["lb"]], writes=[b["lb"]])
            lbcol = AP(lbv, d_ * 8, [[16, 128], [2, 4]])
            omcol = AP(lbv, d_ * 8 + 1, [[16, 128], [2, 4]])
            S.op("dve", lambda e, lbcol=lbcol, num=num, tot=tot: e.tensor_tensor(out=lbcol, in0=num, in1=tot, op=ALU.mult),
                 reads=[b["lb"]], writes=[b["lb"]])
            S.op("dve", lambda e, lbcol=lbcol, omcol=omcol: e.tensor_scalar(out=omcol, in0=lbcol, scalar1=-1.0, scalar2=1.0,
                                                                            op0=ALU.mult, op1=ALU.add),
                 reads=[b["lb"]], writes=[b["lb"]])
        S.op("pool", lambda e: e.memset(maskF[:], 1.0), writes=[b["mask"]])
        S.op("pool", lambda e: e.affine_select(out=maskF[:], in_=maskF[:], pattern=[[1, 64]], compare_op=ALU.is_ge,
                                               fill=0.0, base=0, channel_multiplier=-1), reads=[b["mask"]],
             writes=[b["mask"]])
        S.op("pool", lambda e: e.memset(maskB[:], 1.0), writes=[b["mask"]])
        S.op("pool", lambda e: e.affine_select(out=maskB[:], in_=maskB[:], pattern=[[-1, 64]], compare_op=ALU.is_ge,
                                               fill=0.0, base=0, channel_multiplier=1), reads=[b["mask"]],
             writes=[b["mask"]])
        S.op("dve", lambda e: e.memset(Gp[:, 0:1], 0.0), writes=[b["Gp"]])

        def view(t, off, W=NTOK + 1):
            return AP(t, off, [[W, 128], [64, NCH], [1, 64]])

        def anchor(off):
            return AP(Gp, off, [[NTOK + 1, 128], [64, NCH], [0, 64]])

        def v3(t):
            return AP(t, 0, [[NTOK, 128], [64, NCH], [1, 64]])

        for h in range(4):
            S.dma("sp", T4[:], k.pAq.ap()[h * 128:(h + 1) * 128, :], reads=[k.B["pAq"]], writes=[b["T4"]])
            S.dma("sp", vS[:], k.pAv.ap().rearrange("(c s) d -> s c d", s=64)[:, :, h * 128:(h + 1) * 128],
                  reads=[k.B["pAv"]], writes=[b["vS"]])
            for d_ in range(2):
                sg = 1.0 if d_ == 0 else -1.0
                zsrc = k.pAzf if d_ == 0 else k.pAzb
                zb_ = k.B["pAzf"] if d_ == 0 else k.B["pAzb"]
                lbc = lbv[:, d_ * 4 + h, 0:1]
                omc = lbv[:, d_ * 4 + h, 1:2]
                S.dma("sp", T1[:], zsrc.ap()[h * 128:(h + 1) * 128, :], reads=[zb_], writes=[b["T1"]])
                S.op("act", lambda e: e.activation(out=T1[:], in_=T1[:], func=AF.Sigmoid), reads=[b["T1"]], writes=[b["T1"]])
                S.op("dve", lambda e, lbc=lbc, omc=omc: e.tensor_scalar(out=T1[:], in0=T1[:], scalar1=omc, scalar2=lbc,
                                                                        op0=ALU.mult, op1=ALU.add),
                     reads=[b["T1"], b["lb"]], writes=[b["T1"]])
                S.op("dve", lambda e: e.tensor_scalar(out=T2[:], in0=T1[:], scalar1=-1.0, scalar2=1.0, op0=ALU.mult,
                                                      op1=ALU.add), reads=[b["T1"]], writes=[b["T2"]])
                S.op("act", lambda e: e.activation(out=T1[:], in_=T1[:], func=AF.Ln), reads=[b["T1"]], writes=[b["T1"]])
                S.op("dve", lambda e: e.tensor_tensor_scan(out=Gp[:, 1:NTOK + 1],
                                                           data0=AP(k.onecol, 0, [[1, 128], [0, NTOK]]), data1=T1[:],
                                                           initial=0.0, op0=ALU.mult, op1=ALU.add),
                     reads=[b["T1"], k.Bc], writes=[b["Gp"]])
                eoff = 1 if d_ == 0 else 0
                E = view(Gp, eoff)
                a_mid = anchor(32)
                a_q2 = anchor(0) if d_ == 0 else anchor(64)
                a_k2 = anchor(64) if d_ == 0 else anchor(0)

                def prep(anch, scale, src, srcb, dst, dstb, E=E):
                    S.op("dve", lambda e: e.tensor_tensor(out=v3(T1), in0=E, in1=anch, op=ALU.subtract), reads=[b["Gp"]],
                         writes=[b["T1"]])
                    S.op("act", lambda e: e.activation(out=T1[:], in_=T1[:], func=AF.Exp, scale=scale), reads=[b["T1"]],
                         writes=[b["T1"]])
                    S.op("dve", lambda e: e.tensor_tensor(out=dst[:], in0=src[:], in1=T1[:], op=ALU.mult),
                         reads=[b["T1"], srcb], writes=[dstb])

                prep(a_mid, sg, T4, b["T4"], q1, b["q1"])
                prep(a_mid, -sg, T2, b["T2"], k1, b["k1"])
                oq = 0 if d_ == 0 else 64
                ok_ = 64 if d_ == 0 else 0
                gmid = AP(Gp, 32, [[NTOK + 1, 128], [64, NCH]])
                S.op("dve", lambda e, oq=oq: e.tensor_tensor(out=cqk[:, 0, :], in0=gmid, in1=AP(Gp, oq, [[NTOK + 1, 128], [64, NCH]]),
                                                             op=ALU.subtract), reads=[b["Gp"]], writes=[b["cqk"]])
                S.op("dve", lambda e, ok_=ok_: e.tensor_tensor(out=cqk[:, 1, :], in0=gmid, in1=AP(Gp, ok_, [[NTOK + 1, 128], [64, NCH]]),
                                                               op=ALU.subtract), reads=[b["Gp"]], writes=[b["cqk"]])
                S.op("act", lambda e, sg=sg: e.activation(out=cqk[:, 0, :], in_=cqk[:, 0, :], func=AF.Exp, scale=sg),
                     reads=[b["cqk"]], writes=[b["cqk"]])
                S.op("act", lambda e, sg=sg: e.activation(out=cqk[:, 1, :], in_=cqk[:, 1, :], func=AF.Exp, scale=-sg),
                     reads=[b["cqk"]], writes=[b["cqk"]])

                def v3b(t):
                    return AP(t, 0, [[NTOK, 128], [64, NCH], [1, 64]])

                S.op("dve", lambda e: e.tensor_tensor(out=v3b(q2), in0=v3b(q1), in1=AP(cqk, 0, [[2 * NCH, 128], [1, NCH], [0, 64]]),
                                                      op=ALU.mult), reads=[b["q1"], b["cqk"]], writes=[b["q2"]])
                S.op("dve", lambda e: e.tensor_tensor(out=v3b(k2f), in0=v3b(k1), in1=AP(cqk, NCH, [[2 * NCH, 128], [1, NCH], [0, 64]]),
                                                      op=ALU.mult), reads=[b["k1"], b["cqk"]], writes=[b["k2f"]])
                zoff, koff = (32, 0) if d_ == 0 else (0, 32)
                zv = AP(k1z, zoff, [[NTOK, 128], [64, NCH], [1, 32]])
                kv_o = AP(k1z, koff, [[NTOK, 128], [64, NCH], [1, 32]])
                kv_i = AP(k1, koff, [[NTOK, 128], [64, NCH], [1, 32]])
                S.op("pool", lambda e, zv=zv: e.memset(zv, 0.0), writes=[b["k1z"]])
                S.op("pool", lambda e, kv_o=kv_o, kv_i=kv_i: e.tensor_copy(out=kv_o, in_=kv_i), reads=[b["k1"]],
                     writes=[b["k1z"]])
                S.op("dve", lambda e: e.tensor_tensor(out=dec[:], in0=AP(Gp, 64, [[NTOK + 1, 128], [64, NCH]]),
                                                      in1=AP(Gp, 0, [[NTOK + 1, 128], [64, NCH]]), op=ALU.subtract),
                     reads=[b["Gp"]], writes=[b["dec"]])
                S.op("act", lambda e: e.activation(out=dec[:], in_=dec[:], func=AF.Exp), reads=[b["dec"]],
                     writes=[b["dec"]])
                for c4 in range(NCH // 4):
                    pt, ptb = ptr.next()
                    S.op("pe", [lambda e, pt=pt, j=j, c4=c4: e.transpose(
                        out=pt[0:64, j * 128:(j + 1) * 128], in_=k2f[:, (c4 * 4 + j) * 64:(c4 * 4 + j + 1) * 64],
                        identity=k.ident[:]) for j in range(4)], reads=[b["k2f"], k.Bc], writes=[ptb])
                    o = k2T[:, c4 * 4:c4 * 4 + 4, :]
                    a = AP(pt, 0, [[512, 64], [128, 4], [1, 128]])
                    if c4 % 2 == 0:
                        S.op("act", lambda e, o=o, a=a: e.activation(out=o, in_=a, func=AF.Copy), reads=[ptb],
                             writes=[b["k2T"]])
                    else:
                        S.op("dve", lambda e, o=o, a=a: e.tensor_copy(out=o, in_=a), reads=[ptb], writes=[b["k2T"]])
                order = list(range(NCH)) if d_ == 0 else [3, 2, 1, 0] + list(range(NCH - 1, 3, -1))
                mask = maskF if d_ == 0 else maskB
                S.op("dve", lambda e: e.memset(S32p[0][0][:], 0.0), writes=[S32p[0][1]])
                S.op("dve", lambda e: e.memset(SbA[:, 0:128], 0.0), writes=[slotb[0]])

                granges = [(0, 4)] + [(4 + 8 * g, 8) for g in range(8)]
                if d_ == 0:
                    gorder = granges
                else:
                    gorder = [granges[0]] + granges[:0:-1]
                pos_of = {c: i for i, c in enumerate(order)}

                def att_group(g0, n, mask=mask, d_=d_):
                    pA, pAb = pa.next()
                    safe, unsafe = (32, 0) if d_ == 0 else (0, 32)
                    fns = []
                    for s_ in range(n):
                        cs_ = (g0 + s_) * 64
                        fns.append(lambda e, cs_=cs_, s_=s_: e.matmul(pA[0:64, s_ * 64 + safe:s_ * 64 + safe + 32],
                                                                      lhsT=k1[:, cs_:cs_ + 64],
                                                                      rhs=q1[:, cs_ + safe:cs_ + safe + 32], start=True, stop=True))
                        fns.append(lambda e, cs_=cs_, s_=s_: e.matmul(pA[0:64, s_ * 64 + unsafe:s_ * 64 + unsafe + 32],
                                                                      lhsT=k1z[:, cs_:cs_ + 64],
                                                                      rhs=q1[:, cs_ + unsafe:cs_ + unsafe + 32], start=True,
                                                                      stop=True))
                    S.op("pe", fns, reads=[b["k1"], b["k1z"], b["q1"]], writes=[pAb])
                    aT, aTb = attTs.next()
                    S.op("dve", lambda e: e.tensor_tensor(out=AP(aT, 0, [[512, 64], [64, n], [1, 64]]),
                                                          in0=AP(pA, 0, [[512, 64], [64, n], [1, 64]]),
                                                          in1=AP(mask, 0, [[64, 64], [0, n], [1, 64]]), op=ALU.mult),
                         reads=[pAb, b["mask"]], writes=[aTb])
                    return aT, aTb

                def state_step(i, c):
                    pS, pSb = psS.next()
                    src, srcb = S32p[i % 2]
                    dst, dstb = S32p[(i + 1) % 2]
                    S.op("pe", lambda e: e.matmul(pS[:, 0:128], lhsT=k2T[:, c, :], rhs=vS[:, c, :], start=True, stop=True),
                         reads=[b["k2T"], b["vS"]], writes=[pSb])
                    S.op("dve", lambda e: e.scalar_tensor_tensor(out=dst[:], in0=src[:], scalar=dec[:, c:c + 1],
                                                                 in1=pS[:, 0:128], op0=ALU.mult, op1=ALU.add),
                         reads=[pSb, srcb, b["dec"]], writes=[dstb])
                    S.op("act", lambda e: e.activation(out=SbA[:, (i + 1) * 128:(i + 2) * 128], in_=dst[:], func=AF.Copy),
                         reads=[dstb], writes=[slotb[i + 1]])

                def out_group(g0, n, aT, aTb, d_=d_):
                    pO, pOb = po.next()
                    fns = []
                    rd = [b["vS"], aTb, b["q2"]]
                    for s_ in range(n):
                        c = g0 + s_
                        i = pos_of[c]
                        cs_ = c * 64
                        rd.append(slotb[i])
                        fns.append(lambda e, c=c, s_=s_: e.matmul(pO[:, s_ * 64:(s_ + 1) * 64], lhsT=vS[:, c, :],
                                                                  rhs=aT[:, s_ * 64:(s_ + 1) * 64], start=True, stop=False))
                        fns.append(lambda e, i=i, s_=s_, cs_=cs_: e.matmul(pO[:, s_ * 64:(s_ + 1) * 64],
                                                                           lhsT=SbA[:, i * 128:(i + 1) * 128],
                                                                           rhs=q2[:, cs_:cs_ + 64], start=False, stop=True))
                    S.op("pe", fns, reads=rd, writes=[pOb])
                    c0_, c1_ = g0 * 64, (g0 + n) * 64
                    if d_ == 0:
                        S.op("act", lambda e: e.activation(out=oacc[:, c0_:c1_], in_=pO[:, 0:n * 64], func=AF.Copy),
                             reads=[pOb], writes=[b["oacc"]])
                    else:
                        S.op("dve", lambda e: e.tensor_tensor(out=oacc[:, c0_:c1_], in0=oacc[:, c0_:c1_], in1=pO[:, 0:n * 64],
                                                              op=ALU.add), reads=[pOb, b["oacc"]], writes=[b["oacc"]])

                n_ = len(order)
                pend = None
                for (g0, n) in gorder:
                    aT_, aTb_ = att_group(g0, n)
                    cl = list(range(g0, g0 + n)) if d_ == 0 else list(range(g0 + n - 1, g0 - 1, -1))
                    for c in cl:
                        i = pos_of[c]
                        if i + 1 < n_:
                            state_step(i, c)
                    if pend is not None:
                        out_group(*pend)
                    pend = (g0, n, aT_, aTb_)
                out_group(*pend)
            if True:
                og = k1
                S.dma("sp", og[:], k.pAog.ap()[h * 128:(h + 1) * 128, :], reads=[k.B["pAog"]], writes=[b["k1"]])
                for (t0, W) in BLOCKS:
                    if last and t0 == 0:
                        continue
                    head_readout(k, S, st, "hg", oacc[:, t0:t0 + W], b["oacc"], t0, W, gn[:, layer * 4 + h:layer * 4 + h + 1],
                                 b["gn"], og[:, t0:t0 + W], b["k1"], q1[:, t0:t0 + W], b["q1"], pa, sqs, tmps)
                S.dma("pool", k.ybr.ap()[0, h * 128:(h + 1) * 128, :], q1[:], reads=[b["q1"]], writes=[k.B["ybr0"]])
        S.emit()


def phase_mlp(k, layer, last):
    nc, S, I = k.nc, k.S, k.I
    with ExitStack() as st:
        sb = lambda n, s, d: st.enter_context(nc.sbuf_tensor(U("ml_" + n), s, d))
        uT = sb("uT", [128, 4, NTOK], BF16)
        vB = sb("vB", [128, NT, 512], BF16)
        yb = sb("yb", [128, 4, NTOK], BF16)
        wsT32 = sb("wsT32", [128, 4, 128], F32)
        wsT = sb("wsT", [128, 4, 128], BF16)
        bsr = sb("bsr", [128, 512], F32)
        tmps = Rot([(sb("tmp%d" % i, [128, 512], F32), Buf()) for i in range(2)])
        pms = Rot([(st.enter_context(nc.psum_tensor(U("ml_pm%d" % i), [128, 512], F32)), Buf()) for i in range(2)])
        bu, bv, by, bw, bb = Buf(), Buf(), Buf(), Buf(), Buf()
        S.dma("sp", uT[:], k.pBu.ap().rearrange("(g d) t -> d g t", d=128), reads=[k.B["pBu"]], writes=[bu])
        S.dma("sp", vB[:], k.pBv.ap().rearrange("(n p) c -> p n c", p=128), reads=[k.B["pBv"]], writes=[bv])
        S.dma("sp", wsT32[:], I["wsT"].ap()[layer], writes=[bw])
        S.dma("sp", bsr[:], AP(I["bs"], layer * 512, [[0, 128], [1, 512]]), writes=[bb])
        S.op("dve", lambda e: e.tensor_copy(out=wsT[:], in_=wsT32[:]), reads=[bw], writes=[bw])
        for i in range(NT):
            if last and i < 2:
                continue
            pm, pmb = pms.next()
            for g in range(4):
                S.op("pe", lambda e, g=g, pm=pm, i=i: e.matmul(pm[:, g * 128:(g + 1) * 128], lhsT=vB[:, i, g * 128:(g + 1) * 128],
                                                               rhs=wsT[:, g, :], start=True, stop=True), reads=[bv, bw],
                     writes=[pmb])
            tmp, tb = tmps.next()
            S.op("dve", lambda e, pm=pm, tmp=tmp: e.tensor_tensor(out=tmp[:], in0=pm[:], in1=bsr[:], op=ALU.add),
                 reads=[pmb, bb], writes=[tb])
            S.op("dve", lambda e, tmp=tmp, i=i: e.tensor_tensor(
                out=yb[:, :, i * 128:(i + 1) * 128], in0=AP(tmp, 0, [[512, 128], [128, 4], [1, 128]]),
                in1=uT[:, :, i * 128:(i + 1) * 128], op=ALU.mult), reads=[tb, bu], writes=[by])
        if last:
            S.op("dve", lambda e: e.memset(yb[:, :, 0:256], 0.0), writes=[by])
        S.dma("sp", k.ybr.ap()[1].rearrange("(g d) t -> d g t", d=128), yb[:], reads=[by], writes=[k.B["ybr1"]])
        S.emit()


def phase_attn(k, layer, last):
    nc, S, I = k.nc, k.S, k.I
    lam_init = 0.8 - 0.6 * math.exp(-0.3 * layer)
    with ExitStack() as st:
        sb = lambda n, s, d: st.enter_context(nc.sbuf_tensor(U("at_" + n), s, d))
        qT = sb("qT", [128, NTOK], BF16)
        kT = sb("kT", [128, NTOK], BF16)
        vC = sb("vC", [128, NT, 128], BF16)
        yc = sb("yc", [128, NTOK], BF16)
        dl = sb("dl", [128, 256], F32)
        dl2 = sb("dl2", [128, 128], F32)
        lam = sb("lam", [128, 4], F32)
        sl = sb("sl", [128, 8], F32)
        slc = sb("slc", [128, 8], F32)
        mx = sb("mx", [128, 2, 2, 16], F32)
        nb = sb("nb", [128, 8], F32)
        pTs = Rot([(sb("pT%d" % i, [128, 1024], BF16), Buf()) for i in range(4)])
        t2s = Rot([(sb("t2_%d" % i, [128, 512], BF16), Buf()) for i in range(6)])
        accPs = [(sb("accP%d" % i, [128, 512], F32), Buf()) for i in range(2)]
        ones32 = sb("ones32", [128, 128], F32)
        sqs = Rot([(sb("sq%d" % i, [128, 512], BF16), Buf()) for i in range(2)])
        tmps = Rot([(sb("tmp%d" % i, [128, 512], F32), Buf()) for i in range(2)])
        o0 = sb("o0", [128, 512], F32)
        o1 = sb("o1", [128, 512], F32)
        rl = sb("rl", [128, 512], F32)
        b = {n: Buf(n) for n in "qT kT vC yc dl lam sl mx nb o0 o1 rl ones32".split()}
        pss = Rot([(st.enter_context(nc.psum_tensor(U("at_ps%d" % i), [128, 1024], F32)), Buf()) for i in range(2)])
        pos = [(st.enter_context(nc.psum_tensor(U("at_po%d" % i), [128, 512], F32)), Buf()) for i in range(2)]
        prs = Rot([(st.enter_context(nc.psum_tensor(U("at_pr%d" % i), [128, 512], F32)), Buf()) for i in range(2)])

        S.dma("sp", dl[:], AP(I["dlam"], layer * 256, [[0, 128], [1, 256]]), writes=[b["dl"]])
        S.dma("sp", sl[:], I["sublnT"].ap().rearrange("p a b -> p (a b)"), writes=[b["sl"]])
        S.op("dve", lambda e: e.tensor_tensor(out=AP(dl2, 0, [[128, 128], [64, 2], [1, 64]]),
                                              in0=AP(dl, 0, [[256, 128], [128, 2], [1, 64]]),
                                              in1=AP(dl, 64, [[256, 128], [128, 2], [1, 64]]), op=ALU.mult),
             reads=[b["dl"]], writes=[b["dl"]])
        S.op("dve", lambda e: e.tensor_reduce(out=lam[:, 0:2], in_=AP(dl2, 0, [[128, 128], [64, 2], [1, 64]]), axis=AX.X,
                                              op=ALU.add), reads=[b["dl"]], writes=[b["lam"]])
        S.op("act", lambda e: e.activation(out=lam[:, 0:2], in_=lam[:, 0:2], func=AF.Exp), reads=[b["lam"]],
             writes=[b["lam"]])
        S.op("dve", lambda e: e.tensor_tensor(out=lam[:, 2:3], in0=lam[:, 1:2], in1=lam[:, 0:1], op=ALU.subtract),
             reads=[b["lam"]], writes=[b["lam"]])
        S.op("dve", lambda e: e.tensor_scalar(out=lam[:, 3:4], in0=lam[:, 2:3], scalar1=-lam_init, scalar2=None,
                                              op0=ALU.add), reads=[b["lam"]], writes=[b["lam"]])
        S.op("dve", lambda e: e.tensor_scalar(out=slc[:], in0=sl[:], scalar1=(1.0 - lam_init), scalar2=None, op0=ALU.mult),
             reads=[b["sl"]], writes=[b["sl"]])

        for h in range(4):
            S.dma("sp", qT[:], k.pCq.ap()[h * 128:(h + 1) * 128, :], reads=[k.B["pCq"]], writes=[b["qT"]])
            S.dma("sp", kT[:], k.pCk.ap()[h * 128:(h + 1) * 128, :], reads=[k.B["pCk"]], writes=[b["kT"]])
            S.dma("sp", vC[:], k.pCv.ap().rearrange("(n p) d -> p n d", p=128)[:, :, h * 128:(h + 1) * 128],
                  reads=[k.B["pCv"]], writes=[b["vC"]])
            S.op("dve", lambda e: e.memset(mx[:], 0.0), writes=[b["mx"]])
            for qi, (src, srcb) in enumerate([(qT, b["qT"]), (kT, b["kT"])]):
                for bi, (t0, W) in enumerate(BLOCKS):
                    sq, sqb = sqs.next()
                    S.op("act", lambda e, sq=sq, src=src, t0=t0, W=W: e.activation(out=sq[:, 0:W], in_=src[:, t0:t0 + W],
                                                                                   func=AF.Square), reads=[srcb],
                         writes=[sqb])
                    for c in range(2):
                        pr, prb = prs.next()
                        sel = k.sel0 if c == 0 else k.sel1
                        S.op("pe", lambda e, pr=pr, sel=sel, sq=sq, W=W: e.matmul(pr[:, 0:W], lhsT=sel[:], rhs=sq[:, 0:W],
                                                                                  start=True, stop=True),
                             reads=[sqb, k.Bc], writes=[prb])
                        S.op("dve", lambda e, pr=pr, qi=qi, c=c, bi=bi, W=W: e.tensor_reduce(
                            out=mx[:, qi, c, bi:bi + 1], in_=pr[:, 0:W], axis=AX.X, op=ALU.max), reads=[prb],
                            writes=[b["mx"]])
            S.op("dve", lambda e: e.tensor_reduce(out=nb[:, 0:4], in_=AP(mx, 0, [[64, 128], [16, 4], [1, 16]]), axis=AX.X,
                                                  op=ALU.max), reads=[b["mx"]], writes=[b["nb"]])
            S.op("dve", lambda e: e.tensor_tensor(out=nb[:, 4:6], in0=nb[:, 0:2], in1=nb[:, 2:4], op=ALU.mult),
                 reads=[b["nb"]], writes=[b["nb"]])
            S.op("act", lambda e: e.activation(out=nb[:, 4:6], in_=nb[:, 4:6], func=AF.Ln), reads=[b["nb"]], writes=[b["nb"]])
            S.op("act", lambda e: e.activation(out=nb[:, 4:6], in_=nb[:, 4:6], func=AF.Exp, scale=0.5), reads=[b["nb"]],
                 writes=[b["nb"]])
            S.op("dve", lambda e: e.tensor_scalar(out=nb[:, 6:8], in0=nb[:, 4:6], scalar1=-0.125, scalar2=None,
                                                  op0=ALU.mult), reads=[b["nb"]], writes=[b["nb"]])
            S.op("dve", lambda e: e.tensor_tensor(out=nb[:, 5:6], in0=nb[:, 6:7], in1=nb[:, 7:8], op=ALU.min),
                 reads=[b["nb"]], writes=[b["nb"]])
            seq = []
            for (t0, W) in BLOCKS:
                if t0 == 0:
                    if last:
                        continue
                    keys = list(range(0, 2))
                else:
                    keys = list(range(0, NT))
                for ji, j in enumerate(keys):
                    seq.append((t0, W, ji, j, len(keys)))

            def pair(t, W):
                return AP(t, 0, [[1024, 128], [512, 2], [1, W]])

            def qk_exp(t0, W, ji, j, nk):
                ps, psb = pss.next()
                S.op("pe", [lambda e, c=c: e.matmul(ps[:, c * 512:c * 512 + W],
                                                    lhsT=kT[c * 64:(c + 1) * 64, j * 128:(j + 1) * 128],
                                                    rhs=qT[c * 64:(c + 1) * 64, t0:t0 + W], start=True, stop=True)
                            for c in range(2)], reads=[b["kT"], b["qT"]], writes=[psb])
                pT, pTb = pTs.next()
                S.op("act", lambda e: e.activation(out=pair(pT, W), in_=pair(ps, W), func=AF.Exp, scale=0.125,
                                                   bias=nb[:, 5:6]), reads=[psb, b["nb"]], writes=[pTb])
                return pT, pTb

            prev = {}

            def av(t0, W, ji, j, nk, pT, pTb, h=h):
                S.op("pe", [lambda e, c=c: e.matmul(pos[c][0][:, 0:W], lhsT=vC[:, j, :], rhs=pT[:, c * 512:c * 512 + W],
                                                    start=(ji == 0), stop=(ji == nk - 1)) for c in range(2)],
                     reads=[b["vC"], pTb], writes=[pos[0][1], pos[1][1]])
                if ji % 2 == 0:
                    prev["p"] = (pT, pTb)
                    return
                pP, pPb = prev["p"]
                for c in range(2):
                    aP, aPb = accPs[c]
                    if ji == 1:
                        S.op("dve", lambda e, c=c, aP=aP: e.tensor_tensor(out=aP[:, 0:W], in0=pP[:, c * 512:c * 512 + W],
                                                                          in1=pT[:, c * 512:c * 512 + W], op=ALU.add),
                             reads=[pTb, pPb], writes=[aPb])
                    else:
                        t2, t2b = t2s.next()
                        S.op("dve", lambda e, c=c, t2=t2: e.tensor_tensor(out=t2[:, 0:W], in0=pP[:, c * 512:c * 512 + W],
                                                                          in1=pT[:, c * 512:c * 512 + W], op=ALU.add),
                             reads=[pTb, pPb], writes=[t2b])
                        S.op("dve", lambda e, aP=aP, t2=t2: e.tensor_tensor(out=aP[:, 0:W], in0=aP[:, 0:W], in1=t2[:, 0:W],
                                                                            op=ALU.add), reads=[t2b, aPb], writes=[aPb])
                if ji != nk - 1:
                    return
                for c in range(2):
                    aP, aPb = accPs[c]
                    po, pob = pos[c]
                    pl, plb = prs.next()
                    hi, hib = t2s.next()
                    lo, lob = t2s.next()
                    S.op("dve", lambda e, hi=hi, aP=aP: e.tensor_copy(out=hi[:, 0:W], in_=aP[:, 0:W]), reads=[aPb], writes=[hib])
                    S.op("dve", lambda e, hi=hi, lo=lo, aP=aP: e.tensor_tensor(out=lo[:, 0:W], in0=aP[:, 0:W], in1=hi[:, 0:W],
                                                                               op=ALU.subtract), reads=[aPb, hib], writes=[lob])
                    S.op("pe", [lambda e, pl=pl, hi=hi: e.matmul(pl[:, 0:W], lhsT=k.ones[:], rhs=hi[:, 0:W], start=True, stop=False),
                                lambda e, pl=pl, lo=lo: e.matmul(pl[:, 0:W], lhsT=k.ones[:], rhs=lo[:, 0:W], start=False, stop=True)],
                         reads=[hib, lob, k.Bc], writes=[plb])
                    oc, ocb = (o0, b["o0"]) if c == 0 else (o1, b["o1"])
                    S.op("act", lambda e, pl=pl: e.activation(out=rl[:, 0:W], in_=pl[:, 0:W], func=AF.Ln), reads=[plb],
                         writes=[b["rl"]])
                    S.op("act", lambda e: e.activation(out=rl[:, 0:W], in_=rl[:, 0:W], func=AF.Exp, scale=-1.0), reads=[b["rl"]],
                         writes=[b["rl"]])
                    S.op("dve", lambda e, po=po, oc=oc: e.tensor_tensor(out=oc[:, 0:W], in0=po[:, 0:W], in1=rl[:, 0:W],
                                                                        op=ALU.mult), reads=[pob, b["rl"]], writes=[ocb])
                S.op("dve", lambda e: e.scalar_tensor_tensor(out=o0[:, 0:W], in0=o1[:, 0:W], scalar=lam[:, 3:4], in1=o0[:, 0:W],
                                                             op0=ALU.mult, op1=ALU.add), reads=[b["o0"], b["o1"], b["lam"]],
                     writes=[b["o0"]])
                head_readout(k, S, st, "at", o0[:, 0:W], b["o0"], t0, W, slc[:, layer * 4 + h:layer * 4 + h + 1], b["sl"], None,
                             None, yc[:, t0:t0 + W], b["yc"], prs, sqs, tmps)

            LOOK = 1
            pend = []
            for idx in range(len(seq) + LOOK):
                if idx < len(seq):
                    pend.append(qk_exp(*seq[idx]))
                if idx >= LOOK:
                    pT_, pTb_ = pend.pop(0)
                    av(*seq[idx - LOOK], pT_, pTb_)
            if last:
                S.op("dve", lambda e: e.memset(yc[:, 0:256], 0.0), writes=[b["yc"]])
            S.dma("pool", k.ybr.ap()[2, h * 128:(h + 1) * 128, :], yc[:], reads=[b["yc"]], writes=[k.B["ybr2"]])
        S.emit()


def phase_merge(k, layer, last):
    nc, S, I = k.nc, k.S, k.I
    with ExitStack() as st:
        sb = lambda n, s, d: st.enter_context(nc.sbuf_tensor(U("mg_" + n), s, d))
        wb = sb("wb", [128, 3, 4, D], BF16)
        wo = sb("wo", [128, 8, D], BF16)
        bw = Buf()
        for i in range(3):
            S.dma("pool", wb[:, i, :, :], I["w_branch"].ap()[layer, i].rearrange("(k p) n -> p k n", p=128), writes=[bw])
        S.dma("pool", wo[:], I["w_out"].ap()[layer].rearrange("(k p) n -> p k n", p=128), writes=[bw])
        G1 = [sb("G1_%d" % w, [128, D], F32) for w in range(2)]
        A2 = [sb("A2_%d" % w, [128, D], F32) for w in range(2)]
        B2 = [sb("B2_%d" % w, [128, D], F32) for w in range(2)]
        mb = Buf()
        for w in range(2):
            if last and w == 1:
                continue
            load_rep(k, S, G1[w], mb, w, 2)
            load_rep(k, S, A2[w], mb, w, 4)
            load_rep(k, S, B2[w], mb, w, 3)
        ys = Rot([(sb("y%d" % i, [128, 3, 4, 512], BF16), Buf()) for i in range(2)])
        sgs = Rot([(sb("sg%d" % i, [128, 24, 512], BF16), Buf()) for i in range(2)])
        mTs = Rot([(sb("mT%d" % i, [128, 8, 512], BF16), Buf()) for i in range(2)])
        macc = sb("macc", [128, 512], F32)
        maccb = Buf()
        tmps = Rot([(sb("tmp%d" % i, [128, 512], F32), Buf()) for i in range(2)])
        xts = Rot([(sb("xt%d" % i, [128, D], F32), Buf()) for i in range(2)])
        junks = Rot([(sb("junk%d" % i, [128, D], F32), Buf()) for i in range(2)])
        hbs = Rot([(sb("hb%d" % i, [128, D], BF16), Buf()) for i in range(2)])
        sss = Rot([(sb("ss%d" % i, [128, 4], F32), Buf()) for i in range(2)])
        h2s = Rot([(sb("h2s%d" % i, [128, 8, 512], BF16), Buf()) for i in range(2)])
        pbs = Rot([(st.enter_context(nc.psum_tensor(U("mg_pb%d" % i), [128, 512], F32)), Buf()) for i in range(3)])
        pms = Rot([(st.enter_context(nc.psum_tensor(U("mg_pm%d" % i), [128, 512], F32)), Buf()) for i in range(2)])
        pts = Rot([(st.enter_context(nc.psum_tensor(U("mg_pt%d" % i), [128, 512], BF16)), Buf()) for i in range(2)])
        ybufs = [k.B["ybr0"], k.B["ybr1"], k.B["ybr2"]]
        def part1(t0, W):
            mT, mTb = mTs.next()
            y, yb_ = ys.next()
            sg, sgb = sgs.next()
            for i in range(3):
                S.dma("sp", y[:, i, :, 0:W], k.ybr.ap()[i].rearrange("(k p) t -> p k t", p=128)[:, :, t0:t0 + W],
                      reads=[ybufs[i]], writes=[yb_])
            S.dma("sp", sg[:, :, 0:W], k.pG.ap().rearrange("(k p) t -> p k t", p=128)[:, :, t0:t0 + W], reads=[k.B["pG"]],
                  writes=[sgb])
            for oc in range(8):
                for i in range(3):
                    pb, pbb = pbs.next()
                    S.op("pe", [lambda e, pb=pb, i=i, kk=kk, oc=oc, y=y: e.matmul(
                        pb[:, 0:W], lhsT=wb[:, i, kk, oc * 128:(oc + 1) * 128], rhs=y[:, i, kk, 0:W], start=(kk == 0),
                        stop=(kk == 3)) for kk in range(4)], reads=[bw, yb_], writes=[pbb])
                    if i == 0:
                        S.op("dve", lambda e, pb=pb, sg=sg, oc=oc: e.tensor_tensor(out=macc[:, 0:W], in0=pb[:, 0:W],
                                                                                   in1=sg[:, oc, 0:W], op=ALU.mult),
                             reads=[pbb, sgb], writes=[maccb])
                    else:
                        tmp, tb = tmps.next()
                        S.op("dve", lambda e, pb=pb, sg=sg, oc=oc, i=i, tmp=tmp: e.tensor_tensor(
                            out=tmp[:, 0:W], in0=pb[:, 0:W], in1=sg[:, i * 8 + oc, 0:W], op=ALU.mult), reads=[pbb, sgb],
                            writes=[tb])
                        if i == 1:
                            S.op("dve", lambda e, tmp=tmp: e.tensor_tensor(out=macc[:, 0:W], in0=macc[:, 0:W], in1=tmp[:, 0:W],
                                                                           op=ALU.add), reads=[tb, maccb], writes=[maccb])
                        else:
                            S.op("dve", lambda e, tmp=tmp, oc=oc: e.tensor_tensor(out=mT[:, oc, 0:W], in0=macc[:, 0:W],
                                                                                  in1=tmp[:, 0:W], op=ALU.add),
                                 reads=[tb, maccb], writes=[mTb])
            return mT, mTb

        def part2(t0, W, mT, mTb):
            w_ = 1 if t0 == 0 else 0
            h2, h2b = h2s.next()
            for ts in range(W // 128):
                row0 = t0 + ts * 128
                xt, xb = xts.next()
                S.dma("sp", xt[:], k.xs.ap()[row0:row0 + 128, :], reads=[k.Bxs[row0 // 128]], writes=[xb])
                for half in range(2):
                    pm, pmb = pms.next()
                    S.op("pe", [lambda e, pm=pm, kk=kk, ts=ts, half=half: e.matmul(
                        pm[:], lhsT=mT[:, kk, ts * 128:(ts + 1) * 128], rhs=wo[:, kk, half * 512:(half + 1) * 512],
                        start=(kk == 0), stop=(kk == 7)) for kk in range(8)], reads=[mTb, bw], writes=[pmb])
                    tmp, tb = tmps.next()
                    S.op("dve", lambda e, pm=pm, tmp=tmp, half=half: e.tensor_tensor(
                        out=tmp[:], in0=pm[:], in1=G1[w_][:, half * 512:(half + 1) * 512], op=ALU.mult), reads=[pmb, mb],
                        writes=[tb])
                    S.op("dve", lambda e, tmp=tmp, xt=xt, half=half: e.tensor_tensor(
                        out=xt[:, half * 512:(half + 1) * 512], in0=xt[:, half * 512:(half + 1) * 512], in1=tmp[:],
                        op=ALU.add), reads=[tb, xb], writes=[xb])
                S.dma("pool", k.xs.ap()[row0:row0 + 128, :], xt[:], reads=[xb], writes=[k.Bxs[row0 // 128]])
                junk, jb = junks.next()
                hb, hbb = hbs.next()
                ss, ssb = sss.next()
                rms_mod(S, xt, xb, A2[w_], mb, B2[w_], mb, hb, hbb, ss, ssb, junk, jb)
                for half in range(2):
                    pt, ptb = pts.next()
                    S.op("pe", [lambda e, pt=pt, hb=hb, j=j, half=half: e.transpose(
                        out=pt[:, j * 128:(j + 1) * 128], in_=hb[:, (half * 4 + j) * 128:(half * 4 + j + 1) * 128],
                        identity=k.ident[:]) for j in range(4)], reads=[hbb, k.Bc], writes=[ptb])
                    outap = h2[:, half * 4:half * 4 + 4, ts * 128:(ts + 1) * 128]
                    inap = AP(pt, 0, [[512, 128], [128, 4], [1, 128]])
                    S.op("act", lambda e, o=outap, a=inap: e.activation(out=o, in_=a, func=AF.Copy), reads=[ptb],
                         writes=[h2b])
            S.dma("pool", k.h2T.ap().rearrange("(k p) t -> p k t", p=128)[:, :, t0:t0 + W], h2[:, :, 0:W], reads=[h2b],
                  writes=[k.B["h2T"]])

        pend = None
        for (t0, W) in BLOCKS:
            if last and t0 == 0:
                continue
            mT_, mTb_ = part1(t0, W)
            if pend is not None:
                part2(*pend)
            pend = (t0, W, mT_, mTb_)
        if pend is not None:
            part2(*pend)
        S.emit()


def phase_ffn(k, layer, last):
    nc, S, I = k.nc, k.S, k.I
    moe = (layer % 2 == 1)
    if moe:
        FU = 4
        experts = [(I["moe_wg"].ap()[0, e], I["moe_wu"].ap()[0, e], I["moe_wd"].ap()[0, e], DFFE) for e in range(NEXP)]
        groups = [list(range(2, 18)), list(range(18, 34))]
    else:
        FU = 2
        experts = [(I["ffn_wg"].ap()[0], I["ffn_wu"].ap()[0], I["ffn_wd"].ap()[0], DFF)]
        groups = [list(range(0, 17)), list(range(17, 34))]
    with ExitStack() as st:
        sb = lambda n, s, d: st.enter_context(nc.sbuf_tensor(U("ff_" + n), s, d))
        NTG = 17
        acc = sb("acc", [128, NTG, D], F32)
        accb = [Buf() for _ in range(NTG)]
        h2 = sb("h2", [128, 8, NTG * 128], BF16)
        h2b = Buf()
        wgu = Rot([(sb("wgu%d" % i, [128, 8, 2, FU * 128], BF16), Buf()) for i in range(2)])
        wds = Rot([(sb("wd%d" % i, [128, FU, D], BF16), Buf()) for i in range(2)])
        acts = Rot([(sb("act%d" % i, [128, FU, 512], BF16), Buf()) for i in range(2)])
        sgs = Rot([(sb("sg%d" % i, [128, 512], F32), Buf()) for i in range(2)])
        G2 = sb("G2", [128, D], F32)
        gf = sb("gf", [128, D], F32)
        mb = Buf()
        xts = Rot([(sb("xt%d" % i, [128, D], F32), Buf()) for i in range(2)])
        junks = Rot([(sb("junk%d" % i, [128, D], F32), Buf()) for i in range(2)])
        sss = Rot([(sb("ss%d" % i, [128, 4], F32), Buf()) for i in range(2)])
        comb = sb("comb", [128, NTG, 8], F32)
        combb = Buf()
        wr32 = sb("wr32", [128, 8, 8], F32)
        wr = sb("wr", [128, 8, 8], BF16)
        wrb = Buf()
        rt = sb("rt", [128, 8, 8], F32)
        rtb = Buf()
        pgs = Rot([(st.enter_context(nc.psum_tensor(U("ff_pg%d" % i), [128, 512], F32)), Buf()) for i in range(2)])
        pus = Rot([(st.enter_context(nc.psum_tensor(U("ff_pu%d" % i), [128, 512], F32)), Buf()) for i in range(2)])
        pds = Rot([(st.enter_context(nc.psum_tensor(U("ff_pd%d" % i), [128, 512], F32)), Buf()) for i in range(3)])
        prt = st.enter_context(nc.psum_tensor(U("ff_pr"), [128, 512], F32))
        prtb = Buf()
        if moe:
            S.dma("sp", wr32[:], I["moe_router"].ap()[0].rearrange("(k p) e -> p k e", p=128), writes=[wrb])
            S.op("dve", lambda e: e.tensor_copy(out=wr[:], in_=wr32[:]), reads=[wrb], writes=[wrb])
        if last:
            S.dma("sp", gf[:], AP(I["g_final"], 0, [[0, 128], [1, D]]), writes=[mb])
        for gi, tiles in enumerate(groups):
            ntg = len(tiles)
            tok0 = tiles[0] * 128
            ntok = ntg * 128
            blocks = [(o, min(512, ntok - o)) for o in range(0, ntok, 512)]
            S.dma("sp", h2[:, :, 0:ntok], k.h2T.ap().rearrange("(k p) t -> p k t", p=128)[:, :, tok0:tok0 + ntok],
                  reads=[k.B["h2T"]], writes=[h2b])
            if moe:
                for ti in range(ntg):
                    S.op("pe", [lambda e, kk=kk, ti=ti: e.matmul(prt[:, 0:8], lhsT=h2[:, kk, ti * 128:(ti + 1) * 128],
                                                                 rhs=wr[:, kk, :], start=(kk == 0), stop=(kk == 7))
                                for kk in range(8)], reads=[h2b, wrb], writes=[prtb])
                    S.op("dve", lambda e: e.tensor_copy(out=rt[:, 0, :], in_=prt[:, 0:8]), reads=[prtb], writes=[rtb])
                    S.op("dve", lambda e: e.max(out=rt[:, 1, :], in_=rt[:, 0, :]), reads=[rtb], writes=[rtb])
                    S.op("dve", lambda e: e.tensor_scalar(out=rt[:, 2, :], in0=rt[:, 0, :], scalar1=rt[:, 1, 1:2], scalar2=None,
                                                          op0=ALU.is_ge), reads=[rtb], writes=[rtb])
                    S.op("dve", lambda e: e.tensor_scalar(out=rt[:, 5, 0:1], in0=rt[:, 1, 0:1], scalar1=-1.0, scalar2=None,
                                                          op0=ALU.mult), reads=[rtb], writes=[rtb])
                    S.op("act", lambda e: e.activation(out=rt[:, 3, :], in_=rt[:, 0, :], func=AF.Exp, bias=rt[:, 5, 0:1]),
                         reads=[rtb], writes=[rtb])
                    S.op("dve", lambda e: e.tensor_tensor(out=rt[:, 4, :], in0=rt[:, 3, :], in1=rt[:, 2, :], op=ALU.mult),
                         reads=[rtb], writes=[rtb])
                    S.op("dve", lambda e: e.tensor_reduce(out=rt[:, 5, 1:2], in_=rt[:, 4, :], axis=AX.X, op=ALU.add),
                         reads=[rtb], writes=[rtb])
                    S.op("dve", lambda e: e.reciprocal(out=rt[:, 5, 2:3], in_=rt[:, 5, 1:2]), reads=[rtb], writes=[rtb])
                    S.op("dve", lambda e, ti=ti: e.tensor_scalar(out=comb[:, ti, :], in0=rt[:, 4, :], scalar1=rt[:, 5, 2:3],
                                                                 scalar2=None, op0=ALU.mult), reads=[rtb], writes=[combb])
            def up(o, W, w, wbuf):
                at, atb = acts.next()
                for fc in range(FU):
                    pg, pgb = pgs.next()
                    pu, pub = pus.next()
                    S.op("pe", [lambda e, pg=pg, kk=kk, fc=fc: e.matmul(
                        pg[:, 0:W], lhsT=w[:, kk, 0, fc * 128:(fc + 1) * 128], rhs=h2[:, kk, o:o + W], start=(kk == 0),
                        stop=(kk == 7)) for kk in range(8)], reads=[wbuf, h2b], writes=[pgb])
                    S.op("pe", [lambda e, pu=pu, kk=kk, fc=fc: e.matmul(
                        pu[:, 0:W], lhsT=w[:, kk, 1, fc * 128:(fc + 1) * 128], rhs=h2[:, kk, o:o + W], start=(kk == 0),
                        stop=(kk == 7)) for kk in range(8)], reads=[wbuf, h2b], writes=[pub])
                    sg, sgb = sgs.next()
                    S.op("act", lambda e, pg=pg, sg=sg: e.activation(out=sg[:, 0:W], in_=pg[:, 0:W], func=AF.Silu),
                         reads=[pgb], writes=[sgb])
                    S.op("dve", lambda e, pu=pu, sg=sg, fc=fc: e.tensor_tensor(
                        out=at[:, fc, 0:W], in0=sg[:, 0:W], in1=pu[:, 0:W], op=ALU.mult), reads=[sgb, pub],
                        writes=[atb])
                return at, atb

            def down(o, W, at, atb, wd, wdb, ei, first):
                for ts in range(W // 128):
                    ti = o // 128 + ts
                    for half in range(2):
                        pd, pdb = pds.next()
                        S.op("pe", [lambda e, pd=pd, fc=fc, ts=ts, half=half: e.matmul(
                            pd[:], lhsT=at[:, fc, ts * 128:(ts + 1) * 128], rhs=wd[:, fc, half * 512:(half + 1) * 512],
                            start=(fc == 0), stop=(fc == FU - 1)) for fc in range(FU)], reads=[atb, wdb],
                            writes=[pdb])
                        av = acc[:, ti, half * 512:(half + 1) * 512]
                        if moe:
                            cs_ = comb[:, ti, ei:ei + 1]
                            if first:
                                S.op("dve", lambda e, pd=pd, av=av, cs_=cs_: e.tensor_scalar(
                                    out=av, in0=pd[:], scalar1=cs_, scalar2=None, op0=ALU.mult),
                                    reads=[pdb, combb], writes=[accb[ti]])
                            else:
                                S.op("dve", lambda e, pd=pd, av=av, cs_=cs_: e.scalar_tensor_tensor(
                                    out=av, in0=pd[:], scalar=cs_, in1=av, op0=ALU.mult, op1=ALU.add),
                                    reads=[pdb, combb, accb[ti]], writes=[accb[ti]])
                        else:
                            if first:
                                S.op("act", lambda e, pd=pd, av=av: e.activation(out=av, in_=pd[:], func=AF.Copy),
                                     reads=[pdb], writes=[accb[ti]])
                            else:
                                S.op("dve", lambda e, pd=pd, av=av: e.tensor_tensor(out=av, in0=av, in1=pd[:], op=ALU.add),
                                     reads=[pdb, accb[ti]], writes=[accb[ti]])

            first = True
            pending = None
            for ei, (wg_ap, wu_ap, wd_ap, dff) in enumerate(experts):
                wg_v = wg_ap.rearrange("(k p) n -> p k n", p=128)
                wu_v = wu_ap.rearrange("(k p) n -> p k n", p=128)
                wd_v = wd_ap.rearrange("(f p) n -> p f n", p=128)
                nfc = dff // 128
                for u0 in range(0, nfc, FU):
                    w, wbuf = wgu.next()
                    wd, wdb = wds.next()
                    S.dma("pool", w[:, :, 0, :], wg_v[:, :, u0 * 128:(u0 + FU) * 128], writes=[wbuf])
                    S.dma("pool", w[:, :, 1, :], wu_v[:, :, u0 * 128:(u0 + FU) * 128], writes=[wbuf])
                    S.dma("pool", wd[:], wd_v[:, u0:u0 + FU, :], writes=[wdb])
                    for (o, W) in blocks:
                        at, atb = up(o, W, w, wbuf)
                        if pending is not None:
                            down(*pending)
                        pending = (o, W, at, atb, wd, wdb, ei, first)
                    first = False
            if pending is not None:
                down(*pending)
            for ti, tile in enumerate(tiles):
                w_ = 1 if tile < 2 else 0
                if ti == 0 or (tile == 2 and not moe):
                    load_rep(k, S, G2, mb, w_, 5)
                xt, xb = xts.next()
                row0 = tile * 128
                S.dma("sp", xt[:], k.xs.ap()[row0:row0 + 128, :], reads=[k.Bxs[row0 // 128]], writes=[xb])
                S.op("dve", lambda e, ti=ti: e.tensor_tensor(out=acc[:, ti, :], in0=acc[:, ti, :], in1=G2[:], op=ALU.mult),
                     reads=[accb[ti], mb], writes=[accb[ti]])
                S.op("dve", lambda e, ti=ti, xt=xt: e.tensor_tensor(out=xt[:], in0=xt[:], in1=acc[:, ti, :], op=ALU.add),
                     reads=[accb[ti], xb], writes=[xb])
                if not last:
                    S.dma("pool", k.xs.ap()[row0:row0 + 128, :], xt[:], reads=[xb], writes=[k.Bxs[row0 // 128]])
                else:
                    junk, jb = junks.next()
                    ss, ssb = sss.next()
                    rms_mod(S, xt, xb, gf, mb, None, None, None, None, ss, ssb, junk, jb)
                    S.dma("pool", k.out.ap()[row0 - NCTX:row0 - NCTX + 128, :], junk[:], reads=[jb], writes=[k.B["out"]],
                          is_output=True)
        S.emit()


def _rope_tables():
    t = np.arange(NLAT)
    row = (t // 64).astype(np.float32)
    col = (t % 64).astype(np.float32)
    freqs = (10000.0 ** (-np.arange(16, dtype=np.float32) / 16)).astype(np.float32)
    C = np.ones((128, NTOK), np.float32)
    Sn = np.zeros((128, NTOK), np.float32)
    for p in range(128):
        d = p % 64
        axis, half, i = d // 32, (d % 32) // 16, d % 16
        ang = (row if axis == 0 else col) * freqs[i]
        C[p, NCTX:] = np.cos(ang)
        Sn[p, NCTX:] = np.sin(ang) * (-1.0 if half == 0 else 1.0)
    return C, Sn


def _swap_perm():
    p = np.arange(512)
    d = p % 64
    half = (d % 32) // 16
    return p + np.where(half == 0, 16, -16)


_NC_CACHE = {}


def prepare_inputs(inputs):
    f = lambda a: np.ascontiguousarray(np.asarray(a, dtype=np.float32))
    x, c, ctx, c_ctx = f(inputs["x"]), f(inputs["c"]), f(inputs["ctx"]), f(inputs["c_ctx"])
    w_in = f(inputs["w_in"])
    perm = _swap_perm()
    w_in_sw = np.ascontiguousarray(np.concatenate([w_in[:, :, 3584 + perm], w_in[:, :, 4096 + perm]], axis=2))
    ropeC, ropeS = _rope_tables()
    hl = f(inputs["hgrn_lb"])
    lbT = np.ascontiguousarray(hl.reshape(2, 2, 4, 128).transpose(3, 0, 1, 2))
    gnA = np.ascontiguousarray(f(inputs["hgrn_gnorm"]).reshape(2, 4, 128).transpose(2, 0, 1))
    sublnT = np.ascontiguousarray(f(inputs["diff_subln"]).reshape(2, 4, 128).transpose(2, 0, 1))
    wsT = np.ascontiguousarray(f(inputs["mlp_ws"]).transpose(0, 3, 1, 2))
    shared = {
        "w_ada": f(inputs["w_ada"]), "b_ada": f(inputs["b_ada"]), "g_norm1": f(inputs["g_norm1"]),
        "g_norm2": f(inputs["g_norm2"]), "w_in": w_in, "w_in_sw": w_in_sw, "lbT": lbT, "gnA": gnA,
        "vnorm": f(inputs["mlp_vnorm"]), "wsT": wsT, "bs": f(inputs["mlp_bs"]).reshape(2, 512),
        "dlam": f(inputs["diff_lambda"]).reshape(2, 256), "sublnT": sublnT, "w_branch": f(inputs["w_branch"]),
        "w_out": f(inputs["w_out"]), "ffn_wg": f(inputs["ffn_wg"]), "ffn_wu": f(inputs["ffn_wu"]),
        "ffn_wd": f(inputs["ffn_wd"]), "moe_router": f(inputs["moe_router"]), "moe_wg": f(inputs["moe_wg"]),
        "moe_wu": f(inputs["moe_wu"]), "moe_wd": f(inputs["moe_wd"]), "g_final": f(inputs["g_final"]),
        "ropeC": ropeC, "ropeS": ropeS,
    }
    in_maps = []
    for b in range(8):
        cT = np.stack([c[b].reshape(8, 128).T, c_ctx.reshape(8, 128).T], axis=2)
        m = dict(shared)
        m["x"] = x[b]
        m["ctx"] = ctx[b]
        m["cT"] = np.ascontiguousarray(cT)
        in_maps.append(m)
    return in_maps


def kernel(**inputs):
    if "nc" not in _NC_CACHE:
        _NC_CACHE["nc"] = build()
    nc = _NC_CACHE["nc"]
    in_maps = prepare_inputs(inputs)
    res = run_bass_kernel_spmd(nc, in_maps, core_ids=list(range(8)))
    return np.stack([np.asarray(r["out"], dtype=np.float32) for r in res.results], axis=0)
```

```python
import math
import numpy as np
import concourse.bass as bass
import concourse.mybir as mybir
from contextlib import ExitStack
from concourse.bass_utils import run_bass_kernel_spmd

F32 = mybir.dt.float32
BF16 = mybir.dt.bfloat16
AF = mybir.ActivationFunctionType
ALU = mybir.AluOpType
AX = mybir.AxisListType

D = 1024
NLAT = 4096
NCTX = 256
NTOK = NLAT + NCTX
NT = NTOK // 128
EPS = 1e-6
DFF = 2816
DFFE = 3584
NEXP = 8
BLOCKS = [(0, 256)] + [(256 + 512 * i, 512) for i in range(8)]
NCH = NTOK // 64


class Buf:
    __slots__ = ("name", "last_w", "reads")

    def __init__(self, name=""):
        self.name = name
        self.last_w = None
        self.reads = []


class Sched:
    ENG = ("pe", "act", "dve", "pool", "sp")
    NDS = 6

    def __init__(self, nc, stack):
        self.nc = nc
        self.ops = {e: [] for e in self.ENG}
        self.sem = {e: stack.enter_context(nc.semaphore("s_" + e)) for e in self.ENG}
        self.cnt = {e: 0 for e in self.ENG}
        self.seen = {e: {} for e in self.ENG}
        self.dsem = {}
        self.dcnt = {}
        self.drr = {}
        for q in ("sp", "act", "pool"):
            self.dsem[q] = [stack.enter_context(nc.semaphore("d_%s%d" % (q, i))) for i in range(self.NDS)]
            self.dcnt[q] = [0] * self.NDS
            self.drr[q] = 0
        self.out_events = []

    def _deps(self, eng, reads, writes, extra=()):
        need = {}

        def add(ev):
            if ev is None:
                return
            key, sem, val = ev
            if self.seen[eng].get(key, 0) >= val:
                return
            if key not in need or need[key][1] < val:
                need[key] = (sem, val)

        for r in reads:
            add(r.last_w)
        for w in writes:
            add(w.last_w)
            for ev in w.reads:
                add(ev)
        for ev in extra:
            add(ev)
        waits = []
        for key, (sem, val) in need.items():
            self.seen[eng][key] = val
            waits.append((sem, val))
        return waits

    def _commit(self, ev, reads, writes):
        for r in reads:
            r.reads.append(ev)
            if len(r.reads) > 48:
                best = {}
                for e in r.reads:
                    if e[0] not in best or best[e[0]][2] < e[2]:
                        best[e[0]] = e
                r.reads = list(best.values())
        for w in writes:
            w.last_w = ev
            w.reads = []

    def op(self, eng, fns, reads=(), writes=()):
        if callable(fns):
            fns = [fns]
        waits = self._deps(eng, reads, writes)
        self.cnt[eng] += 1
        ev = ("c_" + eng, self.sem[eng], self.cnt[eng])
        self.ops[eng].append((waits, fns, (self.sem[eng], 1)))
        self._commit(ev, reads, writes)
        return ev

    def dma(self, q, out, in_, reads=(), writes=(), is_output=False, **kw):
        i = self.drr[q]
        self.drr[q] = (i + 1) % self.NDS
        sem = self.dsem[q][i]
        key = "d_%s%d" % (q, i)
        prev = self.dcnt[q][i]
        extra = [(key, sem, prev)] if prev > 0 else []
        waits = self._deps(q, reads, writes, extra)
        self.dcnt[q][i] = prev + 16
        ev = (key, sem, prev + 16)
        self.ops[q].append((waits, [lambda e, o=out, a=in_, k=kw: e.dma_start(out=o, in_=a, **k)], (sem, 16)))
        self._commit(ev, reads, writes)
        if is_output:
            self.out_events.append(ev)
        return ev

    def barrier(self):
        allw = {}
        for q in ("sp", "act", "pool"):
            for i in range(self.NDS):
                if self.dcnt[q][i] > 0:
                    allw["d_%s%d" % (q, i)] = (self.dsem[q][i], self.dcnt[q][i])
        for e in self.ENG:
            if self.cnt[e] > 0:
                allw["c_" + e] = (self.sem[e], self.cnt[e])
        for e in self.ENG:
            waits = []
            for key, (sem, val) in allw.items():
                if self.seen[e].get(key, 0) < val:
                    self.seen[e][key] = val
                    waits.append((sem, val))
            self.ops[e].append((waits, [], None))

    def emit(self):
        nc = self.nc
        self.barrier()
        hmap = {"pe": "tensor", "act": "scalar", "dve": "vector", "pool": "gpsimd", "sp": "sync"}
        with nc.Block() as block:
            for e in self.ENG:
                ops = self.ops[e]

                def body(eng, ops=ops):
                    for waits, fns, inc in ops:
                        for sem, val in waits:
                            eng.wait_ge(sem, val)
                        n = len(fns)
                        for j, fn in enumerate(fns):
                            ins = fn(eng)
                            if j == n - 1 and inc is not None:
                                ins.then_inc(inc[0], inc[1])

                getattr(block, hmap[e])(body)
        self.ops = {e: [] for e in self.ENG}


_UID = [0]


def U(name):
    _UID[0] += 1
    return "%s_u%d" % (name, _UID[0])


def AP(t, off, dims):
    return bass.AP(t, off, [list(d) for d in dims])


class Rot:
    def __init__(self, items):
        self.items = items
        self.i = 0

    def next(self):
        it = self.items[self.i]
        self.i = (self.i + 1) % len(self.items)
        return it


class K:
    pass


def build(n_layers=2, debug=None, stop_after=None, small_moe=False):
    nc = bass.Bass("TRN2", target_bir_lowering=False)
    k = K()
    k.nc = nc
    k.debug = debug or []
    dt_in = lambda name, shape: nc.dram_tensor(name, shape, F32, kind="ExternalInput")

    def scratch(name, shape, dt):
        kind = "ExternalOutput" if name in k.debug else "Internal"
        return nc.dram_tensor(name, shape, dt, kind=kind)

    I = {}
    I["x"] = dt_in("x", [NLAT, D])
    I["ctx"] = dt_in("ctx", [NCTX, D])
    I["cT"] = dt_in("cT", [128, 8, 2])
    I["w_ada"] = dt_in("w_ada", [2, D, 6 * D])
    I["b_ada"] = dt_in("b_ada", [2, 6 * D])
    I["g_norm1"] = dt_in("g_norm1", [2, D])
    I["g_norm2"] = dt_in("g_norm2", [2, D])
    I["w_in"] = dt_in("w_in", [2, D, 8192])
    I["w_in_sw"] = dt_in("w_in_sw", [2, D, 1024])
    I["lbT"] = dt_in("lbT", [128, 2, 2, 4])
    I["gnA"] = dt_in("gnA", [128, 2, 4])
    I["vnorm"] = dt_in("vnorm", [2, 512])
    I["wsT"] = dt_in("wsT", [2, 128, 4, 128])
    I["bs"] = dt_in("bs", [2, 512])
    I["dlam"] = dt_in("dlam", [2, 256])
    I["sublnT"] = dt_in("sublnT", [128, 2, 4])
    I["w_branch"] = dt_in("w_branch", [2, 3, 512, D])
    I["w_out"] = dt_in("w_out", [2, D, D])
    I["ffn_wg"] = dt_in("ffn_wg", [1, D, DFF])
    I["ffn_wu"] = dt_in("ffn_wu", [1, D, DFF])
    I["ffn_wd"] = dt_in("ffn_wd", [1, DFF, D])
    I["moe_router"] = dt_in("moe_router", [1, D, NEXP])
    if small_moe:
        I["moe_wg"] = dt_in("moe_wg", [1, 1, 8, 8])
        I["moe_wu"] = dt_in("moe_wu", [1, 1, 8, 8])
        I["moe_wd"] = dt_in("moe_wd", [1, 1, 8, 8])
    else:
        I["moe_wg"] = dt_in("moe_wg", [1, NEXP, D, DFFE])
        I["moe_wu"] = dt_in("moe_wu", [1, NEXP, D, DFFE])
        I["moe_wd"] = dt_in("moe_wd", [1, NEXP, DFFE, D])
    I["g_final"] = dt_in("g_final", [D])
    I["ropeC"] = dt_in("ropeC", [128, NTOK])
    I["ropeS"] = dt_in("ropeS", [128, NTOK])
    k.I = I
    k.out = nc.dram_tensor("out", [NLAT, D], F32, kind="ExternalOutput")
    k.xs = scratch("xs", [NTOK, D], F32)
    k.modrow = scratch("modrow", [2, 6, D], F32)
    k.pAq = scratch("pAq", [512, NTOK], F32)
    k.pAzf = scratch("pAzf", [512, NTOK], F32)
    k.pAzb = scratch("pAzb", [512, NTOK], F32)
    k.pAv = scratch("pAv", [NTOK, 512], BF16)
    k.pAog = scratch("pAog", [512, NTOK], BF16)
    k.pBu = scratch("pBu", [512, NTOK], BF16)
    k.pBv = scratch("pBv", [NTOK, 512], BF16)
    k.pCq = scratch("pCq", [512, NTOK], BF16)
    k.pCk = scratch("pCk", [512, NTOK], BF16)
    k.pCv = scratch("pCv", [NTOK, 512], BF16)
    k.pG = scratch("pG", [3072, NTOK], BF16)
    k.ybr = scratch("ybr", [3, 512, NTOK], BF16)
    k.h2T = scratch("h2T", [D, NTOK], BF16)
    k.B = {n: Buf(n) for n in ["xs", "modrow", "pAq", "pAzf", "pAzb", "pAv", "pAog", "pBu", "pBv", "pCq", "pCk",
                               "pCv", "pG", "ybr0", "ybr1", "ybr2", "h2T", "out"]}

    k.Bxs = [Buf("xs%d" % i) for i in range(NT)]
    with ExitStack() as top:
        S = Sched(nc, top)
        k.S = S
        k.ident = top.enter_context(nc.sbuf_tensor("ident", [128, 128], BF16))
        k.ones = top.enter_context(nc.sbuf_tensor("ones", [128, 128], BF16))
        k.sel0 = top.enter_context(nc.sbuf_tensor("sel0", [128, 128], BF16))
        k.sel1 = top.enter_context(nc.sbuf_tensor("sel1", [128, 128], BF16))
        k.onecol = top.enter_context(nc.sbuf_tensor("onecol", [128, 1], F32))
        k.Bc = Buf("consts")
        phase_consts(k)
        for layer in range(n_layers):
            last = layer == 1
            phases = [("ada", phase_ada), ("n1p", phase_n1p), ("hgrn", phase_hgrn), ("mlp", phase_mlp),
                      ("attn", phase_attn), ("merge", phase_merge), ("ffn", phase_ffn)]
            done = False
            for name, fn in phases:
                fn(k, layer, last)
                if stop_after == (layer, name):
                    done = True
                    break
            if done:
                break
        S.emit()
    return nc


def phase_consts(k):
    nc, S = k.nc, k.S
    with ExitStack() as st:
        tf = st.enter_context(nc.sbuf_tensor(U("c_tf"), [128, 128], F32))
        b = Buf()
        S.op("pool", lambda e: e.memset(tf[:], 0.0), writes=[b])
        S.op("pool", lambda e: e.affine_select(out=tf[:], in_=tf[:], pattern=[[1, 128]], compare_op=ALU.not_equal,
                                               fill=1.0, base=0, channel_multiplier=-1), reads=[b], writes=[b])
        S.op("dve", lambda e: e.tensor_copy(out=k.ident[:], in_=tf[:]), reads=[b], writes=[k.Bc])
        S.op("dve", lambda e: e.memset(k.ones[:], 1.0), writes=[k.Bc])
        S.op("dve", lambda e: e.memset(k.sel0[:], 0.0), writes=[k.Bc])
        S.op("dve", lambda e: e.memset(k.sel0[0:64, :], 1.0), writes=[k.Bc])
        S.op("dve", lambda e: e.memset(k.sel1[:], 0.0), writes=[k.Bc])
        S.op("dve", lambda e: e.memset(k.sel1[64:128, :], 1.0), writes=[k.Bc])
        S.op("dve", lambda e: e.memset(k.onecol[:], 1.0), writes=[k.Bc])
        S.dma("sp", k.xs.ap()[0:NCTX, :], k.I["ctx"].ap(), writes=[k.B["xs"]])
        for i in range(4):
            S.dma("sp", k.xs.ap()[NCTX + i * 1024:NCTX + (i + 1) * 1024, :], k.I["x"].ap()[i * 1024:(i + 1) * 1024, :],
                  writes=[k.B["xs"]])
        S.emit()


def phase_ada(k, layer, last):
    nc, S, I = k.nc, k.S, k.I
    with ExitStack() as st:
        sb = lambda n, s, d: st.enter_context(nc.sbuf_tensor(U("ad_" + n), s, d))
        cT = sb("cT", [128, 8, 2], F32)
        cs = sb("cs", [128, 8, 2], BF16)
        wts = [(sb("w%d" % i, [128, 8, 1024], BF16), Buf()) for i in range(2)]
        brow = sb("brow", [1, 6 * D], F32)
        g1 = sb("g1", [1, D], F32)
        g2 = sb("g2", [1, D], F32)
        res = [(sb("res%d" % i, [1, D], F32), Buf()) for i in range(2)]
        pss = [(st.enter_context(nc.psum_tensor(U("ad_ps%d" % i), [128, 512], F32)), Buf()) for i in range(2)]
        bc, bcs, bb = Buf(), Buf(), Buf()
        S.dma("sp", cT[:], I["cT"].ap(), writes=[bc])
        S.dma("sp", brow[:], I["b_ada"].ap()[layer:layer + 1, :], writes=[bb])
        S.dma("sp", g1[:], I["g_norm1"].ap()[layer:layer + 1, :], writes=[bb])
        S.dma("sp", g2[:], I["g_norm2"].ap()[layer:layer + 1, :], writes=[bb])
        S.op("act", lambda e: e.activation(out=cs[:], in_=cT[:], func=AF.Silu), reads=[bc], writes=[bcs])
        wsrc = I["w_ada"].ap()[layer].rearrange("(k p) n -> p k n", p=128)
        ri = 0
        for j in range(6):
            w, wb = wts[j % 2]
            S.dma("pool", w[:], wsrc[:, :, j * 1024:(j + 1) * 1024], writes=[wb])
            for which in range(2):
                r, rb = res[ri % 2]
                ri += 1
                for half in range(2):
                    ps, pb = pss[half]
                    S.op("pe", [lambda e, kk=kk, ps=ps, w=w, half=half, which=which: e.matmul(
                        ps[0:1, :], lhsT=cs[:, kk, which:which + 1], rhs=w[:, kk, half * 512:(half + 1) * 512],
                        start=(kk == 0), stop=(kk == 7)) for kk in range(8)], reads=[bcs, wb], writes=[pb])
                    S.op("dve", lambda e, ps=ps, r=r, half=half, j=j: e.tensor_tensor(
                        out=r[0:1, half * 512:(half + 1) * 512], in0=ps[0:1, :],
                        in1=brow[0:1, j * 1024 + half * 512:j * 1024 + (half + 1) * 512], op=ALU.add),
                        reads=[pb, bb], writes=[rb])
                if j in (1, 4):
                    g = g1 if j == 1 else g2
                    S.op("dve", lambda e, r=r, g=g: e.scalar_tensor_tensor(out=r[:], in0=r[:], scalar=1.0, in1=g[:],
                                                                            op0=ALU.add, op1=ALU.mult),
                         reads=[rb, bb], writes=[rb])
                S.dma("sp", k.modrow.ap()[which, j:j + 1, :], r[:], reads=[rb], writes=[k.B["modrow"]])
        S.emit()


def load_rep(k, S, tile, buf, which, j):
    S.dma("sp", tile[:], AP(k.modrow, (which * 6 + j) * D, [[0, 128], [1, D]]), reads=[k.B["modrow"]], writes=[buf])


def rms_mod(S, xt, xb, A, Ab, Bt, Bb, hb, hbb, ss, ssb, junk, junkb, D_=D):
    S.op("act", lambda e: e.activation(out=junk[:], in_=xt[:], func=AF.Square, accum_out=ss[:, 0:1]), reads=[xb],
         writes=[junkb, ssb])
    S.op("act", lambda e: e.activation(out=ss[:, 1:2], in_=ss[:, 0:1], func=AF.Ln, scale=1.0 / D_, bias=EPS),
         reads=[ssb], writes=[ssb])
    S.op("act", lambda e: e.activation(out=ss[:, 2:3], in_=ss[:, 1:2], func=AF.Exp, scale=-0.5), reads=[ssb],
         writes=[ssb])
    S.op("dve", lambda e: e.scalar_tensor_tensor(out=junk[:], in0=xt[:], scalar=ss[:, 2:3], in1=A[:], op0=ALU.mult,
                                                 op1=ALU.mult), reads=[xb, ssb, Ab], writes=[junkb])
    if Bt is None:
        return
    S.op("dve", lambda e: e.tensor_tensor(out=hb[:], in0=junk[:], in1=Bt[:], op=ALU.add), reads=[junkb, Bb],
         writes=[hbb])


def phase_n1p(k, layer, last):
    nc, S, I = k.nc, k.S, k.I
    with ExitStack() as st0:
      hT = st0.enter_context(nc.sbuf_tensor(U("np_hT"), [128, 8, NTOK], BF16))
      hTb = Buf()
      with ExitStack() as st:
        sb = lambda n, s, d: st.enter_context(nc.sbuf_tensor(U("np_" + n), s, d))
        A = [sb("A%d" % w, [128, D], F32) for w in range(2)]
        Bm = [sb("B%d" % w, [128, D], F32) for w in range(2)]
        mb = Buf()
        for w in range(2):
            load_rep(k, S, A[w], mb, w, 1)
            load_rep(k, S, Bm[w], mb, w, 0)
        xts = Rot([(sb("xt%d" % i, [128, D], F32), Buf()) for i in range(2)])
        junks = Rot([(sb("junk%d" % i, [128, D], F32), Buf()) for i in range(2)])
        hbs = Rot([(sb("hb%d" % i, [128, D], BF16), Buf()) for i in range(2)])
        sss = Rot([(sb("ss%d" % i, [128, 4], F32), Buf()) for i in range(2)])
        pts = Rot([(st.enter_context(nc.psum_tensor(U("np_pt%d" % i), [128, 512], BF16)), Buf()) for i in range(2)])
        for i in range(NT):
            w = 1 if i < 2 else 0
            xt, xb = xts.next()
            junk, jb = junks.next()
            hb, hbb = hbs.next()
            ss, ssb = sss.next()
            S.dma("sp", xt[:], k.xs.ap()[i * 128:(i + 1) * 128, :], reads=[k.B["xs"]], writes=[xb])
            rms_mod(S, xt, xb, A[w], mb, Bm[w], mb, hb, hbb, ss, ssb, junk, jb)
            for half in range(2):
                pt, ptb = pts.next()
                S.op("pe", [lambda e, pt=pt, hb=hb, j=j, half=half: e.transpose(
                    out=pt[:, j * 128:(j + 1) * 128], in_=hb[:, (half * 4 + j) * 128:(half * 4 + j + 1) * 128],
                    identity=k.ident[:]) for j in range(4)], reads=[hbb, k.Bc], writes=[ptb])
                eng = "act" if half == 0 else "dve"
                outap = hT[:, half * 4:half * 4 + 4, i * 128:(i + 1) * 128]
                inap = AP(pt, 0, [[512, 128], [128, 4], [1, 128]])
                if eng == "act":
                    S.op("act", lambda e, o=outap, a=inap: e.activation(out=o, in_=a, func=AF.Copy), reads=[ptb],
                         writes=[hTb])
                else:
                    S.op("dve", lambda e, o=outap, a=inap: e.tensor_copy(out=o, in_=a), reads=[ptb], writes=[hTb])

        S.emit()
      with ExitStack() as st:
        sb = lambda n, s, d: st.enter_context(nc.sbuf_tensor(U("nq_" + n), s, d))
        junks = Rot([(sb("junk%d" % i, [128, D], F32), Buf()) for i in range(2)])
        sss = Rot([(sb("ss%d" % i, [128, 4], F32), Buf()) for i in range(2)])
        ropeC = sb("ropeC", [128, NTOK], F32)
        ropeS = sb("ropeS", [128, NTOK], F32)
        rb = Buf()
        S.dma("sp", ropeC[:], I["ropeC"].ap(), writes=[rb])
        S.dma("sp", ropeS[:], I["ropeS"].ap(), writes=[rb])
        vn = sb("vn", [128, 512], F32)
        S.dma("sp", vn[:], AP(I["vnorm"], layer * 512, [[0, 128], [1, 512]]), writes=[rb])
        wts = Rot([(sb("w%d" % i, [128, 8, 1024], BF16), Buf()) for i in range(2)])
        wsw = sb("wsw", [128, 8, 512], BF16)
        wswb = Buf()
        stg32 = Rot([(sb("s32_%d" % i, [128, NTOK], F32), Buf()) for i in range(2)])
        pps = Rot([(st.enter_context(nc.psum_tensor(U("nq_pp%d" % i), [128, 512], F32)), Buf()) for i in range(4)])
        wsrc = I["w_in"].ap()[layer].rearrange("(k p) n -> p k n", p=128)
        wswsrc = I["w_in_sw"].ap()[layer].rearrange("(k p) n -> p k n", p=128)

        def fm_mm(ps, pb, w, wb, c0, t0, W):
            S.op("pe", [lambda e, kk=kk: e.matmul(ps[:, 0:W], lhsT=w[:, kk, c0:c0 + 128], rhs=hT[:, kk, t0:t0 + W],
                                                   start=(kk == 0), stop=(kk == 7)) for kk in range(8)],
                 reads=[wb, hTb], writes=[pb])

        def fm_job(c_in, ncols, dst, dstbuf, func, out_dt):
            for g0 in range(0, ncols, 1024):
                gw = min(1024, ncols - g0)
                w, wb = wts.next()
                S.dma("pool", w[:, :, 0:gw], wsrc[:, :, c_in + g0:c_in + g0 + gw], writes=[wb])
                for cc in range(gw // 128):
                    stg, sgb = stg32.next()
                    if out_dt == BF16:
                        stgv = stg[:].bitcast(BF16)[:, 0:NTOK]
                    else:
                        stgv = stg[:]
                    for (t0, W) in BLOCKS:
                        ps, pb = pps.next()
                        fm_mm(ps, pb, w, wb, cc * 128, t0, W)
                        if func is None:
                            S.op("dve", lambda e, ps=ps, o=stgv[:, t0:t0 + W], W=W: e.tensor_copy(out=o, in_=ps[:, 0:W]),
                                 reads=[pb], writes=[sgb])
                        else:
                            S.op("act", lambda e, ps=ps, o=stgv[:, t0:t0 + W], W=W: e.activation(out=o, in_=ps[:, 0:W],
                                                                                             func=func),
                                 reads=[pb], writes=[sgb])
                    r0 = g0 + cc * 128
                    S.dma("sp", dst[r0:r0 + 128, :], stgv, reads=[sgb], writes=[dstbuf])

        def tm_job(c_in, dst, dstbuf, mode):
            w, wb = wts.next()
            S.dma("pool", w[:, :, 0:512], wsrc[:, :, c_in:c_in + 512], writes=[wb])
            stg, sgb = stg32.next()
            stgv = AP(stg, 0, [[NTOK, 128], [1, NTOK]]).bitcast(BF16)
            for i in range(NT):
                ps, pb = pps.next()
                S.op("pe", [lambda e, kk=kk, ps=ps, i=i: e.matmul(ps[:], lhsT=hT[:, kk, i * 128:(i + 1) * 128],
                                                                  rhs=w[:, kk, 0:512], start=(kk == 0), stop=(kk == 7))
                            for kk in range(8)], reads=[wb, hTb], writes=[pb])
                o = stgv[:, (i % 16) * 512:(i % 16 + 1) * 512]
                if mode == "copy":
                    S.op("act", lambda e, ps=ps, o=o: e.activation(out=o, in_=ps[:], func=AF.Copy), reads=[pb],
                         writes=[sgb])
                else:
                    gl, glb = junks.next()
                    ss, ssb = sss.next()
                    S.op("act", lambda e, ps=ps, gl=gl: e.activation(out=gl[:, 0:512], in_=ps[:], func=AF.Gelu_apprx_tanh),
                         reads=[pb], writes=[glb])
                    S.op("act", lambda e, gl=gl, ss=ss: e.activation(out=gl[:, 512:1024], in_=gl[:, 0:512], func=AF.Square,
                                                                     accum_out=ss[:, 0:1]), reads=[glb], writes=[glb, ssb])
                    S.op("act", lambda e, ss=ss: e.activation(out=ss[:, 1:2], in_=ss[:, 0:1], func=AF.Ln, scale=1.0 / 512,
                                                              bias=EPS), reads=[ssb], writes=[ssb])
                    S.op("act", lambda e, ss=ss: e.activation(out=ss[:, 2:3], in_=ss[:, 1:2], func=AF.Exp, scale=-0.5),
                         reads=[ssb], writes=[ssb])
                    S.op("dve", lambda e, gl=gl, ss=ss, o=o: e.scalar_tensor_tensor(
                        out=o, in0=gl[:, 0:512], scalar=ss[:, 2:3], in1=vn[:], op0=ALU.mult, op1=ALU.mult),
                        reads=[glb, ssb, rb], writes=[sgb])
                if i % 16 == 15 or i == NT - 1:
                    i0 = (i // 16) * 16
                    n = i - i0 + 1
                    S.dma("sp", dst.ap()[i0 * 128:(i + 1) * 128, :].rearrange("(n p) c -> p n c", p=128),
                          AP(stg, 0, [[NTOK, 128], [1, NTOK]]).bitcast(BF16)[:, 0:n * 512].rearrange("p (n c) -> p n c", c=512),
                          reads=[sgb], writes=[dstbuf])
                    if i != NT - 1:
                        stg, sgb = stg32.next()
                        stgv = AP(stg, 0, [[NTOK, 128], [1, NTOK]]).bitcast(BF16)

        def rope_job(c_in, sw0, dst, dstbuf):
            w, wb = wts.next()
            S.dma("pool", w[:, :, 0:512], wsrc[:, :, c_in:c_in + 512], writes=[wb])
            S.dma("pool", wsw[:], wswsrc[:, :, sw0:sw0 + 512], writes=[wswb])
            t1s = Rot([(junks.items[0][0], junks.items[0][1]), (junks.items[1][0], junks.items[1][1])])
            for cc in range(4):
                stg, sgb = stg32.next()
                stgv = stg[:].bitcast(BF16)[:, 0:NTOK]
                for (t0, W) in BLOCKS:
                    ps, pb = pps.next()
                    ps2, pb2 = pps.next()
                    fm_mm(ps, pb, w, wb, cc * 128, t0, W)
                    fm_mm(ps2, pb2, wsw, wswb, cc * 128, t0, W)
                    t1, t1b = t1s.next()
                    S.op("dve", lambda e, ps=ps, t1=t1, t0=t0, W=W: e.tensor_tensor(
                        out=t1[:, 0:W], in0=ps[:, 0:W], in1=ropeC[:, t0:t0 + W], op=ALU.mult), reads=[pb, rb], writes=[t1b])
                    S.op("dve", lambda e, ps2=ps2, t1=t1, t0=t0, W=W: e.tensor_tensor(
                        out=t1[:, 512:512 + W], in0=ps2[:, 0:W], in1=ropeS[:, t0:t0 + W], op=ALU.mult), reads=[pb2, rb],
                        writes=[t1b])
                    S.op("dve", lambda e, t1=t1, o=stgv[:, t0:t0 + W], W=W: e.tensor_tensor(
                        out=o, in0=t1[:, 0:W], in1=t1[:, 512:512 + W], op=ALU.add), reads=[t1b], writes=[sgb])
                S.dma("sp", dst[cc * 128:(cc + 1) * 128, :], stgv, reads=[sgb], writes=[dstbuf])

        B = k.B
        fm_job(0, 512, k.pAq.ap(), B["pAq"], None, F32)
        fm_job(512, 512, k.pAzf.ap(), B["pAzf"], None, F32)
        fm_job(1024, 512, k.pAzb.ap(), B["pAzb"], None, F32)
        tm_job(1536, k.pAv, B["pAv"], "copy")
        fm_job(2048, 512, k.pAog.ap(), B["pAog"], AF.Silu, BF16)
        fm_job(2560, 512, k.pBu.ap(), B["pBu"], AF.Gelu_apprx_tanh, BF16)
        tm_job(3072, k.pBv, B["pBv"], "gelu_rms")
        rope_job(3584, 0, k.pCq.ap(), B["pCq"])
        rope_job(4096, 512, k.pCk.ap(), B["pCk"])
        tm_job(4608, k.pCv, B["pCv"], "copy")
        fm_job(5120, 3072, k.pG.ap(), B["pG"], AF.Sigmoid, BF16)
        S.emit()


def head_readout(k, S, st, prefix, src, srcb, t0, W, scale_col, scb, mul_tile, mulb, dst, dstb, pss, sq_rot, tmp_rot):
    sq, sqb = sq_rot.next()
    tmp, tb = tmp_rot.next()
    ps, pb = pss.next()
    S.op("act", lambda e: e.activation(out=sq[:, 0:W], in_=src, func=AF.Square), reads=[srcb], writes=[sqb])
    S.op("pe", lambda e: e.matmul(ps[:, 0:W], lhsT=k.ones[:], rhs=sq[:, 0:W], start=True, stop=True), reads=[sqb, k.Bc],
         writes=[pb])
    S.op("act", lambda e: e.activation(out=tmp[:, 0:W], in_=ps[:, 0:W], func=AF.Ln, scale=1.0 / 128, bias=EPS),
         reads=[pb], writes=[tb])
    S.op("act", lambda e: e.activation(out=tmp[:, 0:W], in_=tmp[:, 0:W], func=AF.Exp, scale=-0.5), reads=[tb],
         writes=[tb])
    if mul_tile is None:
        S.op("dve", lambda e: e.scalar_tensor_tensor(out=dst, in0=src, scalar=scale_col, in1=tmp[:, 0:W],
                                                     op0=ALU.mult, op1=ALU.mult), reads=[srcb, scb, tb], writes=[dstb])
    else:
        S.op("dve", lambda e: e.scalar_tensor_tensor(out=tmp[:, 0:W], in0=src, scalar=scale_col, in1=tmp[:, 0:W],
                                                     op0=ALU.mult, op1=ALU.mult), reads=[srcb, scb, tb], writes=[tb])
        S.op("dve", lambda e: e.tensor_tensor(out=dst, in0=tmp[:, 0:W], in1=mul_tile, op=ALU.mult), reads=[tb, mulb],
             writes=[dstb])


def phase_hgrn(k, layer, last):
    nc, S, I = k.nc, k.S, k.I
    with ExitStack() as st:
        sb = lambda n, s, d: st.enter_context(nc.sbuf_tensor(U("hg_" + n), s, d))
        T1 = sb("T1", [128, NTOK], F32)
        T2 = sb("T2", [128, NTOK], F32)
        Gp = sb("Gp", [128, NTOK + 1], F32)
        T4 = sb("T4", [128, NTOK], F32)
        q1 = sb("q1", [128, NTOK], BF16)
        k1 = sb("k1", [128, NTOK], BF16)
        k1z = sb("k1z", [128, NTOK], BF16)
        q2 = sb("q2", [128, NTOK], BF16)
        k2f = sb("k2f", [128, NTOK], BF16)
        k2T = sb("k2T", [64, NCH, 128], BF16)
        vS = sb("vS", [64, NCH, 128], BF16)
        oacc = sb("oacc", [128, NTOK], F32)
        dec = sb("dec", [128, NCH], F32)
        cqk = sb("cqk", [128, 2, NCH], F32)
        S32p = [(sb("S32_%d" % i, [128, 128], F32), Buf()) for i in range(2)]
        SbA = sb("SbA", [128, NCH * 128], BF16)
        slotb = [Buf() for _ in range(NCH + 1)]
        lbt = sb("lbt", [128, 16], F32)
        lbe = sb("lbe", [128, 16], F32)
        lbv = sb("lbv", [128, 8, 2], F32)
        gn = sb("gn", [128, 8], F32)
        maskF = sb("maskF", [64, 64], F32)
        maskB = sb("maskB", [64, 64], F32)
        attTs = Rot([(sb("attT%d" % i, [64, 512], BF16), Buf()) for i in range(3)])
        sqs = Rot([(sb("sq%d" % i, [128, 512], BF16), Buf()) for i in range(2)])
        tmps = Rot([(sb("tmp%d" % i, [128, 512], F32), Buf()) for i in range(2)])
        b = {n: Buf(n) for n in "T1 T2 Gp T4 q1 k1 k1z q2 k2f k2T vS oacc dec S32 Sb lb gn mask cqk".split()}
        pa = Rot([(st.enter_context(nc.psum_tensor(U("hg_pa%d" % i), [128, 512], F32)), Buf()) for i in range(2)])
        po = Rot([(st.enter_context(nc.psum_tensor(U("hg_po%d" % i), [128, 512], F32)), Buf()) for i in range(2)])
        psS = Rot([(st.enter_context(nc.psum_tensor(U("hg_ps%d" % i), [128, 512], F32)), Buf()) for i in range(2)])
        ptr = Rot([(st.enter_context(nc.psum_tensor(U("hg_pt%d" % i), [128, 512], BF16)), Buf()) for i in range(2)])

        S.dma("sp", lbt[:], I["lbT"].ap().rearrange("p a b c -> p (a b c)"), writes=[b["lb"]])
        S.dma("sp", gn[:], I["gnA"].ap().rearrange("p a b -> p (a b)"), writes=[b["gn"]])
        S.op("act", lambda e: e.activation(out=lbe[:], in_=lbt[:], func=AF.Exp), reads=[b["lb"]], writes=[b["lb"]])
        for d_ in range(2):
            e0 = lbe[:, d_ * 8:d_ * 8 + 4]
            e1 = lbe[:, d_ * 8 + 4:d_ * 8 + 8]
            tot = lbt[:, d_ * 8:d_ * 8 + 4]
            S.op("dve", lambda e, e0=e0, e1=e1, tot=tot: e.tensor_tensor(out=tot, in0=e0, in1=e1, op=ALU.add),
                 reads=[b["lb"]], writes=[b["lb"]])
            S.op("dve", lambda e, tot=tot: e.reciprocal(out=tot, in_=tot), reads=[b["lb"]], writes=[b["lb"]])
            num = lbt[:, d_ * 8 + 4:d_ * 8 + 8]
            if layer == 0:
                S.op("dve", lambda e, num=num, e0=e0: e.tensor_tensor(out=num, in0=e0, in1=e0, op=ALU.subtract),
                     reads=[b["lb"]], writes=[b["lb"]])
            else:
                S.op("dve", lambda e, num=num, e1=e1: e.tensor_copy(out=num, in_=e1), reads=[b["lb"]], writes=[b["lb"]])
            lbcol = AP(lbv, d_ * 8, [[16, 128], [2, 4]])
            omcol = AP(lbv, d_ * 8 + 1, [[16, 128], [2, 4]])
            S.op("dve", lambda e, lbcol=lbcol, num=num, tot=tot: e.tensor_tensor(out=lbcol, in0=num, in1=tot, op=ALU.mult),
                 reads=[b["lb"]], writes=[b["lb"]])
            S.op("dve", lambda e, lbcol=lbcol, omcol=omcol: e.tensor_scalar(out=omcol, in0=lbcol, scalar1=-1.0, scalar2=1.0,
                                                                            op0=ALU.mult, op1=ALU.add),
                 reads=[b["lb"]], writes=[b["lb"]])
        S.op("pool", lambda e: e.memset(maskF[:], 1.0), writes=[b["mask"]])
        S.op("pool", lambda e: e.affine_select(out=maskF[:], in_=maskF[:], pattern=[[1, 64]], compare_op=ALU.is_ge,
                                               fill=0.0, base=0, channel_multiplier=-1), reads=[b["mask"]],
             writes=[b["mask"]])
        S.op("pool", lambda e: e.memset(maskB[:], 1.0), writes=[b["mask"]])
        S.op("pool", lambda e: e.affine_select(out=maskB[:], in_=maskB[:], pattern=[[-1, 64]], compare_op=ALU.is_ge,
                                               fill=0.0, base=0, channel_multiplier=1), reads=[b["mask"]],
             writes=[b["mask"]])
        S.op("dve", lambda e: e.memset(Gp[:, 0:1], 0.0), writes=[b["Gp"]])

        def view(t, off, W=NTOK + 1):
            return AP(t, off, [[W, 128], [64, NCH], [1, 64]])

        def anchor(off):
            return AP(Gp, off, [[NTOK + 1, 128], [64, NCH], [0, 64]])

        def v3(t):
            return AP(t, 0, [[NTOK, 128], [64, NCH], [1, 64]])

        def early_stage(stage, h, d_):
            zsrc = k.pAzf if d_ == 0 else k.pAzb
            zb_ = k.B["pAzf"] if d_ == 0 else k.B["pAzb"]
            lbc = lbv[:, d_ * 4 + h, 0:1]
            omc = lbv[:, d_ * 4 + h, 1:2]
            if stage == 0:
                if d_ == 0:
                    S.dma("sp", T4[:], k.pAq.ap()[h * 128:(h + 1) * 128, :], reads=[k.B["pAq"]], writes=[b["T4"]])
                S.dma("sp", T1[:], zsrc.ap()[h * 128:(h + 1) * 128, :], reads=[zb_], writes=[b["T1"]])
                S.op("act", lambda e: e.activation(out=T1[:], in_=T1[:], func=AF.Sigmoid), reads=[b["T1"]], writes=[b["T1"]])
            elif stage == 1:
                S.op("dve", lambda e: e.tensor_scalar(out=T1[:], in0=T1[:], scalar1=omc, scalar2=lbc, op0=ALU.mult,
                                                      op1=ALU.add), reads=[b["T1"], b["lb"]], writes=[b["T1"]])
                S.op("dve", lambda e: e.tensor_scalar(out=T2[:], in0=T1[:], scalar1=-1.0, scalar2=1.0, op0=ALU.mult,
                                                      op1=ALU.add), reads=[b["T1"]], writes=[b["T2"]])
                S.op("act", lambda e: e.activation(out=T1[:], in_=T1[:], func=AF.Ln), reads=[b["T1"]], writes=[b["T1"]])
            else:
                S.op("dve", lambda e: e.tensor_tensor_scan(out=Gp[:, 1:NTOK + 1],
                                                           data0=AP(k.onecol, 0, [[1, 128], [0, NTOK]]), data1=T1[:],
                                                           initial=0.0, op0=ALU.mult, op1=ALU.add),
                     reads=[b["T1"], k.Bc], writes=[b["Gp"]])

        hd_seq = [(h_, dd) for h_ in range(4) for dd in range(2)]
        for st_ in range(3):
            early_stage(st_, 0, 0)
        for h in range(4):
            S.dma("sp", vS[:], k.pAv.ap().rearrange("(c s) d -> s c d", s=64)[:, :, h * 128:(h + 1) * 128],
                  reads=[k.B["pAv"]], writes=[b["vS"]])
            for d_ in range(2):
                sg = 1.0 if d_ == 0 else -1.0
                hd_idx = h * 2 + d_
                nxt_hd = hd_seq[hd_idx + 1] if hd_idx + 1 < len(hd_seq) else None
                eoff = 1 if d_ == 0 else 0
                E = view(Gp, eoff)
                a_mid = anchor(32)
                a_q2 = anchor(0) if d_ == 0 else anchor(64)
                a_k2 = anchor(64) if d_ == 0 else anchor(0)

                def prep(anch, scale, src, srcb, dst, dstb, E=E):
                    S.op("dve", lambda e: e.tensor_tensor(out=v3(T1), in0=E, in1=anch, op=ALU.subtract), reads=[b["Gp"]],
                         writes=[b["T1"]])
                    S.op("act", lambda e: e.activation(out=T1[:], in_=T1[:], func=AF.Exp, scale=scale), reads=[b["T1"]],
                         writes=[b["T1"]])
                    S.op("dve", lambda e: e.tensor_tensor(out=dst[:], in0=src[:], in1=T1[:], op=ALU.mult),
                         reads=[b["T1"], srcb], writes=[dstb])

                prep(a_mid, sg, T4, b["T4"], q1, b["q1"])
                prep(a_mid, -sg, T2, b["T2"], k1, b["k1"])
                oq = 0 if d_ == 0 else 64
                ok_ = 64 if d_ == 0 else 0
                gmid = AP(Gp, 32, [[NTOK + 1, 128], [64, NCH]])
                S.op("dve", lambda e, oq=oq: e.tensor_tensor(out=cqk[:, 0, :], in0=gmid, in1=AP(Gp, oq, [[NTOK + 1, 128], [64, NCH]]),
                                                             op=ALU.subtract), reads=[b["Gp"]], writes=[b["cqk"]])
                S.op("dve", lambda e, ok_=ok_: e.tensor_tensor(out=cqk[:, 1, :], in0=gmid, in1=AP(Gp, ok_, [[NTOK + 1, 128], [64, NCH]]),
                                                               op=ALU.subtract), reads=[b["Gp"]], writes=[b["cqk"]])
                S.op("act", lambda e, sg=sg: e.activation(out=cqk[:, 0, :], in_=cqk[:, 0, :], func=AF.Exp, scale=sg),
                     reads=[b["cqk"]], writes=[b["cqk"]])
                S.op("act", lambda e, sg=sg: e.activation(out=cqk[:, 1, :], in_=cqk[:, 1, :], func=AF.Exp, scale=-sg),
                     reads=[b["cqk"]], writes=[b["cqk"]])

                def v3b(t):
                    return AP(t, 0, [[NTOK, 128], [64, NCH], [1, 64]])

                S.op("dve", lambda e: e.tensor_tensor(out=v3b(q2), in0=v3b(q1), in1=AP(cqk, 0, [[2 * NCH, 128], [1, NCH], [0, 64]]),
                                                      op=ALU.mult), reads=[b["q1"], b["cqk"]], writes=[b["q2"]])
                S.op("dve", lambda e: e.tensor_tensor(out=v3b(k2f), in0=v3b(k1), in1=AP(cqk, NCH, [[2 * NCH, 128], [1, NCH], [0, 64]]),
                                                      op=ALU.mult), reads=[b["k1"], b["cqk"]], writes=[b["k2f"]])
                zoff, koff = (32, 0) if d_ == 0 else (0, 32)
                zv = AP(k1z, zoff, [[NTOK, 128], [64, NCH], [1, 32]])
                kv_o = AP(k1z, koff, [[NTOK, 128], [64, NCH], [1, 32]])
                kv_i = AP(k1, koff, [[NTOK, 128], [64, NCH], [1, 32]])
                S.op("pool", lambda e, zv=zv: e.memset(zv, 0.0), writes=[b["k1z"]])
                S.op("pool", lambda e, kv_o=kv_o, kv_i=kv_i: e.tensor_copy(out=kv_o, in_=kv_i), reads=[b["k1"]],
                     writes=[b["k1z"]])
                S.op("dve", lambda e: e.tensor_tensor(out=dec[:], in0=AP(Gp, 64, [[NTOK + 1, 128], [64, NCH]]),
                                                      in1=AP(Gp, 0, [[NTOK + 1, 128], [64, NCH]]), op=ALU.subtract),
                     reads=[b["Gp"]], writes=[b["dec"]])
                S.op("act", lambda e: e.activation(out=dec[:], in_=dec[:], func=AF.Exp), reads=[b["dec"]],
                     writes=[b["dec"]])
                for c4 in range(NCH // 4):
                    pt, ptb = ptr.next()
                    S.op("pe", [lambda e, pt=pt, j=j, c4=c4: e.transpose(
                        out=pt[0:64, j * 128:(j + 1) * 128], in_=k2f[:, (c4 * 4 + j) * 64:(c4 * 4 + j + 1) * 64],
                        identity=k.ident[:]) for j in range(4)], reads=[b["k2f"], k.Bc], writes=[ptb])
                    o = k2T[:, c4 * 4:c4 * 4 + 4, :]
                    a = AP(pt, 0, [[512, 64], [128, 4], [1, 128]])
                    if c4 % 2 == 0:
                        S.op("act", lambda e, o=o, a=a: e.activation(out=o, in_=a, func=AF.Copy), reads=[ptb],
                             writes=[b["k2T"]])
                    else:
                        S.op("dve", lambda e, o=o, a=a: e.tensor_copy(out=o, in_=a), reads=[ptb], writes=[b["k2T"]])
                order = list(range(NCH)) if d_ == 0 else [3, 2, 1, 0] + list(range(NCH - 1, 3, -1))
                mask = maskF if d_ == 0 else maskB
                S.op("dve", lambda e: e.memset(S32p[0][0][:], 0.0), writes=[S32p[0][1]])
                S.op("dve", lambda e: e.memset(SbA[:, 0:128], 0.0), writes=[slotb[0]])

                granges = [(0, 4)] + [(4 + 8 * g, 8) for g in range(8)]
                if d_ == 0:
                    gorder = granges
                else:
                    gorder = [granges[0]] + granges[:0:-1]
                pos_of = {c: i for i, c in enumerate(order)}

                def att_group(g0, n, mask=mask, d_=d_):
                    pA, pAb = pa.next()
                    safe, unsafe = (32, 0) if d_ == 0 else (0, 32)
                    fns = []
                    for s_ in range(n):
                        cs_ = (g0 + s_) * 64
                        fns.append(lambda e, cs_=cs_, s_=s_: e.matmul(pA[0:64, s_ * 64 + safe:s_ * 64 + safe + 32],
                                                                      lhsT=k1[:, cs_:cs_ + 64],
                                                                      rhs=q1[:, cs_ + safe:cs_ + safe + 32], start=True, stop=True))
                        fns.append(lambda e, cs_=cs_, s_=s_: e.matmul(pA[0:64, s_ * 64 + unsafe:s_ * 64 + unsafe + 32],
                                                                      lhsT=k1z[:, cs_:cs_ + 64],
                                                                      rhs=q1[:, cs_ + unsafe:cs_ + unsafe + 32], start=True,
                                                                      stop=True))
                    S.op("pe", fns, reads=[b["k1"], b["k1z"], b["q1"]], writes=[pAb])
                    aT, aTb = attTs.next()
                    S.op("dve", lambda e: e.tensor_tensor(out=AP(aT, 0, [[512, 64], [64, n], [1, 64]]),
                                                          in0=AP(pA, 0, [[512, 64], [64, n], [1, 64]]),
                                                          in1=AP(mask, 0, [[64, 64], [0, n], [1, 64]]), op=ALU.mult),
                         reads=[pAb, b["mask"]], writes=[aTb])
                    return aT, aTb

                def state_step(i, c):
                    pS, pSb = psS.next()
                    src, srcb = S32p[i % 2]
                    dst, dstb = S32p[(i + 1) % 2]
                    S.op("pe", lambda e: e.matmul(pS[:, 0:128], lhsT=k2T[:, c, :], rhs=vS[:, c, :], start=True, stop=True),
                         reads=[b["k2T"], b["vS"]], writes=[pSb])
                    S.op("dve", lambda e: e.scalar_tensor_tensor(out=dst[:], in0=src[:], scalar=dec[:, c:c + 1],
                                                                 in1=pS[:, 0:128], op0=ALU.mult, op1=ALU.add),
                         reads=[pSb, srcb, b["dec"]], writes=[dstb])
                    S.op("act", lambda e: e.activation(out=SbA[:, (i + 1) * 128:(i + 2) * 128], in_=dst[:], func=AF.Copy),
                         reads=[dstb], writes=[slotb[i + 1]])

                def out_group(g0, n, aT, aTb, d_=d_):
                    pO, pOb = po.next()
                    fns = []
                    rd = [b["vS"], aTb, b["q2"]]
                    for s_ in range(n):
                        c = g0 + s_
                        i = pos_of[c]
                        cs_ = c * 64
                        rd.append(slotb[i])
                        fns.append(lambda e, c=c, s_=s_: e.matmul(pO[:, s_ * 64:(s_ + 1) * 64], lhsT=vS[:, c, :],
                                                                  rhs=aT[:, s_ * 64:(s_ + 1) * 64], start=True, stop=False))
                        fns.append(lambda e, i=i, s_=s_, cs_=cs_: e.matmul(pO[:, s_ * 64:(s_ + 1) * 64],
                                                                           lhsT=SbA[:, i * 128:(i + 1) * 128],
                                                                           rhs=q2[:, cs_:cs_ + 64], start=False, stop=True))
                    S.op("pe", fns, reads=rd, writes=[pOb])
                    c0_, c1_ = g0 * 64, (g0 + n) * 64
                    if d_ == 0:
                        S.op("act", lambda e: e.activation(out=oacc[:, c0_:c1_], in_=pO[:, 0:n * 64], func=AF.Copy),
                             reads=[pOb], writes=[b["oacc"]])
                    else:
                        S.op("dve", lambda e: e.tensor_tensor(out=oacc[:, c0_:c1_], in0=oacc[:, c0_:c1_], in1=pO[:, 0:n * 64],
                                                              op=ALU.add), reads=[pOb, b["oacc"]], writes=[b["oacc"]])

                n_ = len(order)
                pend = None
                for gidx, (g0, n) in enumerate(gorder):
                    aT_, aTb_ = att_group(g0, n)
                    cl = list(range(g0, g0 + n)) if d_ == 0 else list(range(g0 + n - 1, g0 - 1, -1))
                    for c in cl:
                        i = pos_of[c]
                        if i + 1 < n_:
                            state_step(i, c)
                    if pend is not None:
                        out_group(*pend)
                    pend = (g0, n, aT_, aTb_)
                    if nxt_hd is not None and gidx in (0, 2, 4):
                        early_stage(gidx // 2, nxt_hd[0], nxt_hd[1])
                out_group(*pend)
            if True:
                og = k1
                S.dma("sp", og[:], k.pAog.ap()[h * 128:(h + 1) * 128, :], reads=[k.B["pAog"]], writes=[b["k1"]])
                for (t0, W) in BLOCKS:
                    if last and t0 == 0:
                        continue
                    head_readout(k, S, st, "hg", oacc[:, t0:t0 + W], b["oacc"], t0, W, gn[:, layer * 4 + h:layer * 4 + h + 1],
                                 b["gn"], og[:, t0:t0 + W], b["k1"], q1[:, t0:t0 + W], b["q1"], pa, sqs, tmps)
                S.dma("pool", k.ybr.ap()[0, h * 128:(h + 1) * 128, :], q1[:], reads=[b["q1"]], writes=[k.B["ybr0"]])
        S.emit()


def phase_mlp(k, layer, last):
    nc, S, I = k.nc, k.S, k.I
    with ExitStack() as st:
        sb = lambda n, s, d: st.enter_context(nc.sbuf_tensor(U("ml_" + n), s, d))
        uT = sb("uT", [128, 4, NTOK], BF16)
        vB = sb("vB", [128, NT, 512], BF16)
        yb = sb("yb", [128, 4, NTOK], BF16)
        wsT32 = sb("wsT32", [128, 4, 128], F32)
        wsT = sb("wsT", [128, 4, 128], BF16)
        bsr = sb("bsr", [128, 512], F32)
        tmps = Rot([(sb("tmp%d" % i, [128, 512], F32), Buf()) for i in range(2)])
        pms = Rot([(st.enter_context(nc.psum_tensor(U("ml_pm%d" % i), [128, 512], F32)), Buf()) for i in range(2)])
        bu, bv, by, bw, bb = Buf(), Buf(), Buf(), Buf(), Buf()
        S.dma("sp", uT[:], k.pBu.ap().rearrange("(g d) t -> d g t", d=128), reads=[k.B["pBu"]], writes=[bu])
        S.dma("sp", vB[:], k.pBv.ap().rearrange("(n p) c -> p n c", p=128), reads=[k.B["pBv"]], writes=[bv])
        S.dma("sp", wsT32[:], I["wsT"].ap()[layer], writes=[bw])
        S.dma("sp", bsr[:], AP(I["bs"], layer * 512, [[0, 128], [1, 512]]), writes=[bb])
        S.op("dve", lambda e: e.tensor_copy(out=wsT[:], in_=wsT32[:]), reads=[bw], writes=[bw])
        for i in range(NT):
            if last and i < 2:
                continue
            pm, pmb = pms.next()
            for g in range(4):
                S.op("pe", lambda e, g=g, pm=pm, i=i: e.matmul(pm[:, g * 128:(g + 1) * 128], lhsT=vB[:, i, g * 128:(g + 1) * 128],
                                                               rhs=wsT[:, g, :], start=True, stop=True), reads=[bv, bw],
                     writes=[pmb])
            tmp, tb = tmps.next()
            S.op("dve", lambda e, pm=pm, tmp=tmp: e.tensor_tensor(out=tmp[:], in0=pm[:], in1=bsr[:], op=ALU.add),
                 reads=[pmb, bb], writes=[tb])
            S.op("dve", lambda e, tmp=tmp, i=i: e.tensor_tensor(
                out=yb[:, :, i * 128:(i + 1) * 128], in0=AP(tmp, 0, [[512, 128], [128, 4], [1, 128]]),
                in1=uT[:, :, i * 128:(i + 1) * 128], op=ALU.mult), reads=[tb, bu], writes=[by])
        if last:
            S.op("dve", lambda e: e.memset(yb[:, :, 0:256], 0.0), writes=[by])
        S.dma("sp", k.ybr.ap()[1].rearrange("(g d) t -> d g t", d=128), yb[:], reads=[by], writes=[k.B["ybr1"]])
        S.emit()


def phase_attn(k, layer, last):
    nc, S, I = k.nc, k.S, k.I
    lam_init = 0.8 - 0.6 * math.exp(-0.3 * layer)
    with ExitStack() as st:
        sb = lambda n, s, d: st.enter_context(nc.sbuf_tensor(U("at_" + n), s, d))
        qT = sb("qT", [128, NTOK], BF16)
        kT = sb("kT", [128, NTOK], BF16)
        vC = sb("vC", [128, NT, 128], BF16)
        yc = sb("yc", [128, NTOK], BF16)
        dl = sb("dl", [128, 256], F32)
        dl2 = sb("dl2", [128, 128], F32)
        lam = sb("lam", [128, 4], F32)
        sl = sb("sl", [128, 8], F32)
        slc = sb("slc", [128, 8], F32)
        mx = sb("mx", [128, 2, 2, 16], F32)
        nb = sb("nb", [128, 8], F32)
        pTs = Rot([(sb("pT%d" % i, [128, 1024], BF16), Buf()) for i in range(4)])
        t2s = Rot([(sb("t2_%d" % i, [128, 512], BF16), Buf()) for i in range(6)])
        accPs = [(sb("accP%d" % i, [128, 512], F32), Buf()) for i in range(2)]
        ones32 = sb("ones32", [128, 128], F32)
        sqs = Rot([(sb("sq%d" % i, [128, 512], BF16), Buf()) for i in range(2)])
        tmps = Rot([(sb("tmp%d" % i, [128, 512], F32), Buf()) for i in range(2)])
        o0 = sb("o0", [128, 512], F32)
        o1 = sb("o1", [128, 512], F32)
        rl = sb("rl", [128, 512], F32)
        b = {n: Buf(n) for n in "qT kT vC yc dl lam sl mx nb o0 o1 rl ones32".split()}
        pss = Rot([(st.enter_context(nc.psum_tensor(U("at_ps%d" % i), [128, 1024], F32)), Buf()) for i in range(2)])
        pos = [(st.enter_context(nc.psum_tensor(U("at_po%d" % i), [128, 512], F32)), Buf()) for i in range(2)]
        prs = Rot([(st.enter_context(nc.psum_tensor(U("at_pr%d" % i), [128, 512], F32)), Buf()) for i in range(2)])

        S.dma("sp", dl[:], AP(I["dlam"], layer * 256, [[0, 128], [1, 256]]), writes=[b["dl"]])
        S.dma("sp", sl[:], I["sublnT"].ap().rearrange("p a b -> p (a b)"), writes=[b["sl"]])
        S.op("dve", lambda e: e.tensor_tensor(out=AP(dl2, 0, [[128, 128], [64, 2], [1, 64]]),
                                              in0=AP(dl, 0, [[256, 128], [128, 2], [1, 64]]),
                                              in1=AP(dl, 64, [[256, 128], [128, 2], [1, 64]]), op=ALU.mult),
             reads=[b["dl"]], writes=[b["dl"]])
        S.op("dve", lambda e: e.tensor_reduce(out=lam[:, 0:2], in_=AP(dl2, 0, [[128, 128], [64, 2], [1, 64]]), axis=AX.X,
                                              op=ALU.add), reads=[b["dl"]], writes=[b["lam"]])
        S.op("act", lambda e: e.activation(out=lam[:, 0:2], in_=lam[:, 0:2], func=AF.Exp), reads=[b["lam"]],
             writes=[b["lam"]])
        S.op("dve", lambda e: e.tensor_tensor(out=lam[:, 2:3], in0=lam[:, 1:2], in1=lam[:, 0:1], op=ALU.subtract),
             reads=[b["lam"]], writes=[b["lam"]])
        S.op("dve", lambda e: e.tensor_scalar(out=lam[:, 3:4], in0=lam[:, 2:3], scalar1=-lam_init, scalar2=None,
                                              op0=ALU.add), reads=[b["lam"]], writes=[b["lam"]])
        S.op("dve", lambda e: e.tensor_scalar(out=slc[:], in0=sl[:], scalar1=(1.0 - lam_init), scalar2=None, op0=ALU.mult),
             reads=[b["sl"]], writes=[b["sl"]])

        for h in range(4):
            S.dma("sp", qT[:], k.pCq.ap()[h * 128:(h + 1) * 128, :], reads=[k.B["pCq"]], writes=[b["qT"]])
            S.dma("sp", kT[:], k.pCk.ap()[h * 128:(h + 1) * 128, :], reads=[k.B["pCk"]], writes=[b["kT"]])
            S.dma("sp", vC[:], k.pCv.ap().rearrange("(n p) d -> p n d", p=128)[:, :, h * 128:(h + 1) * 128],
                  reads=[k.B["pCv"]], writes=[b["vC"]])
            S.op("dve", lambda e: e.memset(mx[:], 0.0), writes=[b["mx"]])
            for qi, (src, srcb) in enumerate([(qT, b["qT"]), (kT, b["kT"])]):
                for bi, (t0, W) in enumerate(BLOCKS):
                    sq, sqb = sqs.next()
                    S.op("act", lambda e, sq=sq, src=src, t0=t0, W=W: e.activation(out=sq[:, 0:W], in_=src[:, t0:t0 + W],
                                                                                   func=AF.Square), reads=[srcb],
                         writes=[sqb])
                    for c in range(2):
                        pr, prb = prs.next()
                        sel = k.sel0 if c == 0 else k.sel1
                        S.op("pe", lambda e, pr=pr, sel=sel, sq=sq, W=W: e.matmul(pr[:, 0:W], lhsT=sel[:], rhs=sq[:, 0:W],
                                                                                  start=True, stop=True),
                             reads=[sqb, k.Bc], writes=[prb])
                        S.op("dve", lambda e, pr=pr, qi=qi, c=c, bi=bi, W=W: e.tensor_reduce(
                            out=mx[:, qi, c, bi:bi + 1], in_=pr[:, 0:W], axis=AX.X, op=ALU.max), reads=[prb],
                            writes=[b["mx"]])
            S.op("dve", lambda e: e.tensor_reduce(out=nb[:, 0:4], in_=AP(mx, 0, [[64, 128], [16, 4], [1, 16]]), axis=AX.X,
                                                  op=ALU.max), reads=[b["mx"]], writes=[b["nb"]])
            S.op("dve", lambda e: e.tensor_tensor(out=nb[:, 4:6], in0=nb[:, 0:2], in1=nb[:, 2:4], op=ALU.mult),
                 reads=[b["nb"]], writes=[b["nb"]])
            S.op("act", lambda e: e.activation(out=nb[:, 4:6], in_=nb[:, 4:6], func=AF.Ln), reads=[b["nb"]], writes=[b["nb"]])
            S.op("act", lambda e: e.activation(out=nb[:, 4:6], in_=nb[:, 4:6], func=AF.Exp, scale=0.5), reads=[b["nb"]],
                 writes=[b["nb"]])
            S.op("dve", lambda e: e.tensor_scalar(out=nb[:, 6:8], in0=nb[:, 4:6], scalar1=-0.125, scalar2=None,
                                                  op0=ALU.mult), reads=[b["nb"]], writes=[b["nb"]])
            S.op("dve", lambda e: e.tensor_tensor(out=nb[:, 5:6], in0=nb[:, 6:7], in1=nb[:, 7:8], op=ALU.min),
                 reads=[b["nb"]], writes=[b["nb"]])
            seq = []
            for (t0, W) in BLOCKS:
                if t0 == 0:
                    if last:
                        continue
                    keys = list(range(0, 2))
                else:
                    keys = list(range(0, NT))
                for ji, j in enumerate(keys):
                    seq.append((t0, W, ji, j, len(keys)))

            def pair(t, W):
                return AP(t, 0, [[1024, 128], [512, 2], [1, W]])

            def qk_exp(t0, W, ji, j, nk):
                ps, psb = pss.next()
                S.op("pe", [lambda e, c=c: e.matmul(ps[:, c * 512:c * 512 + W],
                                                    lhsT=kT[c * 64:(c + 1) * 64, j * 128:(j + 1) * 128],
                                                    rhs=qT[c * 64:(c + 1) * 64, t0:t0 + W], start=True, stop=True)
                            for c in range(2)], reads=[b["kT"], b["qT"]], writes=[psb])
                pT, pTb = pTs.next()
                S.op("act", lambda e: e.activation(out=pair(pT, W), in_=pair(ps, W), func=AF.Exp, scale=0.125,
                                                   bias=nb[:, 5:6]), reads=[psb, b["nb"]], writes=[pTb])
                return pT, pTb

            prev = {}

            def av(t0, W, ji, j, nk, pT, pTb, h=h):
                S.op("pe", [lambda e, c=c: e.matmul(pos[c][0][:, 0:W], lhsT=vC[:, j, :], rhs=pT[:, c * 512:c * 512 + W],
                                                    start=(ji == 0), stop=(ji == nk - 1)) for c in range(2)],
                     reads=[b["vC"], pTb], writes=[pos[0][1], pos[1][1]])
                if ji % 2 == 0:
                    prev["p"] = (pT, pTb)
                    return
                pP, pPb = prev["p"]
                for c in range(2):
                    aP, aPb = accPs[c]
                    if ji == 1:
                        S.op("dve", lambda e, c=c, aP=aP: e.tensor_tensor(out=aP[:, 0:W], in0=pP[:, c * 512:c * 512 + W],
                                                                          in1=pT[:, c * 512:c * 512 + W], op=ALU.add),
                             reads=[pTb, pPb], writes=[aPb])
                    else:
                        t2, t2b = t2s.next()
                        S.op("dve", lambda e, c=c, t2=t2: e.tensor_tensor(out=t2[:, 0:W], in0=pP[:, c * 512:c * 512 + W],
                                                                          in1=pT[:, c * 512:c * 512 + W], op=ALU.add),
                             reads=[pTb, pPb], writes=[t2b])
                        S.op("dve", lambda e, aP=aP, t2=t2: e.tensor_tensor(out=aP[:, 0:W], in0=aP[:, 0:W], in1=t2[:, 0:W],
                                                                            op=ALU.add), reads=[t2b, aPb], writes=[aPb])
                if ji != nk - 1:
                    return
                for c in range(2):
                    aP, aPb = accPs[c]
                    po, pob = pos[c]
                    pl, plb = prs.next()
                    hi, hib = t2s.next()
                    lo, lob = t2s.next()
                    S.op("dve", lambda e, hi=hi, aP=aP: e.tensor_copy(out=hi[:, 0:W], in_=aP[:, 0:W]), reads=[aPb], writes=[hib])
                    S.op("dve", lambda e, hi=hi, lo=lo, aP=aP: e.tensor_tensor(out=lo[:, 0:W], in0=aP[:, 0:W], in1=hi[:, 0:W],
                                                                               op=ALU.subtract), reads=[aPb, hib], writes=[lob])
                    S.op("pe", [lambda e, pl=pl, hi=hi: e.matmul(pl[:, 0:W], lhsT=k.ones[:], rhs=hi[:, 0:W], start=True, stop=False),
                                lambda e, pl=pl, lo=lo: e.matmul(pl[:, 0:W], lhsT=k.ones[:], rhs=lo[:, 0:W], start=False, stop=True)],
                         reads=[hib, lob, k.Bc], writes=[plb])
                    oc, ocb = (o0, b["o0"]) if c == 0 else (o1, b["o1"])
                    S.op("act", lambda e, pl=pl: e.activation(out=rl[:, 0:W], in_=pl[:, 0:W], func=AF.Ln), reads=[plb],
                         writes=[b["rl"]])
                    S.op("act", lambda e: e.activation(out=rl[:, 0:W], in_=rl[:, 0:W], func=AF.Exp, scale=-1.0), reads=[b["rl"]],
                         writes=[b["rl"]])
                    S.op("dve", lambda e, po=po, oc=oc: e.tensor_tensor(out=oc[:, 0:W], in0=po[:, 0:W], in1=rl[:, 0:W],
                                                                        op=ALU.mult), reads=[pob, b["rl"]], writes=[ocb])
                S.op("dve", lambda e: e.scalar_tensor_tensor(out=o0[:, 0:W], in0=o1[:, 0:W], scalar=lam[:, 3:4], in1=o0[:, 0:W],
                                                             op0=ALU.mult, op1=ALU.add), reads=[b["o0"], b["o1"], b["lam"]],
                     writes=[b["o0"]])
                head_readout(k, S, st, "at", o0[:, 0:W], b["o0"], t0, W, slc[:, layer * 4 + h:layer * 4 + h + 1], b["sl"], None,
                             None, yc[:, t0:t0 + W], b["yc"], prs, sqs, tmps)

            LOOK = 1
            pend = []
            for idx in range(len(seq) + LOOK):
                if idx < len(seq):
                    pend.append(qk_exp(*seq[idx]))
                if idx >= LOOK:
                    pT_, pTb_ = pend.pop(0)
                    av(*seq[idx - LOOK], pT_, pTb_)
            if last:
                S.op("dve", lambda e: e.memset(yc[:, 0:256], 0.0), writes=[b["yc"]])
            S.dma("pool", k.ybr.ap()[2, h * 128:(h + 1) * 128, :], yc[:], reads=[b["yc"]], writes=[k.B["ybr2"]])
        S.emit()


def phase_merge(k, layer, last):
    nc, S, I = k.nc, k.S, k.I
    with ExitStack() as st:
        sb = lambda n, s, d: st.enter_context(nc.sbuf_tensor(U("mg_" + n), s, d))
        wb = sb("wb", [128, 3, 4, D], BF16)
        wo = sb("wo", [128, 8, D], BF16)
        bw = Buf()
        for i in range(3):
            S.dma("pool", wb[:, i, :, :], I["w_branch"].ap()[layer, i].rearrange("(k p) n -> p k n", p=128), writes=[bw])
        S.dma("pool", wo[:], I["w_out"].ap()[layer].rearrange("(k p) n -> p k n", p=128), writes=[bw])
        G1 = [sb("G1_%d" % w, [128, D], F32) for w in range(2)]
        A2 = [sb("A2_%d" % w, [128, D], F32) for w in range(2)]
        B2 = [sb("B2_%d" % w, [128, D], F32) for w in range(2)]
        mb = Buf()
        for w in range(2):
            if last and w == 1:
                continue
            load_rep(k, S, G1[w], mb, w, 2)
            load_rep(k, S, A2[w], mb, w, 4)
            load_rep(k, S, B2[w], mb, w, 3)
        ys = Rot([(sb("y%d" % i, [128, 3, 4, 512], BF16), Buf()) for i in range(2)])
        sgs = Rot([(sb("sg%d" % i, [128, 24, 512], BF16), Buf()) for i in range(2)])
        mTs = Rot([(sb("mT%d" % i, [128, 8, 512], BF16), Buf()) for i in range(2)])
        macc = sb("macc", [128, 512], F32)
        maccb = Buf()
        tmps = Rot([(sb("tmp%d" % i, [128, 512], F32), Buf()) for i in range(2)])
        xts = Rot([(sb("xt%d" % i, [128, D], F32), Buf()) for i in range(2)])
        junks = Rot([(sb("junk%d" % i, [128, D], F32), Buf()) for i in range(2)])
        hbs = Rot([(sb("hb%d" % i, [128, D], BF16), Buf()) for i in range(2)])
        sss = Rot([(sb("ss%d" % i, [128, 4], F32), Buf()) for i in range(2)])
        h2s = Rot([(sb("h2s%d" % i, [128, 8, 512], BF16), Buf()) for i in range(2)])
        pbs = Rot([(st.enter_context(nc.psum_tensor(U("mg_pb%d" % i), [128, 512], F32)), Buf()) for i in range(3)])
        pms = Rot([(st.enter_context(nc.psum_tensor(U("mg_pm%d" % i), [128, 512], F32)), Buf()) for i in range(2)])
        pts = Rot([(st.enter_context(nc.psum_tensor(U("mg_pt%d" % i), [128, 512], BF16)), Buf()) for i in range(2)])
        ybufs = [k.B["ybr0"], k.B["ybr1"], k.B["ybr2"]]
        def part1(t0, W):
            mT, mTb = mTs.next()
            y, yb_ = ys.next()
            sg, sgb = sgs.next()
            for i in range(3):
                S.dma("sp", y[:, i, :, 0:W], k.ybr.ap()[i].rearrange("(k p) t -> p k t", p=128)[:, :, t0:t0 + W],
                      reads=[ybufs[i]], writes=[yb_])
            S.dma("sp", sg[:, :, 0:W], k.pG.ap().rearrange("(k p) t -> p k t", p=128)[:, :, t0:t0 + W], reads=[k.B["pG"]],
                  writes=[sgb])
            for oc in range(8):
                for i in range(3):
                    pb, pbb = pbs.next()
                    S.op("pe", [lambda e, pb=pb, i=i, kk=kk, oc=oc, y=y: e.matmul(
                        pb[:, 0:W], lhsT=wb[:, i, kk, oc * 128:(oc + 1) * 128], rhs=y[:, i, kk, 0:W], start=(kk == 0),
                        stop=(kk == 3)) for kk in range(4)], reads=[bw, yb_], writes=[pbb])
                    if i == 0:
                        S.op("dve", lambda e, pb=pb, sg=sg, oc=oc: e.tensor_tensor(out=macc[:, 0:W], in0=pb[:, 0:W],
                                                                                   in1=sg[:, oc, 0:W], op=ALU.mult),
                             reads=[pbb, sgb], writes=[maccb])
                    else:
                        tmp, tb = tmps.next()
                        S.op("dve", lambda e, pb=pb, sg=sg, oc=oc, i=i, tmp=tmp: e.tensor_tensor(
                            out=tmp[:, 0:W], in0=pb[:, 0:W], in1=sg[:, i * 8 + oc, 0:W], op=ALU.mult), reads=[pbb, sgb],
                            writes=[tb])
                        if i == 1:
                            S.op("dve", lambda e, tmp=tmp: e.tensor_tensor(out=macc[:, 0:W], in0=macc[:, 0:W], in1=tmp[:, 0:W],
                                                                           op=ALU.add), reads=[tb, maccb], writes=[maccb])
                        else:
                            S.op("dve", lambda e, tmp=tmp, oc=oc: e.tensor_tensor(out=mT[:, oc, 0:W], in0=macc[:, 0:W],
                                                                                  in1=tmp[:, 0:W], op=ALU.add),
                                 reads=[tb, maccb], writes=[mTb])
            return mT, mTb

        def part2(t0, W, mT, mTb):
            w_ = 1 if t0 == 0 else 0
            h2, h2b = h2s.next()
            for ts in range(W // 128):
                row0 = t0 + ts * 128
                xt, xb = xts.next()
                S.dma("sp", xt[:], k.xs.ap()[row0:row0 + 128, :], reads=[k.Bxs[row0 // 128]], writes=[xb])
                for half in range(2):
                    pm, pmb = pms.next()
                    S.op("pe", [lambda e, pm=pm, kk=kk, ts=ts, half=half: e.matmul(
                        pm[:], lhsT=mT[:, kk, ts * 128:(ts + 1) * 128], rhs=wo[:, kk, half * 512:(half + 1) * 512],
                        start=(kk == 0), stop=(kk == 7)) for kk in range(8)], reads=[mTb, bw], writes=[pmb])
                    tmp, tb = tmps.next()
                    S.op("dve", lambda e, pm=pm, tmp=tmp, half=half: e.tensor_tensor(
                        out=tmp[:], in0=pm[:], in1=G1[w_][:, half * 512:(half + 1) * 512], op=ALU.mult), reads=[pmb, mb],
                        writes=[tb])
                    S.op("dve", lambda e, tmp=tmp, xt=xt, half=half: e.tensor_tensor(
                        out=xt[:, half * 512:(half + 1) * 512], in0=xt[:, half * 512:(half + 1) * 512], in1=tmp[:],
                        op=ALU.add), reads=[tb, xb], writes=[xb])
                S.dma("pool", k.xs.ap()[row0:row0 + 128, :], xt[:], reads=[xb], writes=[k.Bxs[row0 // 128]])
                junk, jb = junks.next()
                hb, hbb = hbs.next()
                ss, ssb = sss.next()
                rms_mod(S, xt, xb, A2[w_], mb, B2[w_], mb, hb, hbb, ss, ssb, junk, jb)
                for half in range(2):
                    pt, ptb = pts.next()
                    S.op("pe", [lambda e, pt=pt, hb=hb, j=j, half=half: e.transpose(
                        out=pt[:, j * 128:(j + 1) * 128], in_=hb[:, (half * 4 + j) * 128:(half * 4 + j + 1) * 128],
                        identity=k.ident[:]) for j in range(4)], reads=[hbb, k.Bc], writes=[ptb])
                    outap = h2[:, half * 4:half * 4 + 4, ts * 128:(ts + 1) * 128]
                    inap = AP(pt, 0, [[512, 128], [128, 4], [1, 128]])
                    S.op("act", lambda e, o=outap, a=inap: e.activation(out=o, in_=a, func=AF.Copy), reads=[ptb],
                         writes=[h2b])
            S.dma("pool", k.h2T.ap().rearrange("(k p) t -> p k t", p=128)[:, :, t0:t0 + W], h2[:, :, 0:W], reads=[h2b],
                  writes=[k.B["h2T"]])

        pend = None
        for (t0, W) in BLOCKS:
            if last and t0 == 0:
                continue
            mT_, mTb_ = part1(t0, W)
            if pend is not None:
                part2(*pend)
            pend = (t0, W, mT_, mTb_)
        if pend is not None:
            part2(*pend)
        S.emit()


def phase_ffn(k, layer, last):
    nc, S, I = k.nc, k.S, k.I
    moe = (layer % 2 == 1)
    if moe:
        FU = 4
        experts = [(I["moe_wg"].ap()[0, e], I["moe_wu"].ap()[0, e], I["moe_wd"].ap()[0, e], DFFE) for e in range(NEXP)]
        groups = [list(range(2, 18)), list(range(18, 34))]
    else:
        FU = 2
        experts = [(I["ffn_wg"].ap()[0], I["ffn_wu"].ap()[0], I["ffn_wd"].ap()[0], DFF)]
        groups = [list(range(0, 17)), list(range(17, 34))]
    with ExitStack() as st:
        sb = lambda n, s, d: st.enter_context(nc.sbuf_tensor(U("ff_" + n), s, d))
        NTG = 17
        acc = sb("acc", [128, NTG, D], F32)
        accb = [Buf() for _ in range(NTG)]
        h2 = sb("h2", [128, 8, NTG * 128], BF16)
        h2b = Buf()
        wgu = Rot([(sb("wgu%d" % i, [128, 8, 2, FU * 128], BF16), Buf()) for i in range(2)])
        wds = Rot([(sb("wd%d" % i, [128, FU, D], BF16), Buf()) for i in range(2)])
        acts = Rot([(sb("act%d" % i, [128, FU, 512], BF16), Buf()) for i in range(2)])
        sgs = Rot([(sb("sg%d" % i, [128, 512], F32), Buf()) for i in range(2)])
        G2 = sb("G2", [128, D], F32)
        gf = sb("gf", [128, D], F32)
        mb = Buf()
        xts = Rot([(sb("xt%d" % i, [128, D], F32), Buf()) for i in range(2)])
        junks = Rot([(sb("junk%d" % i, [128, D], F32), Buf()) for i in range(2)])
        sss = Rot([(sb("ss%d" % i, [128, 4], F32), Buf()) for i in range(2)])
        comb = sb("comb", [128, NTG, 8], F32)
        combb = Buf()
        wr32 = sb("wr32", [128, 8, 8], F32)
        wr = sb("wr", [128, 8, 8], BF16)
        wrb = Buf()
        rt = sb("rt", [128, 8, 8], F32)
        rtb = Buf()
        pgs = Rot([(st.enter_context(nc.psum_tensor(U("ff_pg%d" % i), [128, 512], F32)), Buf()) for i in range(2)])
        pus = Rot([(st.enter_context(nc.psum_tensor(U("ff_pu%d" % i), [128, 512], F32)), Buf()) for i in range(2)])
        pds = Rot([(st.enter_context(nc.psum_tensor(U("ff_pd%d" % i), [128, 512], F32)), Buf()) for i in range(3)])
        prt = st.enter_context(nc.psum_tensor(U("ff_pr"), [128, 512], F32))
        prtb = Buf()
        if moe:
            S.dma("sp", wr32[:], I["moe_router"].ap()[0].rearrange("(k p) e -> p k e", p=128), writes=[wrb])
            S.op("dve", lambda e: e.tensor_copy(out=wr[:], in_=wr32[:]), reads=[wrb], writes=[wrb])
        if last:
            S.dma("sp", gf[:], AP(I["g_final"], 0, [[0, 128], [1, D]]), writes=[mb])
        for gi, tiles in enumerate(groups):
            ntg = len(tiles)
            tok0 = tiles[0] * 128
            ntok = ntg * 128
            blocks = [(o, min(512, ntok - o)) for o in range(0, ntok, 512)]
            S.dma("sp", h2[:, :, 0:ntok], k.h2T.ap().rearrange("(k p) t -> p k t", p=128)[:, :, tok0:tok0 + ntok],
                  reads=[k.B["h2T"]], writes=[h2b])
            if moe:
                for ti in range(ntg):
                    S.op("pe", [lambda e, kk=kk, ti=ti: e.matmul(prt[:, 0:8], lhsT=h2[:, kk, ti * 128:(ti + 1) * 128],
                                                                 rhs=wr[:, kk, :], start=(kk == 0), stop=(kk == 7))
                                for kk in range(8)], reads=[h2b, wrb], writes=[prtb])
                    S.op("dve", lambda e: e.tensor_copy(out=rt[:, 0, :], in_=prt[:, 0:8]), reads=[prtb], writes=[rtb])
                    S.op("dve", lambda e: e.max(out=rt[:, 1, :], in_=rt[:, 0, :]), reads=[rtb], writes=[rtb])
                    S.op("dve", lambda e: e.tensor_scalar(out=rt[:, 2, :], in0=rt[:, 0, :], scalar1=rt[:, 1, 1:2], scalar2=None,
                                                          op0=ALU.is_ge), reads=[rtb], writes=[rtb])
                    S.op("dve", lambda e: e.tensor_scalar(out=rt[:, 5, 0:1], in0=rt[:, 1, 0:1], scalar1=-1.0, scalar2=None,
                                                          op0=ALU.mult), reads=[rtb], writes=[rtb])
                    S.op("act", lambda e: e.activation(out=rt[:, 3, :], in_=rt[:, 0, :], func=AF.Exp, bias=rt[:, 5, 0:1]),
                         reads=[rtb], writes=[rtb])
                    S.op("dve", lambda e: e.tensor_tensor(out=rt[:, 4, :], in0=rt[:, 3, :], in1=rt[:, 2, :], op=ALU.mult),
                         reads=[rtb], writes=[rtb])
                    S.op("dve", lambda e: e.tensor_reduce(out=rt[:, 5, 1:2], in_=rt[:, 4, :], axis=AX.X, op=ALU.add),
                         reads=[rtb], writes=[rtb])
                    S.op("dve", lambda e: e.reciprocal(out=rt[:, 5, 2:3], in_=rt[:, 5, 1:2]), reads=[rtb], writes=[rtb])
                    S.op("dve", lambda e, ti=ti: e.tensor_scalar(out=comb[:, ti, :], in0=rt[:, 4, :], scalar1=rt[:, 5, 2:3],
                                                                 scalar2=None, op0=ALU.mult), reads=[rtb], writes=[combb])
            def up(o, W, w, wbuf):
                at, atb = acts.next()
                for fc in range(FU):
                    pg, pgb = pgs.next()
                    pu, pub = pus.next()
                    S.op("pe", [lambda e, pg=pg, kk=kk, fc=fc: e.matmul(
                        pg[:, 0:W], lhsT=w[:, kk, 0, fc * 128:(fc + 1) * 128], rhs=h2[:, kk, o:o + W], start=(kk == 0),
                        stop=(kk == 7)) for kk in range(8)], reads=[wbuf, h2b], writes=[pgb])
                    S.op("pe", [lambda e, pu=pu, kk=kk, fc=fc: e.matmul(
                        pu[:, 0:W], lhsT=w[:, kk, 1, fc * 128:(fc + 1) * 128], rhs=h2[:, kk, o:o + W], start=(kk == 0),
                        stop=(kk == 7)) for kk in range(8)], reads=[wbuf, h2b], writes=[pub])
                    sg, sgb = sgs.next()
                    S.op("act", lambda e, pg=pg, sg=sg: e.activation(out=sg[:, 0:W], in_=pg[:, 0:W], func=AF.Silu),
                         reads=[pgb], writes=[sgb])
                    S.op("dve", lambda e, pu=pu, sg=sg, fc=fc: e.tensor_tensor(
                        out=at[:, fc, 0:W], in0=sg[:, 0:W], in1=pu[:, 0:W], op=ALU.mult), reads=[sgb, pub],
                        writes=[atb])
                return at, atb

            def down(o, W, at, atb, wd, wdb, ei, first):
                for ts in range(W // 128):
                    ti = o // 128 + ts
                    for half in range(2):
                        pd, pdb = pds.next()
                        S.op("pe", [lambda e, pd=pd, fc=fc, ts=ts, half=half: e.matmul(
                            pd[:], lhsT=at[:, fc, ts * 128:(ts + 1) * 128], rhs=wd[:, fc, half * 512:(half + 1) * 512],
                            start=(fc == 0), stop=(fc == FU - 1)) for fc in range(FU)], reads=[atb, wdb],
                            writes=[pdb])
                        av = acc[:, ti, half * 512:(half + 1) * 512]
                        if moe:
                            cs_ = comb[:, ti, ei:ei + 1]
                            if first:
                                S.op("dve", lambda e, pd=pd, av=av, cs_=cs_: e.tensor_scalar(
                                    out=av, in0=pd[:], scalar1=cs_, scalar2=None, op0=ALU.mult),
                                    reads=[pdb, combb], writes=[accb[ti]])
                            else:
                                S.op("dve", lambda e, pd=pd, av=av, cs_=cs_: e.scalar_tensor_tensor(
                                    out=av, in0=pd[:], scalar=cs_, in1=av, op0=ALU.mult, op1=ALU.add),
                                    reads=[pdb, combb, accb[ti]], writes=[accb[ti]])
                        else:
                            if first:
                                S.op("act", lambda e, pd=pd, av=av: e.activation(out=av, in_=pd[:], func=AF.Copy),
                                     reads=[pdb], writes=[accb[ti]])
                            else:
                                S.op("dve", lambda e, pd=pd, av=av: e.tensor_tensor(out=av, in0=av, in1=pd[:], op=ALU.add),
                                     reads=[pdb, accb[ti]], writes=[accb[ti]])

            first = True
            pending = None
            for ei, (wg_ap, wu_ap, wd_ap, dff) in enumerate(experts):
                wg_v = wg_ap.rearrange("(k p) n -> p k n", p=128)
                wu_v = wu_ap.rearrange("(k p) n -> p k n", p=128)
                wd_v = wd_ap.rearrange("(f p) n -> p f n", p=128)
                nfc = dff // 128
                for u0 in range(0, nfc, FU):
                    w, wbuf = wgu.next()
                    wd, wdb = wds.next()
                    S.dma("pool", w[:, :, 0, :], wg_v[:, :, u0 * 128:(u0 + FU) * 128], writes=[wbuf])
                    S.dma("pool", w[:, :, 1, :], wu_v[:, :, u0 * 128:(u0 + FU) * 128], writes=[wbuf])
                    S.dma("pool", wd[:], wd_v[:, u0:u0 + FU, :], writes=[wdb])
                    for (o, W) in blocks:
                        at, atb = up(o, W, w, wbuf)
                        if pending is not None:
                            down(*pending)
                        pending = (o, W, at, atb, wd, wdb, ei, first)
                    first = False
            if pending is not None:
                down(*pending)
            for ti, tile in enumerate(tiles):
                w_ = 1 if tile < 2 else 0
                if ti == 0 or (tile == 2 and not moe):
                    load_rep(k, S, G2, mb, w_, 5)
                xt, xb = xts.next()
                row0 = tile * 128
                S.dma("sp", xt[:], k.xs.ap()[row0:row0 + 128, :], reads=[k.Bxs[row0 // 128]], writes=[xb])
                S.op("dve", lambda e, ti=ti: e.tensor_tensor(out=acc[:, ti, :], in0=acc[:, ti, :], in1=G2[:], op=ALU.mult),
                     reads=[accb[ti], mb], writes=[accb[ti]])
                S.op("dve", lambda e, ti=ti, xt=xt: e.tensor_tensor(out=xt[:], in0=xt[:], in1=acc[:, ti, :], op=ALU.add),
                     reads=[accb[ti], xb], writes=[xb])
                if not last:
                    S.dma("pool", k.xs.ap()[row0:row0 + 128, :], xt[:], reads=[xb], writes=[k.Bxs[row0 // 128]])
                else:
                    junk, jb = junks.next()
                    ss, ssb = sss.next()
                    rms_mod(S, xt, xb, gf, mb, None, None, None, None, ss, ssb, junk, jb)
                    S.dma("pool", k.out.ap()[row0 - NCTX:row0 - NCTX + 128, :], junk[:], reads=[jb], writes=[k.B["out"]],
                          is_output=True)
        S.emit()


def _rope_tables():
    t = np.arange(NLAT)
    row = (t // 64).astype(np.float32)
    col = (t % 64).astype(np.float32)
    freqs = (10000.0 ** (-np.arange(16, dtype=np.float32) / 16)).astype(np.float32)
    C = np.ones((128, NTOK), np.float32)
    Sn = np.zeros((128, NTOK), np.float32)
    for p in range(128):
        d = p % 64
        axis, half, i = d // 32, (d % 32) // 16, d % 16
        ang = (row if axis == 0 else col) * freqs[i]
        C[p, NCTX:] = np.cos(ang)
        Sn[p, NCTX:] = np.sin(ang) * (-1.0 if half == 0 else 1.0)
    return C, Sn


def _swap_perm():
    p = np.arange(512)
    d = p % 64
    half = (d % 32) // 16
    return p + np.where(half == 0, 16, -16)


_NC_CACHE = {}


def prepare_inputs(inputs):
    f = lambda a: np.ascontiguousarray(np.asarray(a, dtype=np.float32))
    x, c, ctx, c_ctx = f(inputs["x"]), f(inputs["c"]), f(inputs["ctx"]), f(inputs["c_ctx"])
    w_in = f(inputs["w_in"])
    perm = _swap_perm()
    w_in_sw = np.ascontiguousarray(np.concatenate([w_in[:, :, 3584 + perm], w_in[:, :, 4096 + perm]], axis=2))
    ropeC, ropeS = _rope_tables()
    hl = f(inputs["hgrn_lb"])
    lbT = np.ascontiguousarray(hl.reshape(2, 2, 4, 128).transpose(3, 0, 1, 2))
    gnA = np.ascontiguousarray(f(inputs["hgrn_gnorm"]).reshape(2, 4, 128).transpose(2, 0, 1))
    sublnT = np.ascontiguousarray(f(inputs["diff_subln"]).reshape(2, 4, 128).transpose(2, 0, 1))
    wsT = np.ascontiguousarray(f(inputs["mlp_ws"]).transpose(0, 3, 1, 2))
    shared = {
        "w_ada": f(inputs["w_ada"]), "b_ada": f(inputs["b_ada"]), "g_norm1": f(inputs["g_norm1"]),
        "g_norm2": f(inputs["g_norm2"]), "w_in": w_in, "w_in_sw": w_in_sw, "lbT": lbT, "gnA": gnA,
        "vnorm": f(inputs["mlp_vnorm"]), "wsT": wsT, "bs": f(inputs["mlp_bs"]).reshape(2, 512),
        "dlam": f(inputs["diff_lambda"]).reshape(2, 256), "sublnT": sublnT, "w_branch": f(inputs["w_branch"]),
        "w_out": f(inputs["w_out"]), "ffn_wg": f(inputs["ffn_wg"]), "ffn_wu": f(inputs["ffn_wu"]),
        "ffn_wd": f(inputs["ffn_wd"]), "moe_router": f(inputs["moe_router"]), "moe_wg": f(inputs["moe_wg"]),
        "moe_wu": f(inputs["moe_wu"]), "moe_wd": f(inputs["moe_wd"]), "g_final": f(inputs["g_final"]),
        "ropeC": ropeC, "ropeS": ropeS,
    }
    in_maps = []
    for b in range(8):
        cT = np.stack([c[b].reshape(8, 128).T, c_ctx.reshape(8, 128).T], axis=2)
        m = dict(shared)
        m["x"] = x[b]
        m["ctx"] = ctx[b]
        m["cT"] = np.ascontiguousarray(cT)
        in_maps.append(m)
    return in_maps


def kernel(**inputs):
    if "nc" not in _NC_CACHE:
        _NC_CACHE["nc"] = build()
    nc = _NC_CACHE["nc"]
    in_maps = prepare_inputs(inputs)
    res = run_bass_kernel_spmd(nc, in_maps, core_ids=list(range(8)))
    return np.stack([np.asarray(r["out"], dtype=np.float32) for r in res.results], axis=0)
```
